# Optimizing a Trainium2 kernel written in Bass

```python
import jax, jax.numpy as jnp
from jax import lax
import numpy as np

D_MODEL = 1024
BATCH = 8
SEQ = 4096
DEPTH = 1

HEAD_DIM = D_MODEL // 16
RET_HEADS = 8
NSA_HEADS = 8
NSA_KV_GROUPS = 2
HEADS_PER_GROUP = NSA_HEADS // NSA_KV_GROUPS
RET_WIDTH = RET_HEADS * HEAD_DIM
NSA_WIDTH = NSA_HEADS * HEAD_DIM
MIX_WIDTH = RET_WIDTH + NSA_WIDTH
KV_WIDTH = NSA_KV_GROUPS * HEAD_DIM
RET_CHUNK = 128
RET_ROPE_BASE = 10000.0
NSA_ROPE_BASE = 500000.0
ROPE_DIM = HEAD_DIM // 4
CMP_BLOCK = 32
CMP_STRIDE = 16
CMP_HIDDEN = 256
SLC_BLOCK = 64
SLC_TOPK = 16
WINDOW = 512
NSA_QBLK = 64
NEG = -1e30
BIG = 1e9
EPS = 1e-6
SPLITS = (RET_WIDTH, RET_WIDTH, RET_WIDTH, RET_WIDTH, NSA_WIDTH, 6 * KV_WIDTH, 3 * NSA_HEADS, NSA_WIDTH)
PROJ_WIDTH = sum(SPLITS)

kernel_name = "hymba_retention_nsa_hybrid"


def rmsnorm(x, g):
    xf = x.astype(jnp.float32)
    y = xf * lax.rsqrt(jnp.mean(xf * xf, axis=-1, keepdims=True) + EPS) * g.astype(jnp.float32)
    return y.astype(x.dtype)


def rotary(x, pos, base, rot_dim):
    half = rot_dim // 2
    inv = jnp.power(base, -jnp.arange(half, dtype=jnp.float32) / half)
    ang = pos.astype(jnp.float32)[..., None] * inv
    cos = jnp.cos(ang)[:, :, None, :]
    sin = jnp.sin(ang)[:, :, None, :]
    x1 = x[..., :half].astype(jnp.float32)
    x2 = x[..., half:rot_dim].astype(jnp.float32)
    out = jnp.concatenate([(x1 * cos - x2 * sin).astype(x.dtype),
                           (x1 * sin + x2 * cos).astype(x.dtype),
                           x[..., rot_dim:]], axis=-1)
    return out


def masked_softmax(s, mask):
    s = jnp.where(mask, s.astype(jnp.float32), NEG)
    return jax.nn.softmax(s, axis=-1) * mask


def retention(q, k, v, pos):
    B, S, H, Dh = q.shape
    q = rotary(q, pos, RET_ROPE_BASE, Dh).astype(jnp.float32) * (Dh ** -0.5)
    k = rotary(k, pos, RET_ROPE_BASE, Dh).astype(jnp.float32)
    v = v.astype(jnp.float32)
    log_g = jnp.log1p(-jnp.power(2.0, -5.0 - jnp.arange(H, dtype=jnp.float32)))
    C = RET_CHUNK
    N = S // C
    qc = q.reshape(B, N, C, H, Dh)
    kc = k.reshape(B, N, C, H, Dh)
    vc = v.reshape(B, N, C, H, Dh)
    idx = jnp.arange(C, dtype=jnp.float32)
    diff = idx[:, None] - idx[None, :]
    decay = jnp.where(diff[None] >= 0, jnp.exp(jnp.maximum(diff, 0.0)[None] * log_g[:, None, None]), 0.0)
    scores = jnp.einsum('bnihd,bnjhd->bnhij', qc, kc) * decay
    inner = jnp.einsum('bnhij,bnjhe->bnihe', scores, vc)
    zeta = jnp.exp((C - 1 - idx)[None, :] * log_g[:, None])
    kv_chunk = jnp.einsum('bnjhd,hj,bnjhe->nbhde', kc, zeta, vc)
    chunk_decay = jnp.exp(C * log_g)[None, :, None, None]

    def step(R, kv):
        return R * chunk_decay + kv, R

    _, R_prev = lax.scan(step, jnp.zeros((B, H, Dh, Dh), jnp.float32), kv_chunk)
    xi = jnp.exp((idx + 1.0)[None, :] * log_g[:, None])
    cross = jnp.einsum('bnihd,nbhde,hi->bnihe', qc, R_prev, xi)
    o = (inner + cross).reshape(B, S, H, Dh)
    mu = jnp.mean(o, axis=-1, keepdims=True)
    var = jnp.mean(jnp.square(o - mu), axis=-1, keepdims=True)
    return (o - mu) * lax.rsqrt(var + 1e-5)


def compress(t, pe, w1, w2):
    B, S, G, Dh = t.shape
    Nc = (S - CMP_BLOCK) // CMP_STRIDE + 1
    cidx = np.arange(Nc)[:, None] * CMP_STRIDE + np.arange(CMP_BLOCK)[None, :]
    blocks = t[:, cidx] + pe[None, None, :, None, :].astype(t.dtype)
    flat = blocks.transpose(0, 1, 3, 2, 4).reshape(B, Nc, G, CMP_BLOCK * Dh)
    return jax.nn.silu(flat @ w1) @ w2


def nsa(q, kv_all, gate_logits, pos, pe_k, w1_k, w2_k, pe_v, w1_v, w2_v):
    B, S, Hq, Dh = q.shape
    G, hpg = NSA_KV_GROUPS, HEADS_PER_GROUP
    kv = kv_all.reshape(B, S, 6, G, Dh)
    k_c, v_c, k_s, v_s, k_w, v_w = [kv[:, :, i] for i in range(6)]
    q = rotary(q, pos, NSA_ROPE_BASE, ROPE_DIM) * (Dh ** -0.5)
    k_s = rotary(k_s, pos, NSA_ROPE_BASE, ROPE_DIM)
    k_w = rotary(k_w, pos, NSA_ROPE_BASE, ROPE_DIM)
    Nc = (S - CMP_BLOCK) // CMP_STRIDE + 1
    NS = S // SLC_BLOCK
    topk = min(SLC_TOPK, NS)
    cmp_start = np.arange(Nc) * CMP_STRIDE
    cmp_end = cmp_start + CMP_BLOCK - 1
    slc_start = np.arange(NS) * SLC_BLOCK
    overlap = jnp.asarray(((cmp_start[:, None] <= slc_start[None, :] + SLC_BLOCK - 1) &
                           (cmp_end[:, None] >= slc_start[None, :])).astype(np.float32))
    kc = rotary(compress(k_c, pe_k, w1_k, w2_k), pos[:, cmp_end], NSA_ROPE_BASE, ROPE_DIM)
    vc = compress(v_c, pe_v, w1_v, w2_v)
    kcT = kc.transpose(0, 2, 1, 3)
    vcT = vc.transpose(0, 2, 1, 3)
    qg = q.reshape(B, S, G, hpg, Dh).transpose(0, 2, 3, 1, 4)
    ks_blk = k_s.reshape(B, NS, SLC_BLOCK, G, Dh).transpose(0, 3, 1, 2, 4)
    vs_blk = v_s.reshape(B, NS, SLC_BLOCK, G, Dh).transpose(0, 3, 1, 2, 4)
    pad = ((0, 0), (0, 0), (WINDOW, 0), (0, 0))
    kw_pad = jnp.pad(k_w.transpose(0, 2, 1, 3), pad)
    vw_pad = jnp.pad(v_w.transpose(0, 2, 1, 3), pad)
    bidx = jnp.arange(B)[:, None, None, None]
    gidx = jnp.arange(G)[None, :, None, None]
    blk_ids = jnp.arange(NS)

    def block(bi):
        t0 = bi * NSA_QBLK
        qb = lax.dynamic_slice_in_dim(qg, t0, NSA_QBLK, axis=3)
        tq = t0 + jnp.arange(NSA_QBLK)
        s = jnp.einsum('bghqd,bgnd->bghqn', qb, kcT)
        p_cmp = masked_softmax(s, cmp_end[None, :] <= tq[:, None])
        o_cmp = jnp.einsum('bghqn,bgnd->bghqd', p_cmp, vcT.astype(jnp.float32))
        imp = jnp.einsum('bghqn,ns->bgqs', p_cmp, overlap)
        cur = tq // SLC_BLOCK
        valid = blk_ids[None, :] <= cur[:, None]
        forced = (blk_ids[None, :] == 0) | (blk_ids[None, :] == cur[:, None]) | (blk_ids[None, :] == cur[:, None] - 1)
        score = jnp.where(forced, BIG, jnp.where(valid, imp, NEG))
        top_val, top_idx = lax.top_k(score, topk)
        kg = ks_blk[bidx, gidx, top_idx]
        vg = vs_blk[bidx, gidx, top_idx]
        s = jnp.einsum('bghqd,bgqnkd->bghqnk', qb, kg).reshape(B, G, hpg, NSA_QBLK, topk * SLC_BLOCK)
        tok = top_idx[..., None] * SLC_BLOCK + jnp.arange(SLC_BLOCK)
        m = (tok <= tq[None, None, :, None, None]) & (top_val > 0.5 * NEG)[..., None]
        p_slc = masked_softmax(s, m.reshape(B, G, 1, NSA_QBLK, topk * SLC_BLOCK))
        o_slc = jnp.einsum('bghqm,bgqmd->bghqd', p_slc,
                           vg.reshape(B, G, NSA_QBLK, topk * SLC_BLOCK, Dh).astype(jnp.float32))
        kwb = lax.dynamic_slice_in_dim(kw_pad, t0, WINDOW + NSA_QBLK, axis=2)
        vwb = lax.dynamic_slice_in_dim(vw_pad, t0, WINDOW + NSA_QBLK, axis=2)
        kp = t0 - WINDOW + jnp.arange(WINDOW + NSA_QBLK)
        mw = (kp[None, :] >= 0) & (kp[None, :] <= tq[:, None]) & (tq[:, None] - kp[None, :] < WINDOW)
        s = jnp.einsum('bghqd,bgkd->bghqk', qb, kwb)
        p_win = masked_softmax(s, mw)
        o_win = jnp.einsum('bghqk,bgkd->bghqd', p_win, vwb.astype(jnp.float32))
        return o_cmp, o_slc, o_win

    o_cmp, o_slc, o_win = lax.map(block, jnp.arange(S // NSA_QBLK))
    to_bshd = lambda o: o.transpose(1, 0, 4, 2, 3, 5).reshape(B, S, Hq, Dh)
    g = jax.nn.sigmoid(gate_logits.astype(jnp.float32)).reshape(B, S, Hq, 3)
    o = g[..., 0:1] * to_bshd(o_cmp) + g[..., 1:2] * to_bshd(o_slc) + g[..., 2:3] * to_bshd(o_win)
    return o


def setup_inputs(seed: int = 0) -> dict:
    key = jax.random.key(seed)
    ks = jax.random.split(key, 20)
    f32 = jnp.float32
    nrm = lambda k, shape, scale: jax.random.normal(k, shape, f32) * scale
    x = jax.random.normal(ks[0], (BATCH, SEQ, D_MODEL), f32)
    c = jax.random.normal(ks[1], (BATCH, D_MODEL), f32)
    start = jax.random.randint(ks[2], (BATCH, 1), 0, 1024, dtype=jnp.int32)
    positions = start + jnp.arange(SEQ, dtype=jnp.int32)[None, :]
    return {
        "x": x,
        "c": c,
        "positions": positions,
        "w_ada": nrm(ks[3], (DEPTH, D_MODEL, 3 * D_MODEL), 0.5 * D_MODEL ** -0.5),
        "b_ada": nrm(ks[4], (DEPTH, 3 * D_MODEL), 0.02),
        "g_pre": 1.0 + nrm(ks[5], (DEPTH, D_MODEL), 0.05),
        "g_post": 1.0 + nrm(ks[6], (DEPTH, D_MODEL), 0.05),
        "w_in": nrm(ks[7], (DEPTH, D_MODEL, PROJ_WIDTH), D_MODEL ** -0.5),
        "w_out": nrm(ks[8], (DEPTH, MIX_WIDTH, D_MODEL), MIX_WIDTH ** -0.5),
        "cmp_pe_k": nrm(ks[9], (DEPTH, CMP_BLOCK, HEAD_DIM), 0.1),
        "cmp_w1_k": nrm(ks[10], (DEPTH, CMP_BLOCK * HEAD_DIM, CMP_HIDDEN), (CMP_BLOCK * HEAD_DIM) ** -0.5),
        "cmp_w2_k": nrm(ks[11], (DEPTH, CMP_HIDDEN, HEAD_DIM), CMP_HIDDEN ** -0.5),
        "cmp_pe_v": nrm(ks[12], (DEPTH, CMP_BLOCK, HEAD_DIM), 0.1),
        "cmp_w1_v": nrm(ks[13], (DEPTH, CMP_BLOCK * HEAD_DIM, CMP_HIDDEN), (CMP_BLOCK * HEAD_DIM) ** -0.5),
        "cmp_w2_v": nrm(ks[14], (DEPTH, CMP_HIDDEN, HEAD_DIM), CMP_HIDDEN ** -0.5),
    }


def reference(x, c, positions, w_ada, b_ada, g_pre, g_post, w_in, w_out,
              cmp_pe_k, cmp_w1_k, cmp_w2_k, cmp_pe_v, cmp_w1_v, cmp_w2_v):
    B, S, _ = x.shape
    offsets = np.cumsum(SPLITS)[:-1].tolist()
    for l in range(DEPTH):
        mod = jax.nn.silu(c) @ w_ada[l] + b_ada[l]
        shift, scale, gate = jnp.split(mod, 3, axis=-1)
        h = rmsnorm(x, g_pre[l]) * (1.0 + scale[:, None, :]) + shift[:, None, :]
        proj = h @ w_in[l]
        rq, rk, rv, rg, nq, nkv, ngl, ng = jnp.split(proj, offsets, axis=-1)
        hd = lambda t, H: t.reshape(B, S, H, HEAD_DIM)
        ret = retention(hd(rq, RET_HEADS), hd(rk, RET_HEADS), hd(rv, RET_HEADS), positions)
        ret = ret.reshape(B, S, RET_WIDTH) * jax.nn.silu(rg.astype(jnp.float32))
        att = nsa(hd(nq, NSA_HEADS), nkv, ngl, positions,
                  cmp_pe_k[l], cmp_w1_k[l], cmp_w2_k[l], cmp_pe_v[l], cmp_w1_v[l], cmp_w2_v[l])
        att = att.reshape(B, S, NSA_WIDTH) * jax.nn.silu(ng.astype(jnp.float32))
        mixed = jnp.concatenate([ret, att], axis=-1).astype(x.dtype)
        y = rmsnorm(mixed @ w_out[l], g_post[l])
        x = x + gate[:, None, :].astype(x.dtype) * y
    return x
```

```python
import numpy as np
from contextlib import ExitStack
import ml_dtypes
import concourse.bass as bass
import concourse.mybir as mybir
from concourse.bass_utils import run_bass_kernel_spmd

F32 = mybir.dt.float32
BF16 = mybir.dt.bfloat16
I32 = mybir.dt.int32
AF = mybir.ActivationFunctionType
ALU = mybir.AluOpType
AX = mybir.AxisListType

S_LEN = 4096
D = 1024
NT = 32
NS = 8
PW = 3864
PI = float(np.pi)
DBG_T = 0


class Buf:
    __slots__ = ("w", "r", "sem", "semcnt", "name", "uid", "excl", "dram")
    _n = [0]

    def __init__(self, name=""):
        Buf._n[0] += 1
        self.uid = Buf._n[0]
        self.w = None
        self.r = {}
        self.sem = None
        self.semcnt = 0
        self.name = name
        self.excl = False
        self.dram = False


class V:
    __slots__ = ("ap", "bufs")

    def __init__(self, ap, bufs):
        self.ap = ap
        self.bufs = bufs if isinstance(bufs, (list, tuple)) else [bufs]

    def __getitem__(self, k):
        return V(self.ap[k], self.bufs)

    def re(self, s, **kw):
        return V(self.ap.rearrange(s, **kw), self.bufs)

    def bc(self, shape):
        return V(self.ap.to_broadcast(list(shape)), self.bufs)

    def cast(self, dt):
        return V(self.ap.bitcast(dt), self.bufs)

    def sub(self, bufs):
        return V(self.ap, bufs)


COMPUTE = ("pe", "act", "dve", "pool")


class Sched:
    def __init__(self, nc, stack):
        self.nc = nc
        self.stack = stack
        self.streams = {e: [] for e in ("pe", "act", "dve", "pool", "sp")}
        self.cnt = {e: 0 for e in COMPUTE}
        self.waited = {e: {} for e in self.streams}
        self.sems = {e: stack.enter_context(nc.semaphore("s_" + e)) for e in COMPUTE}
        self.dma_sems = {}
        self.dma_bufs = []
        self.needed = {e: set() for e in COMPUTE}
        self.out_toks = []

    def _deps(self, eng, reads, writes):
        deps = {}

        def add(tok):
            k, i = tok
            if deps.get(k, -1) < i:
                deps[k] = i

        for b in reads:
            if b.excl:
                for k, i in b.r.items():
                    if k != eng:
                        add((k, i))
            if b.w is not None:
                if b.w[0] == eng and eng == "pe":
                    continue
                add(b.w)
        for b in writes:
            if b.w is not None and (b.w[0] != eng or eng != "pe"):
                add(b.w)
            for k, i in b.r.items():
                if k != eng or eng != "pe":
                    add((k, i))
        return self._emit_waits(eng, deps)

    def _emit_waits(self, eng, deps):
        waits = []
        wd = self.waited[eng]
        for k, i in deps.items():
            if wd.get(k, -1) < i:
                wd[k] = i
                waits.append((k, i))
                if k in COMPUTE:
                    self.needed[k].add(i)
        return waits

    def _mark(self, tok, reads, writes):
        k, i = tok
        for b in reads:
            if b.r.get(k, -1) < i:
                b.r[k] = i
        for b in writes:
            b.w = tok
            b.r = {}

    def op(self, eng, fn, reads, writes):
        rb = [b for v in reads if isinstance(v, V) for b in v.bufs]
        wb = [b for v in writes if isinstance(v, V) for b in v.bufs]
        waits = self._deps(eng, rb, wb)
        self.cnt[eng] += 1
        idx = self.cnt[eng]
        self.streams[eng].append(("c", waits, fn, idx))
        self._mark((eng, idx), rb, wb)

    def dma(self, q, out, in_, sembuf=None, is_output=False, **kw):
        rb = list(in_.bufs)
        wb = list(out.bufs)
        sb = sembuf or wb[0]
        if sb.sem is None:
            sb.sem = self.stack.enter_context(self.nc.semaphore("d_%d" % sb.uid))
            self.dma_sems[sb.uid] = sb.sem
            self.dma_bufs.append(sb)
        waits = self._deps(q, rb, [b for b in wb if not b.dram])
        sb.semcnt += 16
        tok = (("dma", sb.uid), sb.semcnt)
        self.streams[q].append(("d", waits, (out.ap, in_.ap, kw), sb.sem))
        self._mark(tok, rb, wb)
        if is_output:
            self.out_toks.append(tok)
        return tok

    def barrier(self):
        deps = {}
        for e in COMPUTE:
            if self.cnt[e] > 0:
                deps[e] = self.cnt[e]
        for b in self.dma_bufs:
            deps[("dma", b.uid)] = b.semcnt
        for e in self.streams:
            w = self._emit_waits(e, dict(deps))
            if w:
                self.streams[e].append(("w", w, None, None))

    def emit(self, block):
        fin = self._emit_waits("sp", {k: i for k, i in self.out_toks})
        self.streams["sp"].append(("w", fin, None, None))
        pref = {}
        for e in COMPUTE:
            m = {}
            c = 0
            for i in range(1, self.cnt[e] + 1):
                if i in self.needed[e]:
                    c += 1
                    m[i] = c
            pref[e] = m
        names = {"pe": "tensor", "act": "scalar", "dve": "vector", "pool": "gpsimd", "sp": "sync"}
        for e, st in self.streams.items():
            def body(eng, e=e, st=st):
                for kind, waits, fn, extra in st:
                    for k, i in waits:
                        if k in COMPUTE:
                            eng.wait_ge(self.sems[k], pref[k][i])
                        else:
                            eng.wait_ge(self.dma_sems[k[1]], i)
                    if kind == "c":
                        ins = fn(eng)
                        if extra in self.needed[e]:
                            ins.then_inc(self.sems[e], 1)
                    elif kind == "d":
                        oap, iap, kw = fn
                        eng.dma_start(out=oap, in_=iap, **kw).then_inc(extra, 16)
            getattr(block, names[e])(body)

    def mm(self, out, lhsT, rhs, start=True, stop=True, **kw):
        self.op("pe", lambda e: e.matmul(out.ap, lhsT.ap, rhs.ap, start=start, stop=stop, **kw),
                [lhsT, rhs], [out])

    def tr(self, out, in_, ident):
        self.op("pe", lambda e: e.transpose(out.ap, in_.ap, ident.ap), [in_, ident], [out])

    def act(self, out, in_, func, bias=None, scale=None, accum_out=None):
        kw = {}
        rd = [in_]
        if bias is not None:
            kw["bias"] = bias.ap if isinstance(bias, V) else bias
            rd.append(bias)
        if scale is not None:
            kw["scale"] = scale.ap if isinstance(scale, V) else scale
            rd.append(scale)
        wr = [out]
        if accum_out is not None:
            kw["accum_out"] = accum_out.ap
            wr.append(accum_out)
        self.op("act", lambda e: e.activation(out.ap, in_.ap, func, **kw), rd, wr)

    def tt(self, out, in0, in1, op, eng="dve"):
        self.op(eng, lambda e: e.tensor_tensor(out.ap, in0.ap, in1.ap, op), [in0, in1], [out])

    def ts(self, out, in0, s1, s2, op0, op1=None, eng="dve"):
        a1 = s1.ap if isinstance(s1, V) else s1
        a2 = s2.ap if isinstance(s2, V) else s2
        kw = {}
        if op1 is not None:
            kw["op1"] = op1
        self.op(eng, lambda e: e.tensor_scalar(out.ap, in0.ap, a1, a2, op0, **kw), [in0, s1, s2], [out])

    def stt(self, out, in0, scalar, in1, op0, op1, eng="dve"):
        a = scalar.ap if isinstance(scalar, V) else scalar
        self.op(eng, lambda e: e.scalar_tensor_tensor(out.ap, in0.ap, a, in1.ap, op0, op1),
                [in0, scalar, in1], [out])

    def copy(self, out, in_, eng="dve"):
        if eng == "act":
            self.op("act", lambda e: e.copy(out.ap, in_.ap), [in_], [out])
        else:
            self.op(eng, lambda e: e.tensor_copy(out.ap, in_.ap), [in_], [out])

    def memset(self, out, val, eng="dve"):
        self.op(eng, lambda e: e.memset(out.ap, val), [], [out])

    def reduce(self, out, in_, op, eng="dve"):
        self.op(eng, lambda e: e.tensor_reduce(out.ap, in_.ap, AX.X, op), [in_], [out])

    def recip(self, out, in_):
        self.op("dve", lambda e: e.reciprocal(out.ap, in_.ap), [in_], [out])

    def max8(self, out, in_):
        self.op("dve", lambda e: e.max(out.ap, in_.ap), [in_], [out])

    def match_replace(self, out, to_replace, values, imm):
        self.op("dve", lambda e: e.match_replace(out.ap, to_replace.ap, values.ap, imm),
                [to_replace, values], [out])


def make_consts():
    bf = ml_dtypes.bfloat16
    c = {}
    c["ident_f"] = np.eye(128, dtype=np.float32)
    c["ident_b"] = np.eye(128, dtype=np.float32).astype(bf)
    c["ones_f"] = np.ones((128, 128), np.float32)
    permR = np.zeros((128, 128), np.float32)
    permN = np.zeros((128, 128), np.float32)
    invR = np.zeros((128, 1), np.float32)
    invN = np.zeros((128, 1), np.float32)
    ir = np.power(np.float32(10000.0), -np.arange(32, dtype=np.float32) / np.float32(32))
    inn = np.power(np.float32(500000.0), -np.arange(8, dtype=np.float32) / np.float32(8))
    for m in range(128):
        blk, d = (m // 64) * 64, m % 64
        if d < 32:
            permR[blk + d + 32, m] = -1.0
        else:
            permR[blk + d - 32, m] = 1.0
        invR[m, 0] = ir[d % 32]
        if d < 8:
            permN[blk + d + 8, m] = -1.0
            invN[m, 0] = inn[d]
        elif d < 16:
            permN[blk + d - 8, m] = 1.0
            invN[m, 0] = inn[d - 8]
    c["permR"] = permR.astype(bf)
    c["permN"] = permN.astype(bf)
    c["inv2"] = np.concatenate([invR, invN], axis=1).astype(np.float64) / (2 * np.pi)
    c["inv2"] = c["inv2"].astype(np.float32)
    H = 8
    log_g = np.log1p(-np.power(2.0, -5.0 - np.arange(H, dtype=np.float64)))
    idx = np.arange(128, dtype=np.float64)
    dec = np.zeros((128, H, 128), np.float32)
    for h in range(H):
        diff = idx[None, :] - idx[:, None]
        dec[:, h, :] = np.where(diff >= 0, np.exp(np.maximum(diff, 0) * log_g[h]), 0.0)
    c["decayT"] = dec
    c["xi"] = np.exp((idx[:, None] + 1.0) * log_g[None, :]).astype(np.float32)
    c["zeta"] = np.exp((127.0 - idx[:, None]) * log_g[None, :]).astype(np.float32)
    cd = np.exp(128.0 * log_g)
    cdv = np.zeros((128, 4), np.float32)
    for m in range(128):
        for p in range(4):
            cdv[m, p] = cd[2 * p + m // 64]
    c["cdv"] = cdv
    keys = np.arange(S_LEN)
    c["onehot"] = (keys[None, :] // 64 == np.arange(64)[:, None]).astype(np.float32).astype(bf)
    kk = np.arange(128)[:, None]
    qq = np.arange(128)[None, :]
    c["tri"] = (-30000.0 * (1.0 - np.stack([(kk <= qq), (kk > qq)], axis=1).astype(np.float32))).astype(bf)
    cm = np.zeros((NS, 2, 128, 512), np.float32)
    for t in range(NS):
        for nt in range(2):
            n = np.arange(128)[:, None] + 128 * nt
            q = 512 * t + np.arange(512)[None, :]
            cm[t, nt] = ((16 * n + 31 <= q) & (n < 255))
    c["cmpmask"] = (-30000.0 * (1.0 - cm)).astype(bf)
    fb = np.zeros((NT, 128, 64), np.float32)
    for qt in range(NT):
        for ql in range(128):
            cur = (qt * 128 + ql) // 64
            fb[qt, ql, :] = np.where(np.arange(64) > cur, -1e30, 0.0)
            fb[qt, ql, 0] = 1e9
            if cur - 1 >= 0:
                fb[qt, ql, cur - 1] = 3e9
            fb[qt, ql, cur] = 2e9
    c["fb"] = fb
    Nc = 255
    cs = np.arange(Nc) * 16
    ce = cs + 31
    ss = np.arange(64) * 64
    ov = ((cs[:, None] <= ss[None, :] + 63) & (ce[:, None] >= ss[None, :])).astype(np.float32)
    ova = np.zeros((256, 65), np.float32)
    ova[:Nc, :64] = ov
    ova[:Nc, 64] = 1.0
    c["ovl"] = ova.astype(bf)
    return c


CONST_SPECS = None


def build(debug=()):
    nc = bass.Bass("TRN2", target_bir_lowering=False)
    consts = make_consts()
    dts = {np.dtype(np.float32): F32, np.dtype(ml_dtypes.bfloat16): BF16}

    def dbuf(name):
        b = Buf(name)
        b.dram = True
        return b

    def din(name, shape, dt):
        return V(nc.dram_tensor(name, list(shape), dt, kind="ExternalInput").ap(), dbuf(name))

    x_d = din("x", [S_LEN, D], F32)
    c_d = din("c", [D], F32)
    pos_d = din("positions", [1, S_LEN], I32)
    wada_d = din("w_ada", [D, 3 * D], F32)
    bada_d = din("b_ada", [3 * D], F32)
    gpre_d = din("g_pre", [D], F32)
    gpost_d = din("g_post", [D], F32)
    win_d = din("w_in", [D, PW], F32)
    wout_d = din("w_out", [D, D], F32)
    pek_d = din("cmp_pe_k", [2048], F32)
    w1k_d = din("cmp_w1_k", [2048, 256], F32)
    w2k_d = din("cmp_w2_k", [256, 64], F32)
    pev_d = din("cmp_pe_v", [2048], F32)
    w1v_d = din("cmp_w1_v", [2048, 256], F32)
    w2v_d = din("cmp_w2_v", [256, 64], F32)
    cd_ = {k: din("k_" + k, v.shape, dts[v.dtype]) for k, v in consts.items()}
    out_d = V(nc.dram_tensor("out", [S_LEN, D], F32, kind="ExternalOutput").ap(), dbuf("out"))
    dbg = {}

    def dbg_out(name, shape, dt=F32):
        dbg[name] = V(nc.dram_tensor("dbg_" + name, list(shape), dt, kind="ExternalOutput").ap(), Buf(name))
        return dbg[name]

    def scratch(name, shape, dt):
        return V(nc.dram_tensor(name, list(shape), dt).ap(), dbuf(name))

    tabs_d = scratch("tabs_s", [4, 128, S_LEN], F32)
    qT_d = scratch("qT_s", [8, 64, S_LEN], BF16)
    sng_d = scratch("sng_s", [S_LEN, 512], BF16)
    gates_d = scratch("gates_s", [S_LEN, 24], F32)
    mret_d = scratch("mret_s", [S_LEN, 512], BF16)
    ks_d = scratch("ks_s", [64, 2, S_LEN], BF16)
    kw_d = scratch("kw_s", [64, 2, S_LEN], BF16)
    vs_d = scratch("vs_s", [S_LEN, 2, 64], BF16)
    vw_d = scratch("vw_s", [S_LEN, 2, 64], BF16)

    with ExitStack() as st0:
        S = Sched(nc, st0)

        def alloc(st, name, shape, dt):
            t = st.enter_context(nc.sbuf_tensor(name, list(shape), dt))
            return V(t[:], Buf(name))

        ps = []
        for i in range(8):
            t = st0.enter_context(nc.psum_tensor("ps%d" % i, [128, 512], F32))
            ps.append(V(t[:], Buf("ps%d" % i)))
            ps[-1].bufs[0].excl = True

        K = {}
        deferred = []
        for name in ("ident_f", "ident_b", "ones_f", "permR", "permN", "inv2", "decayT", "xi", "zeta", "cdv",
                     "tri", "ovl"):
            v = consts[name]
            if name == "ovl":
                K[name] = alloc(st0, "c_" + name, [128, 2, 65], BF16)
                deferred.append(lambda name=name: S.dma("sp", K[name], cd_[name].re("(t p) c -> p t c", p=128)))
            else:
                K[name] = alloc(st0, "c_" + name, v.shape, dts[v.dtype])
                deferred.append(lambda name=name: S.dma("sp", K[name], cd_[name]))
        kvcT = alloc(st0, "kvcT", [64, 2, 2, 256], BF16)
        S.memset(kvcT, 0.0)
        ctab = alloc(st0, "ctab", [64, 2, 256], F32)
        Gs = alloc(st0, "Gs", [128, 8], F32)
        shf = alloc(st0, "shf", [128, 8], F32)
        gGb = alloc(st0, "gGb", [128, D], F32)
        b1 = alloc(st0, "b1", [128, 2, 2], F32)

        with ExitStack() as stA:
            win = alloc(stA, "win", [128, 8, PW], BF16)
            for k in range(8):
                deferred.append(lambda k=k: S.dma("pool", win[:, k, :], win_d[k * 128:(k + 1) * 128, :]))
            w1 = alloc(stA, "w1", [128, 2, 16, 256], BF16)
            w2 = alloc(stA, "w2", [128, 2, 2, 64], BF16)
            wkc = alloc(stA, "wkc", [128, 8, 4, 128], BF16)

            with ExitStack() as stP:
                wadaf = [alloc(stP, "wadaf%d" % i, [128, 3 * D], F32) for i in range(2)]
                def rot_tables_multi(posf, n, jobs, tmps):
                    for (inv_col, phase, outv), (u, ki, kf) in zip(jobs, tmps):
                        if phase == 0.0:
                            S.ts(u[:, 0:n], posf, inv_col, None, ALU.mult)
                        else:
                            S.ts(u[:, 0:n], posf, inv_col, phase, ALU.mult, ALU.add)
                    for (inv_col, phase, outv), (u, ki, kf) in zip(jobs, tmps):
                        S.copy(ki[:, 0:n], u[:, 0:n])
                    for (inv_col, phase, outv), (u, ki, kf) in zip(jobs, tmps):
                        S.copy(kf[:, 0:n], ki[:, 0:n])
                    for (inv_col, phase, outv), (u, ki, kf) in zip(jobs, tmps):
                        S.tt(u[:, 0:n], u[:, 0:n], kf[:, 0:n], ALU.subtract)
                    for (inv_col, phase, outv), (u, ki, kf) in zip(jobs, tmps):
                        S.act(outv, u[:, 0:n], AF.Sin, scale=2 * PI)

                posi_all = alloc(stP, "posi_all", [128, S_LEN], I32)
                S.dma("sp", posi_all, V(pos_d.ap[0:1, :].partition_broadcast(128), pos_d.bufs))
                for fn in deferred:
                    fn()
                S.dma("pool", w1[:, 0], w1k_d.re("(j p) h -> p j h", p=128))
                S.dma("pool", w1[:, 1], w1v_d.re("(j p) h -> p j h", p=128))
                S.dma("pool", w2[:, 0], w2k_d.re("(c p) d -> p c d", p=128))
                S.dma("pool", w2[:, 1], w2v_d.re("(c p) d -> p c d", p=128))
                posi = [alloc(stP, "posi%d" % i, [128, 512], I32) for i in range(2)]
                posf = [alloc(stP, "posf%d" % i, [128, 512], F32) for i in range(2)]
                tmps = [(alloc(stP, "tu%d" % i, [128, 512], F32), alloc(stP, "tki%d" % i, [128, 512], I32),
                         alloc(stP, "tkf%d" % i, [128, 512], F32)) for i in range(4)]
                tout2 = [[alloc(stP, "tout%d_%d" % (i, j), [128, 512], F32) for i in range(4)] for j in range(2)]
                for ch in range(NS):
                    tout = tout2[ch % 2]
                    sl = slice(ch * 512, (ch + 1) * 512)
                    pi_, pf_ = posi[ch % 2], posf[ch % 2]
                    S.copy(pf_, posi_all[:, sl])
                    jobs = [(K["inv2"][:, 0:1], 0.25, tout[0]), (K["inv2"][:, 0:1], 0.0, tout[1]),
                            (K["inv2"][:, 1:2], 0.25, tout[2]), (K["inv2"][:, 1:2], 0.0, tout[3])]
                    rot_tables_multi(pf_, 512, jobs, tmps)
                    for i in range(4):
                        S.dma("act", tabs_d[i][:, sl], tout[i], sembuf=tout[i].bufs[0])
                S.memset(posi[0][0:64, 0:256], 0)
                S.dma("sp", posi[0][0:64, 0:255],
                      V(pos_d.ap[0:1, 31:4096:16].partition_broadcast(64), pos_d.bufs),
                      allow_slow_non_contiguous=True)
                S.copy(posf[0][0:64, 0:256], posi[0][0:64, 0:256])
                jobs = [(K["inv2"][0:64, 1:2], 0.25, ctab[:, 0, :]), (K["inv2"][0:64, 1:2], 0.0, ctab[:, 1, :])]
                rot_tables_multi(posf[0][0:64, 0:256], 256, jobs,
                                 [tuple(x[0:64] for x in tmps[0]), tuple(x[0:64] for x in tmps[1])])
                cs_ = alloc(stP, "cs", [128, 8], F32)
                S.dma("sp", cs_, c_d.re("(k p) -> p k", p=128), allow_slow_non_contiguous=True)
                csb = alloc(stP, "csb", [128, 8], F32)
                S.act(csb, cs_, AF.Silu)
                badaT = alloc(stP, "badaT", [128, 24], F32)
                S.dma("sp", badaT, bada_d.re("(k p) -> p k", p=128), allow_slow_non_contiguous=True)
                gpp = alloc(stP, "gpp", [128, 2, 8], F32)
                S.dma("sp", gpp[:, 0], gpre_d.re("(k p) -> p k", p=128), allow_slow_non_contiguous=True)
                S.dma("sp", gpp[:, 1], gpost_d.re("(k p) -> p k", p=128), allow_slow_non_contiguous=True)
                S.memset(ps[0][:, 0:24], 0.0)
                for k in range(8):
                    wf = wadaf[k % 2]
                    S.dma("sp", wf, wada_d[k * 128:(k + 1) * 128, :])
                    for jc in range(24):
                        S.mm(ps[0][:, jc:jc + 1], wf[:, jc * 128:(jc + 1) * 128], csb[:, k:k + 1],
                             start=False, stop=(k == 7), skip_group_check=True)
                mod = alloc(stP, "mod", [128, 24], F32)
                S.tt(mod, ps[0][:, 0:24], badaT, ALU.add)
                S.copy(shf, mod[:, 0:8])
                S.stt(Gs, mod[:, 8:16], 1.0, gpp[:, 0], ALU.add, ALU.mult)
                gG = alloc(stP, "gG", [128, 8], F32)
                S.tt(gG, mod[:, 16:24], gpp[:, 1], ALU.mult)
                dg = alloc(stP, "dg", [128, 128], F32)
                for k in range(8):
                    S.ts(dg, K["ident_f"], gG[:, k:k + 1], None, ALU.mult)
                    S.mm(ps[1 + k // 4][:, (k % 4) * 128:(k % 4 + 1) * 128], K["ones_f"], dg)
                S.copy(gGb[:, 0:512], ps[1])
                S.copy(gGb[:, 512:1024], ps[2])
                pef = alloc(stP, "pef", [128, 2, 16], F32)
                S.dma("sp", pef[:, 0], pek_d.re("(j p) -> p j", p=128), allow_slow_non_contiguous=True)
                S.dma("sp", pef[:, 1], pev_d.re("(j p) -> p j", p=128), allow_slow_non_contiguous=True)
                peb = alloc(stP, "peb", [128, 2, 16], BF16)
                S.copy(peb, pef)
                for kv in range(2):
                    for hc in range(2):
                        for j in range(16):
                            S.mm(ps[3][:, kv * 2 + hc:kv * 2 + hc + 1], w1[:, kv, j, hc * 128:(hc + 1) * 128],
                                 peb[:, kv, j:j + 1], start=(j == 0), stop=(j == 15))
                S.copy(b1.re("p a b -> p (a b)"), ps[3][:, 0:4])

            for i4 in range(4):
                c0 = 2560 + 64 * i4
                S.copy(wkc[:, :, i4, 0:64], win[:, :, c0:c0 + 64], eng="pool")
                S.copy(wkc[:, :, i4, 64:128], win[:, :, c0:c0 + 64], eng="pool")
            S.barrier()

            with ExitStack() as stW:
                xb_ = [alloc(stW, "xt%d" % i, [128, D], F32) for i in range(2)]
                junk = alloc(stW, "junk", [128, D], BF16)
                ssq = alloc(stW, "ssq", [128, 1], F32)
                rstd = alloc(stW, "rstd", [128, 1], F32)
                xn = [alloc(stW, "xn", [128, D], BF16)] * 2
                hTs = [alloc(stW, "hT%d" % i, [128, 8, 512], BF16) for i in range(2)]
                tab = alloc(stW, "tab", [128, 4, 512], F32)
                rqs = [alloc(stW, "rq%d" % i, [128, 4, 512], BF16) for i in range(2)]
                rks = [alloc(stW, "rk%d" % i, [128, 4, 512], BF16) for i in range(2)]
                xbs = [alloc(stW, "xbs%d" % i, [128, 512], BF16) for i in range(2)]
                t1s = [alloc(stW, "t1", [128, 512], F32)] * 2
                t2s = [alloc(stW, "t2", [128, 512], F32)] * 2
                qn = [alloc(stW, "qn%d" % i, [128, 512], BF16) for i in range(2)]
                kst = [alloc(stW, "kst%d" % i, [64, 512], BF16) for i in range(2)]
                vrets = [alloc(stW, "vret%d" % i, [128, 4, 512], BF16) for i in range(2)]
                sgs = [alloc(stW, "sg%d" % i, [128, 4, 512], BF16) for i in range(2)]
                sng = [alloc(stW, "sng%d" % i, [128, 512], BF16) for i in range(2)]
                vst = [alloc(stW, "vst%d" % i, [128, 2, 2, 64], BF16) for i in range(2)]
                gst = [alloc(stW, "gst%d" % i, [128, 24], F32) for i in range(2)]
                KC = [alloc(stW, "KC%d" % i, [128, 2, 2, 528], BF16) for i in range(2)]
                hid = alloc(stW, "hid", [128, 2, 2, 64], BF16)
                kcx = alloc(stW, "kcx", [64, 64], BF16)
                scbs = [alloc(stW, "scb%d" % i, [128, 8, 128], BF16) for i in range(2)]
                kzs = [alloc(stW, "kz%d" % i, [128, 512], BF16) for i in range(2)]
                R32 = alloc(stW, "R32", [128, 4, 128], F32)
                Rb = alloc(stW, "Rb", [128, 4, 128], BF16)
                o1 = alloc(stW, "o1", [128, 8, 64], F32)
                o2 = alloc(stW, "o2", [128, 8, 64], F32)
                st8 = alloc(stW, "st8", [128, 4, 8], F32)
                mst = [alloc(stW, "mst%d" % i, [128, 512], BF16) for i in range(2)]
                S.memset(R32, 0.0)
                S.memset(Rb, 0.0)
                for i in range(2):
                    S.memset(KC[i], 0.0)
                print("passA sbuf_base", nc.sbuf_base, "top", nc.sbuf_top)

                rotc = [0]
                rot_pending = []

                def rotary(src_ps, npart, perm, cosv, sinv, dest, scale, then=None):
                    slot = rotc[0] % 2
                    rotc[0] += 1
                    xb = xbs[slot][0:npart]
                    S.act(xb, src_ps, AF.Copy, scale=scale)

                    def fin():
                        t1, t2 = t1s[slot], t2s[slot]
                        S.mm(ps[7][0:npart, :], perm, xb)
                        S.tt(t1[0:npart], ps[7][0:npart, :], sinv, ALU.mult)
                        S.tt(t2[0:npart], xb, cosv, ALU.mult, eng="pool")
                        S.tt(dest, t1[0:npart], t2[0:npart], ALU.add, eng="pool")
                        if then is not None:
                            then()

                    while rot_pending:
                        rot_pending.pop(0)()
                    rot_pending.append(fin)

                def rot_flush():
                    while rot_pending:
                        rot_pending.pop(0)()

                pscyc = [0]

                def next_ps():
                    i = pscyc[0] % 3
                    pscyc[0] += 1
                    return ps[i]

                def genA1(t):
                    hT = hTs[t % 2]
                    for j in range(4):
                        tt_ = 4 * t + j
                        xt = xb_[tt_ % 2]
                        S.dma("sp", xt, x_d[tt_ * 128:(tt_ + 1) * 128, :])
                        S.act(junk, xt, AF.Square, accum_out=ssq)
                        S.ts(rstd, ssq, 1.0 / D, 1e-6, ALU.mult, ALU.add)
                        S.act(rstd, rstd, AF.Sqrt)
                        S.recip(rstd, rstd)
                        xnb = xn[tt_ % 2]
                        S.act(xnb, xt, AF.Copy, scale=rstd)
                        yield
                        pb = ps[7].cast(BF16)
                        for k in range(8):
                            S.tr(pb[:, k * 128:(k + 1) * 128], xnb[:, k * 128:(k + 1) * 128], K["ident_b"])
                        for k in range(8):
                            S.act(hT[:, k, j * 128:(j + 1) * 128], pb[:, k * 128:(k + 1) * 128], AF.Identity,
                                  scale=Gs[:, k:k + 1], bias=shf[:, k:k + 1])
                        yield

                def genA2(t):
                    T0 = 512 * t
                    hT, rq, rk, vret, sg = hTs[t % 2], rqs[t % 2], rks[t % 2], vrets[t % 2], sgs[t % 2]
                    S.dma("sp", tab, tabs_d.re("a p n -> p a n")[:, :, T0:T0 + 512])

                    def proj_fm(wsel, M):
                        p_ = next_ps()
                        for k in range(8):
                            S.mm(p_[0:M, :], wsel(k), hT[:, k, :], start=(k == 0), stop=(k == 7))
                        return p_

                    for p in range(4):
                        pq = proj_fm(lambda k, p=p: win[:, k, 128 * p:128 * (p + 1)], 128)
                        rotary(pq, 128, K["permR"], tab[:, 0, :], tab[:, 1, :], rq[:, p, :], 0.125)
                        yield
                        pk = proj_fm(lambda k, p=p: win[:, k, 512 + 128 * p:512 + 128 * (p + 1)], 128)
                        rotary(pk, 128, K["permR"], tab[:, 0, :], tab[:, 1, :], rk[:, p, :], 1.0)
                        yield
                    for p in range(4):
                        pq = proj_fm(lambda k, p=p: win[:, k, 2048 + 128 * p:2048 + 128 * (p + 1)], 128)
                        qb = qn[p % 2]

                        def st_q(p=p, qb=qb):
                            S.dma("sp", qT_d[2 * p:2 * p + 2].re("h d n -> (h d) n")[:, T0:T0 + 512], qb,
                                  sembuf=qb.bufs[0])
                        rotary(pq, 128, K["permN"], tab[:, 2, :], tab[:, 3, :], qb, 0.125, then=st_q)
                        yield
                    for i4, (c0, dst) in enumerate(((2816, ks_d), (2880, ks_d), (3072, kw_d), (3136, kw_d))):
                        g = i4 % 2
                        pk = proj_fm(lambda k, c0=c0: win[:, k, c0:c0 + 64], 64)
                        kb = kst[i4 % 2]

                        def st_k(dst=dst, g=g, kb=kb):
                            S.dma("sp", dst[:, g, T0:T0 + 512], kb, sembuf=kb.bufs[0])
                        rotary(pk[0:64, :], 64, K["permN"][0:64, 0:64], tab[0:64, 2, :], tab[0:64, 3, :], kb, 1.0,
                               then=st_k)
                        yield
                    KCc, KCp = KC[t % 2], KC[(t + 1) % 2]
                    for kv in range(2):
                        for g in range(2):
                            pk = proj_fm(lambda k, i4=kv * 2 + g: wkc[:, k, i4, :], 128)
                            S.copy(KCc[0:64, kv, g, 16:528], pk[0:64, :], eng="act")
                            S.copy(KCc[64:128, kv, g, 15:527], pk[64:128, :], eng="act")
                            if kv == 0 and g == 0:
                                rot_flush()
                            yield
                    if t > 0:
                        S.copy(KCc[0:64, :, :, 0:16], KCp[0:64, :, :, 512:528], eng="pool")
                        S.copy(KCc[64:128, :, :, 0:15], KCp[64:128, :, :, 512:527], eng="pool")

                    for j in range(4):
                        tt_ = 4 * t + j
                        lhs = lambda k: hT[:, k, j * 128:(j + 1) * 128]
                        for gi, (c0, n) in enumerate(((1024, 512), (1536, 512), (3352, 512), (2944, 408))):
                            p_ = next_ps()
                            for k in range(8):
                                S.mm(p_[:, 0:n], lhs(k), win[:, k, c0:c0 + n], start=(k == 0), stop=(k == 7))
                            if gi == 0:
                                S.copy(vret[:, j, :], p_, eng="act")
                            elif gi == 1:
                                S.act(sg[:, j, :], p_, AF.Silu)
                            elif gi == 2:
                                sb_ = sng[tt_ % 2]
                                S.act(sb_, p_, AF.Silu)
                                S.dma("sp", sng_d[tt_ * 128:(tt_ + 1) * 128, :], sb_, sembuf=sb_.bufs[0])
                            else:
                                vb = vst[tt_ % 2]
                                S.copy(vb[:, 0].re("p g d -> p (g d)"), p_[:, 0:128], eng="act")
                                S.copy(vb[:, 1].re("p g d -> p (g d)"), p_[:, 256:384], eng="act")
                                S.dma("sp", vs_d[tt_ * 128:(tt_ + 1) * 128], vb[:, 0], sembuf=vb.bufs[0])
                                S.dma("sp", vw_d[tt_ * 128:(tt_ + 1) * 128], vb[:, 1], sembuf=vb.bufs[0])
                                gb = gst[tt_ % 2]
                                S.act(gb, p_[:, 384:408], AF.Sigmoid)
                                S.dma("sp", gates_d[tt_ * 128:(tt_ + 1) * 128, :], gb, sembuf=gb.bufs[0])
                            yield

                    for kv in range(2):
                        for hc in range(2):
                            p_ = next_ps()
                            po = p_[:, 0:64].re("p (g r) -> p g r", g=2)
                            for j in range(16):
                                S.mm(po, w1[:, kv, j, hc * 128:(hc + 1) * 128],
                                     KCc[:, kv, :, 2 * j:2 * j + 16 * 31 + 1:16], start=(j == 0), stop=(j == 15))
                            S.act(hid[:, kv, hc, :], p_[:, 0:64], AF.Silu, bias=b1[:, kv, hc:hc + 1])
                            yield
                        p_ = next_ps()
                        for hc in range(2):
                            S.mm(p_[0:64, 0:64], w2[:, kv, hc, :], hid[:, kv, hc, :], start=(hc == 0), stop=(hc == 1))
                        r0 = 1 if t == 0 else 0
                        n0 = 32 * t - 1
                        for g in range(2):
                            dst = kvcT[:, kv, g, n0 + r0:n0 + 32]
                            src = p_[0:64, g * 32 + r0:g * 32 + 32]
                            if kv == 1:
                                S.copy(dst, src)
                            else:
                                S.copy(kcx[:, g * 32:(g + 1) * 32], p_[0:64, g * 32:(g + 1) * 32], eng="act")
                        if kv == 0:
                            yield
                            t1, t2 = t1s[0], t2s[0]
                            S.mm(ps[7][0:64, 0:64], K["permN"][0:64, 0:64], kcx)
                            for g in range(2):
                                cs0 = ctab[:, 0, n0 + r0:n0 + 32]
                                sn0 = ctab[:, 1, n0 + r0:n0 + 32]
                                S.tt(t1[0:64, 0:32 - r0], ps[7][0:64, g * 32 + r0:g * 32 + 32], sn0, ALU.mult)
                                S.tt(t2[0:64, 0:32 - r0], kcx[:, g * 32 + r0:g * 32 + 32], cs0, ALU.mult)
                                S.tt(kvcT[:, 0, g, n0 + r0:n0 + 32], t1[0:64, 0:32 - r0], t2[0:64, 0:32 - r0],
                                     ALU.add)
                        yield

                def genA(t):
                    a1 = genA1(t + 1) if t + 1 < NS else None
                    for i, _ in enumerate(genA2(t)):
                        yield
                        if a1 is not None and i % 4 == 3:
                            try:
                                next(a1)
                                yield
                            except StopIteration:
                                a1 = None
                    if a1 is not None:
                        for _ in a1:
                            yield

                def genR(t):
                    hT, rq, rk, vret, sg = hTs[t % 2], rqs[t % 2], rks[t % 2], vrets[t % 2], sgs[t % 2]
                    zb = V(K["zeta"].ap.unsqueeze(2).to_broadcast([128, 8, 64]), K["zeta"].bufs)

                    def stage1(j):
                        cs = slice(j * 128, (j + 1) * 128)
                        scb, kz = scbs[j % 2], kzs[j % 2]
                        for p in range(4):
                            for hh in range(2):
                                rows = slice(hh * 64, hh * 64 + 64)
                                S.mm(ps[4 + hh][:, p * 128:(p + 1) * 128], rk[rows, p, cs], rq[rows, p, cs])
                        for hh in range(2):
                            S.tt(scb[:, hh::2, :], ps[4 + hh].re("p (h i) -> p h i", h=4),
                                 K["decayT"][:, hh::2, :], ALU.mult)
                        pb = ps[6].cast(BF16)
                        for p in range(4):
                            S.tr(pb[:, p * 128:(p + 1) * 128], rk[:, p, cs], K["ident_b"])
                        S.tt(kz.re("p (h d) -> p h d", h=8), pb[:, 0:512].re("p (h d) -> p h d", h=8), zb, ALU.mult)

                    stage1(0)
                    yield
                    for j in range(4):
                        tt_ = 4 * t + j
                        cs = slice(j * 128, (j + 1) * 128)
                        scb, kz = scbs[j % 2], kzs[j % 2]
                        pi_ = ps[3]
                        for h in range(8):
                            S.mm(pi_[:, h * 64:(h + 1) * 64], scb[:, h, :], vret[:, j, h * 64:(h + 1) * 64])
                        for p in range(4):
                            for hh in range(2):
                                rows = slice(hh * 64, hh * 64 + 64)
                                S.mm(ps[4 + hh][:, p * 64:(p + 1) * 64], rq[rows, p, cs],
                                     Rb[rows, p, hh * 64:hh * 64 + 64])
                        pkv = ps[6]
                        for p in range(4):
                            S.mm(pkv[:, p * 128:(p + 1) * 128], kz[:, p * 128:(p + 1) * 128],
                                 vret[:, j, p * 128:(p + 1) * 128])
                        for hh in range(2):
                            xib = V(K["xi"].ap[:, hh::2].unsqueeze(2).to_broadcast([128, 4, 64]), K["xi"].bufs)
                            S.tt(o1[:, hh::2, :], ps[4 + hh][:, 0:256].re("p (h d) -> p h d", h=4), xib, ALU.mult)
                        S.tt(o1, o1, pi_.re("p (h d) -> p h d", h=8), ALU.add)
                        for p in range(4):
                            S.stt(R32[:, p, :], R32[:, p, :], K["cdv"][:, p:p + 1], pkv[:, p * 128:(p + 1) * 128],
                                  ALU.mult, ALU.add)
                        S.copy(Rb, R32, eng="pool")
                        yield
                        if j + 1 < 4:
                            stage1(j + 1)
                            yield
                        S.reduce(st8[:, 0, :], o1, ALU.add)
                        S.tt(o2, o1, o1, ALU.mult, eng="pool")
                        S.reduce(st8[:, 1, :], o2, ALU.add)
                        S.ts(st8[:, 0, :], st8[:, 0, :], 1.0 / 64, None, ALU.mult)
                        S.tt(st8[:, 2, :], st8[:, 0, :], st8[:, 0, :], ALU.mult)
                        S.stt(st8[:, 1, :], st8[:, 1, :], 1.0 / 64, st8[:, 2, :], ALU.mult, ALU.subtract)
                        S.ts(st8[:, 1, :], st8[:, 1, :], 1e-5, None, ALU.add)
                        yield
                        S.act(st8[:, 1, :], st8[:, 1, :], AF.Sqrt)
                        yield
                        S.recip(st8[:, 3, :], st8[:, 1, :])
                        yield
                        mb = V(st8.ap[:, 0, :].unsqueeze(2).to_broadcast([128, 8, 64]), st8.bufs)
                        rb_ = V(st8.ap[:, 3, :].unsqueeze(2).to_broadcast([128, 8, 64]), st8.bufs)
                        S.tt(o2, o1, mb, ALU.subtract)
                        S.tt(o2, o2, rb_, ALU.mult)
                        mo = mst[tt_ % 2]
                        S.tt(mo, o2.re("p h d -> p (h d)"), sg[:, j, :], ALU.mult)
                        S.dma("sp", mret_d[tt_ * 128:(tt_ + 1) * 128, :], mo, sembuf=mo.bufs[0])
                        yield

                def interleave(ga, gr, ratio=2):
                    a_done = ga is None
                    r_done = gr is None
                    while not (a_done and r_done):
                        if not r_done:
                            try:
                                next(gr)
                            except StopIteration:
                                r_done = True
                        for _ in range(ratio):
                            if not a_done:
                                try:
                                    next(ga)
                                except StopIteration:
                                    a_done = True

                for _ in genA1(0):
                    pass
                interleave(genA(0), None)
                for t in range(NS):
                    interleave(genA(t + 1) if t + 1 < NS else None, genR(t))
                hT = hTs[(NS - 1) % 2]

                if "passA" in debug:
                    S.barrier()
                    d_hT = dbg_out("hT", [128, 8, 512], BF16)
                    S.dma("sp", d_hT, hT, is_output=True)
                    d_kvc = dbg_out("kvcT", [64, 2, 2, 256], BF16)
                    S.dma("sp", d_kvc, kvcT, is_output=True)
                    for nm, src in (("qT", qT_d), ("sng", sng_d), ("gates", gates_d), ("mret", mret_d), ("ks", ks_d),
                                    ("kw", kw_d), ("vs", vs_d), ("vw", vw_d), ("tabs", tabs_d)):
                        shp = list(src.ap.shape)
                        dd = dbg_out(nm, shp, src.ap.dtype)
                        S.dma("sp", dd, src, is_output=True)
                    dG = dbg_out("gGb", [128, D])
                    S.dma("sp", dG, gGb, is_output=True)
            S.barrier()

        if "passA" in debug:
            with nc.Block() as block:
                S.emit(block)
            return nc, consts

        PASSB(nc, S, st0, alloc, ps, K, cd_, consts, kvcT, gGb, x_d, wout_d, out_d, qT_d, sng_d, gates_d, mret_d,
              ks_d, kw_d, vs_d, vw_d, debug, dbg_out)
        with nc.Block() as block:
            S.emit(block)
    return nc, consts


def PASSB(nc, S, st0, alloc, ps, K, cd_, consts, kvcT, gGb, x_d, wout_d, out_d, qT_d, sng_d, gates_d, mret_d,
          ks_d, kw_d, vs_d, vw_d, debug, dbg_out):
    with ExitStack() as stB:
        wout = alloc(stB, "wout", [128, 8, D], BF16)
        for k in range(8):
            S.dma("pool", wout[:, k, :], wout_d[k * 128:(k + 1) * 128, :])
        late_dmas = []
        ksA = alloc(stB, "ksA", [128, 2, S_LEN], BF16)
        kwT = alloc(stB, "kwT", [128, 2, S_LEN], BF16)
        S.memset(kwT[64:128], 0.0)
        kcA = alloc(stB, "kcA", [128, 2, 256], BF16)
        S.memset(kcA[64:128], 0.0)
        S.copy(kcA[0:64], kvcT[:, 0])
        vsA = alloc(stB, "vsA", [128, NT, 2, 65], BF16)
        vwA = alloc(stB, "vwA", [128, NT, 2, 65], BF16)
        S.memset(vsA[:, :, :, 64:65], 1.0)
        S.memset(vwA[:, :, :, 64:65], 1.0)
        late_dmas.append(lambda: S.dma("sp", kwT[0:64], kw_d))
        for g in range(2):
            late_dmas.append(lambda g=g: S.dma("sp", vwA[:, :, g, 0:64],
                                               vw_d.re("(t p) g d -> p t g d", p=128)[:, :, g, :]))
        late_dmas.append(lambda: S.dma("sp", ksA[0:64], ks_d))
        for g in range(2):
            late_dmas.append(lambda g=g: S.dma("sp", ksA[64:128, g, :], cd_["onehot"]))
        for g in range(2):
            late_dmas.append(lambda g=g: S.dma("sp", vsA[:, :, g, 0:64],
                                               vs_d.re("(t p) g d -> p t g d", p=128)[:, :, g, :]))
        vcA = alloc(stB, "vcA", [128, 2, 2, 65], BF16)
        S.memset(vcA, 1.0)
        for g in range(2):
            for nt in range(2):
                pb = ps[7].cast(BF16)
                S.tr(pb[:, 0:64], kvcT[:, 1, g, nt * 128:(nt + 1) * 128], K["ident_b"][0:64, 0:64])
                S.copy(vcA[:, nt, g, 0:64], pb[:, 0:64])
        Qs = [alloc(stB, "Qa%d" % i, [128, 8, 512], BF16) for i in range(2)]
        Qlo = [Buf("Qlo%d" % i) for i in range(2)]
        Qhi = [[Buf("Qhi%d_%d" % (i, g)) for g in range(2)] for i in range(2)]
        for i in range(2):
            S.memset(V(Qs[i].ap[64:128], Qhi[i]), 0.0)
        cmk = [alloc(stB, "cmk%d" % i, [128, 2, 512], BF16) for i in range(2)]
        fbt = [alloc(stB, "fbt%d" % i, [128, 4, 64], F32) for i in range(2)]
        gts = [alloc(stB, "gts%d" % i, [128, 4, 24], F32) for i in range(2)]
        sngb = [alloc(stB, "sngb%d" % i, [128, 4, 512], BF16) for i in range(2)]
        NPT = 8
        PT = [alloc(stB, "PT%d" % i, [128, 512], BF16) for i in range(NPT)]
        accs = [alloc(stB, "acc%d" % i, [128, 4, 512], F32) for i in range(2)]
        impacc = alloc(stB, "impacc", [128, 4, 64], F32)
        rls = [alloc(stB, "rl%d" % i, [128, 4], F32) for i in range(2)]
        sc4s = [alloc(stB, "sc4%d" % i, [128, 4], F32) for i in range(2)]
        scr = alloc(stB, "scr", [128, 64], F32)
        wk64 = alloc(stB, "wk64", [128, 64], F32)
        m8a = alloc(stB, "m8a", [128, 8], F32)
        m8b = alloc(stB, "m8b", [128, 8], F32)
        thr = alloc(stB, "thr", [128, 1], F32)
        selbs = [alloc(stB, "selb%d" % i, [128, 128], BF16) for i in range(4)]
        for i in range(4):
            S.memset(selbs[i], 0.0)
        scrs = [alloc(stB, "scr%d" % i, [128, 64], F32) for i in range(4)]
        wk64s = [alloc(stB, "wk64%d" % i, [128, 64], F32) for i in range(4)]
        m8as = [alloc(stB, "m8a%d" % i, [128, 8], F32) for i in range(4)]
        m8bs = [alloc(stB, "m8b%d" % i, [128, 8], F32) for i in range(4)]
        thrs = [alloc(stB, "thr%d" % i, [128, 1], F32) for i in range(4)]
        mix = [alloc(stB, "mix%d" % i, [128, D], BF16) for i in range(4)]
        mixTs = [alloc(stB, "mixT%d" % i, [128, 8, 128], BF16) for i in range(4)]
        xres = [alloc(stB, "xres%d" % i, [128, D], F32) for i in range(4)]
        zts = [alloc(stB, "zt%d" % i, [128, D], F32) for i in range(2)]
        junk2 = alloc(stB, "junk2", [128, D], BF16)
        ss2s = [alloc(stB, "ss2%d" % i, [128, 4], F32) for i in range(2)]
        ot = [alloc(stB, "ot%d" % i, [128, D], F32) for i in range(2)]
        cyc = {"pt": 0, "sc": 0, "o": 0, "imp": 0, "ev": 0}
        LOOK = 4
        pend = []

        fins = []

        def run_fins(limit_hseq=None, tick=False):
            keep = []
            ready = []
            for ent in fins:
                if tick:
                    ent[0] -= 1
                if ent[0] <= 0 or (limit_hseq is not None and ent[1] <= limit_hseq):
                    ready.append(ent)
                else:
                    keep.append(ent)
            if ready:
                last = max(fins.index(e) for e in ready)
                ready = fins[:last + 1]
                keep = fins[last + 1:]
            fins[:] = keep
            for ent in ready:
                ent[2]()

        def push(s1, s2, hseq, first_of_head):
            if first_of_head:
                while pend and pend[0][0] <= hseq - 2:
                    pend.pop(0)[1]()
                run_fins(limit_hseq=hseq - 2)
            s1()
            pend.append((hseq, s2))
            while len(pend) > LOOK:
                pend.pop(0)[1]()
            run_fins(tick=True)

        def flush():
            while pend or fins:
                while pend:
                    pend.pop(0)[1]()
                run_fins(limit_hseq=1 << 60)

        def load_inputs(t):
            T0 = 512 * t
            sl = t % 2
            S.dma("sp", V(Qs[sl].ap[0:64], [Qlo[sl]]), qT_d.re("h d n -> d h n")[:, :, T0:T0 + 512], sembuf=Qlo[sl])
            S.dma("sp", cmk[sl], cd_["cmpmask"][t].re("a p n -> p a n"))
            S.dma("sp", fbt[sl], cd_["fb"][4 * t:4 * t + 4].re("a p n -> p a n"))
            S.dma("sp", gts[sl], gates_d[T0:T0 + 512].re("(a p) n -> p a n", p=128))
            S.dma("sp", sngb[sl], sng_d[T0:T0 + 512].re("(a p) n -> p a n", p=128))

        oTs = [alloc(stB, "oT%d" % i, [65, 512], F32) for i in range(2)]
        zeros_f = alloc(stB, "zeros_f", [128, 272], F32)
        S.memset(zeros_f, 0.0)

        def attend(t, h, br, first, ktl, with_imp=False, after=None, otrans=False, act_off=False):
            sl = t % 2
            Q = Qs[sl]
            ob = ps[3 + cyc["o"] % 2]
            cyc["o"] += 1
            impb = None
            tb = None
            if with_imp or otrans:
                impb = ps[5 + cyc["imp"] % 2]
                cyc["imp"] += 1
            n = len(ktl)
            cyc["hseq"] = cyc.get("hseq", 0) + 1
            hseq = cyc["hseq"]

            def evac(src):
                rl = rls[cyc["ev"] % 2]
                sc4 = sc4s[cyc["ev"] % 2]
                cyc["ev"] += 1
                o3 = src[:, 0:272].re("p (q c) -> p q c", q=4)
                S.ts(rl, o3[:, :, 64], 1.0e-30, None, ALU.max)
                S.recip(rl, rl)
                S.tt(sc4, rl, gts[sl][:, :, 3 * h + br], ALU.mult)
                for qs in range(4):
                    dst = accs[t % 2][:, qs, h * 64:(h + 1) * 64]
                    if first and act_off:
                        S.act(dst, o3[:, qs, 0:64], AF.Copy, scale=sc4[:, qs:qs + 1])
                    elif first:
                        S.ts(dst, o3[:, qs, 0:64], sc4[:, qs:qs + 1], None, ALU.mult)
                    else:
                        S.stt(dst, o3[:, qs, 0:64], sc4[:, qs:qs + 1], dst, ALU.mult, ALU.add)
                return rl

            for idx, (kT, va, Krows, M, c0, c1, mask, ov) in enumerate(ktl):
                sp_ = ps[cyc["sc"] % 3]
                cyc["sc"] += 1
                pt = PT[cyc["pt"] % NPT]
                cyc["pt"] += 1

                def s1(idx=idx, kT=kT, Krows=Krows, M=M, c0=c0, c1=c1, mask=mask, sp_=sp_, pt=pt):
                    if idx == 0 and not otrans:
                        if act_off:
                            S.act(ob[:, 0:272], zeros_f[:, 0:272], AF.Copy)
                            S.act(impb[:, 0:272], zeros_f[:, 0:272], AF.Copy)
                        else:
                            S.memset(ob[:, 0:272], 0.0)
                            if impb is not None:
                                S.memset(impb[:, 0:272], 0.0)
                    qv = V(Q.ap[0:Krows, h, c0:c1], [Qlo[sl]] + ([Qhi[sl][h // 4]] if Krows == 128 else []))
                    S.mm(sp_[0:M, c0:c1], kT, qv, start=True, stop=(mask is None))
                    if mask is not None:
                        mv, m0, m1 = mask
                        S.mm(sp_[0:M, m0:m1], K["ident_b"][0:M, 0:M], mv, start=False, stop=True)
                    S.act(pt[0:M, c0:c1], sp_[0:M, c0:c1], AF.Exp)

                def s2(idx=idx, va=va, M=M, c0=c0, c1=c1, ov=ov, pt=pt):
                    if otrans:
                        assert idx > 0 or (c0 == 0 and c1 == 512)
                        S.mm(ob[0:65, c0:c1], va, pt[0:M, c0:c1], start=(idx == 0), stop=(idx == n - 1))
                        if idx == n - 1:
                            oT = oTs[cyc.get("ot", 0) % 2]
                            cyc["ot"] = cyc.get("ot", 0) + 1
                            S.copy(oT, ob[0:65, :])

                            def fin(oT=oT):
                                for qs in range(4):
                                    S.tr(impb[:, qs * 68:qs * 68 + 65], oT[0:65, qs * 128:(qs + 1) * 128],
                                         K["ident_f"][0:65, 0:65])
                                evac(impb)
                            fins.append([2, hseq, fin])
                        return
                    for qs in range(c0 // 128, c1 // 128):
                        S.mm(ob[:, qs * 68:qs * 68 + 65], pt[0:M, qs * 128:(qs + 1) * 128], va,
                             start=False, stop=False, skip_group_check=True)
                        if ov is not None:
                            S.mm(impb[:, qs * 68:qs * 68 + 65], pt[0:M, qs * 128:(qs + 1) * 128], ov,
                                 start=False, stop=False, skip_group_check=True)
                    if idx == n - 1:
                        rl = evac(ob)
                        if after is not None:
                            after(impb, rl)

                push(s1, s2, hseq, idx == 0)

        def b1_head(t, h, act_off):
            T0 = 512 * t
            sl = t % 2
            Q = Qs[sl]
            g, h4 = h // 4, h % 4
            nts = [nt for nt in range(2) if 16 * 128 * nt + 31 <= T0 + 511]
            ktl = []
            for nt in nts:
                M = 128 if nt == 0 else 127
                ktl.append((kcA[:, g, nt * 128:nt * 128 + M], vcA[0:M, nt, g, :], 128, M, 0, 512,
                            (cmk[sl][0:M, nt, :], 0, 512), K["ovl"][0:M, nt, :]))

            def after(impb, rl):
                i3 = impb[:, 0:272].re("p (q c) -> p q c", q=4)
                for qs in range(4):
                    if h4 == 0:
                        S.ts(impacc[:, qs, :], i3[:, qs, 0:64], rl[:, qs:qs + 1], None, ALU.mult)
                    else:
                        S.stt(impacc[:, qs, :], i3[:, qs, 0:64], rl[:, qs:qs + 1], impacc[:, qs, :],
                              ALU.mult, ALU.add)
                if h4 < 3:
                    return
                for qs in range(4):
                    S.tt(scrs[qs], impacc[:, qs, :], fbt[sl][:, qs, :], ALU.add)
                for qs in range(4):
                    S.max8(m8as[qs], scrs[qs])
                for qs in range(4):
                    S.match_replace(wk64s[qs], m8as[qs], scrs[qs], -3.0e38)
                for qs in range(4):
                    S.max8(m8bs[qs], wk64s[qs])
                for qs in range(4):
                    S.reduce(thrs[qs], m8bs[qs], ALU.min)
                for qs in range(4):
                    S.ts(thrs[qs], thrs[qs], -1.0e29, None, ALU.max)
                for qs in range(4):
                    S.ts(selbs[qs][:, 64:128], scrs[qs], thrs[qs], None, ALU.is_ge)
                for qs in range(4):
                    S.ts(selbs[qs][:, 64:128], selbs[qs][:, 64:128], 1.0, 30000.0, ALU.subtract, ALU.mult)
                pb = ps[7].cast(BF16)
                for qs in range(4):
                    S.tr(pb[:, qs * 128:(qs + 1) * 128], selbs[qs], K["ident_b"])
                for qs in range(4):
                    for hh in range(4):
                        dst = V(Q.ap[64:128, 4 * g + hh, qs * 128:(qs + 1) * 128], [Qhi[sl][g]])
                        S.copy(dst, pb[64:128, qs * 128:(qs + 1) * 128], eng=("act" if act_off else "dve"))

            attend(t, h, 0, True, ktl, with_imp=True, after=after, act_off=act_off)

        def b2_head(t, h):
            g = h // 4
            ktl = []
            for m in (4, 0, 1, 2, 3, 5, 6, 7):
                kti = 4 * (t - 1) + m
                if kti < 0:
                    continue
                if m <= 3:
                    c0, c1 = 0, 128 * (m + 1)
                    mask = (K["tri"][:, 1, :], 128 * m, 128 * m + 128)
                else:
                    c0, c1 = 128 * (m - 4), 512
                    mask = (K["tri"][:, 0, :], c0, c0 + 128)
                ktl.append((kwT[:, g, kti * 128:(kti + 1) * 128], vwA[:, kti, g, :], 128, 128, c0, c1, mask, None))
            attend(t, h, 2, False, ktl)

        def b3_head(t, h):
            g = h // 4
            ktl = []
            for kt in range(4 * t + 4):
                if kt < 4 * t:
                    c0, c1, mask = 0, 512, None
                else:
                    c0, c1 = 128 * (kt - 4 * t), 512
                    mask = (K["tri"][:, 0, :], c0, c0 + 128)
                ktl.append((ksA[:, g, kt * 128:(kt + 1) * 128], vsA[:, kt, g, :], 128, 128, c0, c1, mask, None))
            attend(t, h, 1, False, ktl)

        def make_b4(t):
            sl = t % 2

            def b4mix():
                for qs in range(4):
                    S.tt(mix[qs][:, 512:1024], accs[t % 2][:, qs, :], sngb[sl][:, qs, :], ALU.mult)

            def b4a(qs):
                mx = mix[qs]
                pb = ps[7].cast(BF16)
                for k in range(8):
                    S.tr(pb[:, k * 128:(k + 1) * 128], mx[:, k * 128:(k + 1) * 128], K["ident_b"])
                S.copy(mixTs[qs].re("p k n -> p (k n)"), pb, eng="act")

            def b4b(qs):
                tt_ = 4 * t + qs
                xr, mT = xres[qs], mixTs[qs]
                zt_, ssb = zts[tt_ % 2], ss2s[tt_ % 2]
                for half in range(2):
                    zp = ps[5 + half]
                    for k in range(8):
                        S.mm(zp, mT[:, k, :], wout[:, k, half * 512:(half + 1) * 512], start=(k == 0), stop=(k == 7))
                    S.act(junk2[:, half * 512:(half + 1) * 512], zp, AF.Square, accum_out=ssb[:, half:half + 1])
                    S.tt(zt_[:, half * 512:(half + 1) * 512], zp, gGb[:, half * 512:(half + 1) * 512], ALU.mult)
                S.tt(ssb[:, 2:3], ssb[:, 0:1], ssb[:, 1:2], ALU.add)
                S.ts(ssb[:, 2:3], ssb[:, 2:3], 1.0 / D, 1e-6, ALU.mult, ALU.add)
                S.act(ssb[:, 2:3], ssb[:, 2:3], AF.Ln)
                S.act(ssb[:, 3:4], ssb[:, 2:3], AF.Exp, scale=-0.5)
                o_ = ot[tt_ % 2]
                S.stt(o_, zt_, ssb[:, 3:4], xr, ALU.mult, ALU.add)
                S.dma("sp", out_d[tt_ * 128:(tt_ + 1) * 128, :], o_, sembuf=o_.bufs[0], is_output=True)

            return b4mix, [lambda: b4a(0), lambda: b4a(1), lambda: b4a(2), lambda: b4a(3),
                           lambda: b4b(0), lambda: b4b(1), lambda: b4b(2), lambda: b4b(3)]

        load_inputs(0)
        for fn in late_dmas:
            fn()
        for h in range(8):
            b1_head(0, h, True)
        b4_prev = None
        for t in range(NS):
            for h in range(8):
                b2_head(t, h)
                if b4_prev is not None:
                    b4_prev[h]()
            if t + 1 < NS:
                load_inputs(t + 1)
            for qs in range(4):
                tt_ = 4 * t + qs
                S.dma("sp", mix[qs][:, 0:512], mret_d[tt_ * 128:(tt_ + 1) * 128, :])
                S.dma("sp", xres[qs], x_d[tt_ * 128:(tt_ + 1) * 128, :])
            for h in range(8):
                b3_head(t, h)
                if t + 1 < NS:
                    b1_head(t + 1, h, False)
            flush()
            b4mix, b4_prev = make_b4(t)
            b4mix()
        for st_ in b4_prev:
            st_()


def core_inputs(inp, consts, b):
    m = {
        "x": np.ascontiguousarray(inp["x"][b]),
        "c": np.ascontiguousarray(inp["c"][b]),
        "positions": np.ascontiguousarray(inp["positions"][b:b + 1]).astype(np.int32),
        "w_ada": np.ascontiguousarray(inp["w_ada"][0]),
        "b_ada": np.ascontiguousarray(inp["b_ada"][0]),
        "g_pre": np.ascontiguousarray(inp["g_pre"][0]),
        "g_post": np.ascontiguousarray(inp["g_post"][0]),
        "w_in": np.ascontiguousarray(inp["w_in"][0]),
        "w_out": np.ascontiguousarray(inp["w_out"][0]),
        "cmp_pe_k": np.ascontiguousarray(inp["cmp_pe_k"][0]).reshape(-1),
        "cmp_w1_k": np.ascontiguousarray(inp["cmp_w1_k"][0]),
        "cmp_w2_k": np.ascontiguousarray(inp["cmp_w2_k"][0]),
        "cmp_pe_v": np.ascontiguousarray(inp["cmp_pe_v"][0]).reshape(-1),
        "cmp_w1_v": np.ascontiguousarray(inp["cmp_w1_v"][0]),
        "cmp_w2_v": np.ascontiguousarray(inp["cmp_w2_v"][0]),
    }
    for k, v in consts.items():
        m["k_" + k] = v
    return m


_CACHE = {}


def kernel(**inputs):
    if "nc" not in _CACHE:
        _CACHE["nc"] = build()
    nc, consts = _CACHE["nc"]
    inp = {k: np.asarray(v) for k, v in inputs.items()}
    in_maps = [core_inputs(inp, consts, b) for b in range(8)]
    res = run_bass_kernel_spmd(nc, in_maps, core_ids=list(range(8)))
    out = np.stack([np.asarray(r["out"]) for r in res.results], axis=0)
    return out.astype(np.float32)
```

```python
import numpy as np
from contextlib import ExitStack
import ml_dtypes
import concourse.bass as bass
import concourse.mybir as mybir
from concourse.bass_utils import run_bass_kernel_spmd

F32 = mybir.dt.float32
BF16 = mybir.dt.bfloat16
I32 = mybir.dt.int32
AF = mybir.ActivationFunctionType
ALU = mybir.AluOpType
AX = mybir.AxisListType

S_LEN = 4096
D = 1024
NT = 32
NS = 8
PW = 3864
PI = float(np.pi)
DBG_T = 0


class Buf:
    __slots__ = ("w", "r", "sem", "semcnt", "name", "uid", "excl", "dram")
    _n = [0]

    def __init__(self, name=""):
        Buf._n[0] += 1
        self.uid = Buf._n[0]
        self.w = None
        self.r = {}
        self.sem = None
        self.semcnt = 0
        self.name = name
        self.excl = False
        self.dram = False


class V:
    __slots__ = ("ap", "bufs")

    def __init__(self, ap, bufs):
        self.ap = ap
        self.bufs = bufs if isinstance(bufs, (list, tuple)) else [bufs]

    def __getitem__(self, k):
        return V(self.ap[k], self.bufs)

    def re(self, s, **kw):
        return V(self.ap.rearrange(s, **kw), self.bufs)

    def bc(self, shape):
        return V(self.ap.to_broadcast(list(shape)), self.bufs)

    def cast(self, dt):
        return V(self.ap.bitcast(dt), self.bufs)

    def sub(self, bufs):
        return V(self.ap, bufs)


COMPUTE = ("pe", "act", "dve", "pool")


class Sched:
    def __init__(self, nc, stack):
        self.nc = nc
        self.stack = stack
        self.streams = {e: [] for e in ("pe", "act", "dve", "pool", "sp")}
        self.cnt = {e: 0 for e in COMPUTE}
        self.waited = {e: {} for e in self.streams}
        self.sems = {e: stack.enter_context(nc.semaphore("s_" + e)) for e in COMPUTE}
        self.dma_sems = {}
        self.dma_bufs = []
        self.needed = {e: set() for e in COMPUTE}
        self.out_toks = []

    def _deps(self, eng, reads, writes):
        deps = {}

        def add(tok):
            k, i = tok
            if deps.get(k, -1) < i:
                deps[k] = i

        for b in reads:
            if b.excl:
                for k, i in b.r.items():
                    if k != eng:
                        add((k, i))
            if b.w is not None:
                if b.w[0] == eng and eng == "pe":
                    continue
                add(b.w)
        for b in writes:
            if b.w is not None and (b.w[0] != eng or eng != "pe"):
                add(b.w)
            for k, i in b.r.items():
                if k != eng or eng != "pe":
                    add((k, i))
        return self._emit_waits(eng, deps)

    def _emit_waits(self, eng, deps):
        waits = []
        wd = self.waited[eng]
        for k, i in deps.items():
            if wd.get(k, -1) < i:
                wd[k] = i
                waits.append((k, i))
                if k in COMPUTE:
                    self.needed[k].add(i)
        return waits

    def _mark(self, tok, reads, writes):
        k, i = tok
        for b in reads:
            if b.r.get(k, -1) < i:
                b.r[k] = i
        for b in writes:
            b.w = tok
            b.r = {}

    def op(self, eng, fn, reads, writes):
        rb = [b for v in reads if isinstance(v, V) for b in v.bufs]
        wb = [b for v in writes if isinstance(v, V) for b in v.bufs]
        waits = self._deps(eng, rb, wb)
        self.cnt[eng] += 1
        idx = self.cnt[eng]
        self.streams[eng].append(("c", waits, fn, idx))
        self._mark((eng, idx), rb, wb)

    def dma(self, q, out, in_, sembuf=None, is_output=False, **kw):
        rb = list(in_.bufs)
        wb = list(out.bufs)
        sb = sembuf or wb[0]
        if sb.sem is None:
            sb.sem = self.stack.enter_context(self.nc.semaphore("d_%d" % sb.uid))
            self.dma_sems[sb.uid] = sb.sem
            self.dma_bufs.append(sb)
        waits = self._deps(q, rb, [b for b in wb if not b.dram])
        sb.semcnt += 16
        tok = (("dma", sb.uid), sb.semcnt)
        self.streams[q].append(("d", waits, (out.ap, in_.ap, kw), sb.sem))
        self._mark(tok, rb, wb)
        if is_output:
            self.out_toks.append(tok)
        return tok

    def barrier(self):
        deps = {}
        for e in COMPUTE:
            if self.cnt[e] > 0:
                deps[e] = self.cnt[e]
        for b in self.dma_bufs:
            deps[("dma", b.uid)] = b.semcnt
        for e in self.streams:
            w = self._emit_waits(e, dict(deps))
            if w:
                self.streams[e].append(("w", w, None, None))

    def emit(self, block):
        fin = self._emit_waits("sp", {k: i for k, i in self.out_toks})
        self.streams["sp"].append(("w", fin, None, None))
        pref = {}
        for e in COMPUTE:
            m = {}
            c = 0
            for i in range(1, self.cnt[e] + 1):
                if i in self.needed[e]:
                    c += 1
                    m[i] = c
            pref[e] = m
        names = {"pe": "tensor", "act": "scalar", "dve": "vector", "pool": "gpsimd", "sp": "sync"}
        for e, st in self.streams.items():
            def body(eng, e=e, st=st):
                for kind, waits, fn, extra in st:
                    for k, i in waits:
                        if k in COMPUTE:
                            eng.wait_ge(self.sems[k], pref[k][i])
                        else:
                            eng.wait_ge(self.dma_sems[k[1]], i)
                    if kind == "c":
                        ins = fn(eng)
                        if extra in self.needed[e]:
                            ins.then_inc(self.sems[e], 1)
                    elif kind == "d":
                        oap, iap, kw = fn
                        eng.dma_start(out=oap, in_=iap, **kw).then_inc(extra, 16)
            getattr(block, names[e])(body)

    def mm(self, out, lhsT, rhs, start=True, stop=True, **kw):
        self.op("pe", lambda e: e.matmul(out.ap, lhsT.ap, rhs.ap, start=start, stop=stop, **kw),
                [lhsT, rhs], [out])

    def tr(self, out, in_, ident):
        self.op("pe", lambda e: e.transpose(out.ap, in_.ap, ident.ap), [in_, ident], [out])

    def act(self, out, in_, func, bias=None, scale=None, accum_out=None):
        kw = {}
        rd = [in_]
        if bias is not None:
            kw["bias"] = bias.ap if isinstance(bias, V) else bias
            rd.append(bias)
        if scale is not None:
            kw["scale"] = scale.ap if isinstance(scale, V) else scale
            rd.append(scale)
        wr = [out]
        if accum_out is not None:
            kw["accum_out"] = accum_out.ap
            wr.append(accum_out)
        self.op("act", lambda e: e.activation(out.ap, in_.ap, func, **kw), rd, wr)

    def tt(self, out, in0, in1, op, eng="dve"):
        self.op(eng, lambda e: e.tensor_tensor(out.ap, in0.ap, in1.ap, op), [in0, in1], [out])

    def ts(self, out, in0, s1, s2, op0, op1=None, eng="dve"):
        a1 = s1.ap if isinstance(s1, V) else s1
        a2 = s2.ap if isinstance(s2, V) else s2
        kw = {}
        if op1 is not None:
            kw["op1"] = op1
        self.op(eng, lambda e: e.tensor_scalar(out.ap, in0.ap, a1, a2, op0, **kw), [in0, s1, s2], [out])

    def stt(self, out, in0, scalar, in1, op0, op1, eng="dve"):
        a = scalar.ap if isinstance(scalar, V) else scalar
        self.op(eng, lambda e: e.scalar_tensor_tensor(out.ap, in0.ap, a, in1.ap, op0, op1),
                [in0, scalar, in1], [out])

    def copy(self, out, in_, eng="dve"):
        if eng == "act":
            self.op("act", lambda e: e.copy(out.ap, in_.ap), [in_], [out])
        else:
            self.op(eng, lambda e: e.tensor_copy(out.ap, in_.ap), [in_], [out])

    def memset(self, out, val, eng="dve"):
        self.op(eng, lambda e: e.memset(out.ap, val), [], [out])

    def reduce(self, out, in_, op, eng="dve"):
        self.op(eng, lambda e: e.tensor_reduce(out.ap, in_.ap, AX.X, op), [in_], [out])

    def recip(self, out, in_):
        self.op("dve", lambda e: e.reciprocal(out.ap, in_.ap), [in_], [out])

    def max8(self, out, in_):
        self.op("dve", lambda e: e.max(out.ap, in_.ap), [in_], [out])

    def match_replace(self, out, to_replace, values, imm):
        self.op("dve", lambda e: e.match_replace(out.ap, to_replace.ap, values.ap, imm),
                [to_replace, values], [out])


def make_consts():
    bf = ml_dtypes.bfloat16
    c = {}
    c["ident_f"] = np.eye(128, dtype=np.float32)
    c["ident_b"] = np.eye(128, dtype=np.float32).astype(bf)
    c["ones_f"] = np.ones((128, 128), np.float32)
    permR = np.zeros((128, 128), np.float32)
    permN = np.zeros((128, 128), np.float32)
    invR = np.zeros((128, 1), np.float32)
    invN = np.zeros((128, 1), np.float32)
    ir = np.power(np.float32(10000.0), -np.arange(32, dtype=np.float32) / np.float32(32))
    inn = np.power(np.float32(500000.0), -np.arange(8, dtype=np.float32) / np.float32(8))
    for m in range(128):
        blk, d = (m // 64) * 64, m % 64
        if d < 32:
            permR[blk + d + 32, m] = -1.0
        else:
            permR[blk + d - 32, m] = 1.0
        invR[m, 0] = ir[d % 32]
        if d < 8:
            permN[blk + d + 8, m] = -1.0
            invN[m, 0] = inn[d]
        elif d < 16:
            permN[blk + d - 8, m] = 1.0
            invN[m, 0] = inn[d - 8]
    c["permR"] = permR.astype(bf)
    c["permN"] = permN.astype(bf)
    c["inv2"] = np.concatenate([invR, invN], axis=1).astype(np.float64) / (2 * np.pi)
    c["inv2"] = c["inv2"].astype(np.float32)
    H = 8
    log_g = np.log1p(-np.power(2.0, -5.0 - np.arange(H, dtype=np.float64)))
    idx = np.arange(128, dtype=np.float64)
    dec = np.zeros((128, H, 128), np.float32)
    for h in range(H):
        diff = idx[None, :] - idx[:, None]
        dec[:, h, :] = np.where(diff >= 0, np.exp(np.maximum(diff, 0) * log_g[h]), 0.0)
    c["decayT"] = dec
    c["xi"] = np.exp((idx[:, None] + 1.0) * log_g[None, :]).astype(np.float32)
    c["zeta"] = np.exp((127.0 - idx[:, None]) * log_g[None, :]).astype(np.float32)
    cd = np.exp(128.0 * log_g)
    cdv = np.zeros((128, 4), np.float32)
    for m in range(128):
        for p in range(4):
            cdv[m, p] = cd[2 * p + m // 64]
    c["cdv"] = cdv
    keys = np.arange(S_LEN)
    c["onehot"] = (keys[None, :] // 64 == np.arange(64)[:, None]).astype(np.float32).astype(bf)
    kk = np.arange(128)[:, None]
    qq = np.arange(128)[None, :]
    c["tri"] = (-30000.0 * (1.0 - np.stack([(kk <= qq), (kk > qq)], axis=1).astype(np.float32))).astype(bf)
    cm = np.zeros((NS, 2, 128, 512), np.float32)
    for t in range(NS):
        for nt in range(2):
            n = np.arange(128)[:, None] + 128 * nt
            q = 512 * t + np.arange(512)[None, :]
            cm[t, nt] = ((16 * n + 31 <= q) & (n < 255))
    c["cmpmask"] = (-30000.0 * (1.0 - cm)).astype(bf)
    fb = np.zeros((NT, 128, 64), np.float32)
    for qt in range(NT):
        for ql in range(128):
            cur = (qt * 128 + ql) // 64
            fb[qt, ql, :] = np.where(np.arange(64) > cur, -1e30, 0.0)
            fb[qt, ql, 0] = 1e9
            if cur - 1 >= 0:
                fb[qt, ql, cur - 1] = 3e9
            fb[qt, ql, cur] = 2e9
    c["fb"] = fb
    Nc = 255
    cs = np.arange(Nc) * 16
    ce = cs + 31
    ss = np.arange(64) * 64
    ov = ((cs[:, None] <= ss[None, :] + 63) & (ce[:, None] >= ss[None, :])).astype(np.float32)
    ova = np.zeros((256, 65), np.float32)
    ova[:Nc, :64] = ov
    ova[:Nc, 64] = 1.0
    c["ovl"] = ova.astype(bf)
    return c


CONST_SPECS = None


def build(debug=()):
    nc = bass.Bass("TRN2", target_bir_lowering=False)
    consts = make_consts()
    dts = {np.dtype(np.float32): F32, np.dtype(ml_dtypes.bfloat16): BF16}

    def dbuf(name):
        b = Buf(name)
        b.dram = True
        return b

    def din(name, shape, dt):
        return V(nc.dram_tensor(name, list(shape), dt, kind="ExternalInput").ap(), dbuf(name))

    x_d = din("x", [S_LEN, D], F32)
    c_d = din("c", [D], F32)
    pos_d = din("positions", [1, S_LEN], I32)
    wada_d = din("w_ada", [D, 3 * D], F32)
    bada_d = din("b_ada", [3 * D], F32)
    gpre_d = din("g_pre", [D], F32)
    gpost_d = din("g_post", [D], F32)
    win_d = din("w_in", [D, PW], F32)
    wout_d = din("w_out", [D, D], F32)
    pek_d = din("cmp_pe_k", [2048], F32)
    w1k_d = din("cmp_w1_k", [2048, 256], F32)
    w2k_d = din("cmp_w2_k", [256, 64], F32)
    pev_d = din("cmp_pe_v", [2048], F32)
    w1v_d = din("cmp_w1_v", [2048, 256], F32)
    w2v_d = din("cmp_w2_v", [256, 64], F32)
    cd_ = {k: din("k_" + k, v.shape, dts[v.dtype]) for k, v in consts.items()}
    out_d = V(nc.dram_tensor("out", [S_LEN, D], F32, kind="ExternalOutput").ap(), dbuf("out"))
    dbg = {}

    def dbg_out(name, shape, dt=F32):
        dbg[name] = V(nc.dram_tensor("dbg_" + name, list(shape), dt, kind="ExternalOutput").ap(), Buf(name))
        return dbg[name]

    def scratch(name, shape, dt):
        return V(nc.dram_tensor(name, list(shape), dt).ap(), dbuf(name))

    tabs_d = scratch("tabs_s", [4, 128, S_LEN], F32)
    qT_d = scratch("qT_s", [8, 64, S_LEN], BF16)
    sng_d = scratch("sng_s", [S_LEN, 512], BF16)
    gates_d = scratch("gates_s", [S_LEN, 24], F32)
    mret_d = scratch("mret_s", [S_LEN, 512], BF16)
    ks_d = scratch("ks_s", [64, 2, S_LEN], BF16)
    kw_d = scratch("kw_s", [64, 2, S_LEN], BF16)
    vs_d = scratch("vs_s", [S_LEN, 2, 64], BF16)
    vw_d = scratch("vw_s", [S_LEN, 2, 64], BF16)

    with ExitStack() as st0:
        S = Sched(nc, st0)

        def alloc(st, name, shape, dt):
            t = st.enter_context(nc.sbuf_tensor(name, list(shape), dt))
            return V(t[:], Buf(name))

        ps = []
        for i in range(8):
            t = st0.enter_context(nc.psum_tensor("ps%d" % i, [128, 512], F32))
            ps.append(V(t[:], Buf("ps%d" % i)))
            ps[-1].bufs[0].excl = True

        K = {}
        deferred = []
        for name in ("ident_f", "ident_b", "ones_f", "permR", "permN", "inv2", "decayT", "xi", "zeta", "cdv",
                     "tri", "ovl"):
            v = consts[name]
            if name == "ovl":
                K[name] = alloc(st0, "c_" + name, [128, 2, 65], BF16)
                deferred.append(lambda name=name: S.dma("sp", K[name], cd_[name].re("(t p) c -> p t c", p=128)))
            else:
                K[name] = alloc(st0, "c_" + name, v.shape, dts[v.dtype])
                deferred.append(lambda name=name: S.dma("sp", K[name], cd_[name]))
        kvcT = alloc(st0, "kvcT", [64, 2, 2, 256], BF16)
        S.memset(kvcT, 0.0)
        ctab = alloc(st0, "ctab", [64, 2, 256], F32)
        Gs = alloc(st0, "Gs", [128, 8], F32)
        shf = alloc(st0, "shf", [128, 8], F32)
        gGb = alloc(st0, "gGb", [128, D], F32)
        b1 = alloc(st0, "b1", [128, 2, 2], F32)

        with ExitStack() as stA:
            win = alloc(stA, "win", [128, 8, PW], BF16)
            for k in range(8):
                deferred.append(lambda k=k: S.dma("pool", win[:, k, :], win_d[k * 128:(k + 1) * 128, :]))
            w1 = alloc(stA, "w1", [128, 2, 16, 256], BF16)
            w2 = alloc(stA, "w2", [128, 2, 2, 64], BF16)
            wkc = alloc(stA, "wkc", [128, 8, 4, 128], BF16)

            with ExitStack() as stP:
                wadaf = [alloc(stP, "wadaf%d" % i, [128, 3 * D], F32) for i in range(2)]
                def rot_tables_multi(posf, n, jobs, tmps):
                    for (inv_col, phase, outv), (u, ki, kf) in zip(jobs, tmps):
                        if phase == 0.0:
                            S.ts(u[:, 0:n], posf, inv_col, None, ALU.mult)
                        else:
                            S.ts(u[:, 0:n], posf, inv_col, phase, ALU.mult, ALU.add)
                    for (inv_col, phase, outv), (u, ki, kf) in zip(jobs, tmps):
                        S.copy(ki[:, 0:n], u[:, 0:n])
                    for (inv_col, phase, outv), (u, ki, kf) in zip(jobs, tmps):
                        S.copy(kf[:, 0:n], ki[:, 0:n])
                    for (inv_col, phase, outv), (u, ki, kf) in zip(jobs, tmps):
                        S.tt(u[:, 0:n], u[:, 0:n], kf[:, 0:n], ALU.subtract)
                    for (inv_col, phase, outv), (u, ki, kf) in zip(jobs, tmps):
                        S.act(outv, u[:, 0:n], AF.Sin, scale=2 * PI)

                posi_all = alloc(stP, "posi_all", [128, S_LEN], I32)
                S.dma("sp", posi_all, V(pos_d.ap[0:1, :].partition_broadcast(128), pos_d.bufs))
                for fn in deferred:
                    fn()
                S.dma("pool", w1[:, 0], w1k_d.re("(j p) h -> p j h", p=128))
                S.dma("pool", w1[:, 1], w1v_d.re("(j p) h -> p j h", p=128))
                S.dma("pool", w2[:, 0], w2k_d.re("(c p) d -> p c d", p=128))
                S.dma("pool", w2[:, 1], w2v_d.re("(c p) d -> p c d", p=128))
                posi = [alloc(stP, "posi%d" % i, [128, 512], I32) for i in range(2)]
                posf = [alloc(stP, "posf%d" % i, [128, 512], F32) for i in range(2)]
                tmps = [(alloc(stP, "tu%d" % i, [128, 512], F32), alloc(stP, "tki%d" % i, [128, 512], I32),
                         alloc(stP, "tkf%d" % i, [128, 512], F32)) for i in range(4)]
                tout2 = [[alloc(stP, "tout%d_%d" % (i, j), [128, 512], F32) for i in range(4)] for j in range(2)]
                for ch in range(NS):
                    tout = tout2[ch % 2]
                    sl = slice(ch * 512, (ch + 1) * 512)
                    pi_, pf_ = posi[ch % 2], posf[ch % 2]
                    S.copy(pf_, posi_all[:, sl])
                    jobs = [(K["inv2"][:, 0:1], 0.25, tout[0]), (K["inv2"][:, 0:1], 0.0, tout[1]),
                            (K["inv2"][:, 1:2], 0.25, tout[2]), (K["inv2"][:, 1:2], 0.0, tout[3])]
                    rot_tables_multi(pf_, 512, jobs, tmps)
                    for i in range(4):
                        S.dma("act", tabs_d[i][:, sl], tout[i], sembuf=tout[i].bufs[0])
                S.memset(posi[0][0:64, 0:256], 0)
                S.dma("sp", posi[0][0:64, 0:255],
                      V(pos_d.ap[0:1, 31:4096:16].partition_broadcast(64), pos_d.bufs),
                      allow_slow_non_contiguous=True)
                S.copy(posf[0][0:64, 0:256], posi[0][0:64, 0:256])
                jobs = [(K["inv2"][0:64, 1:2], 0.25, ctab[:, 0, :]), (K["inv2"][0:64, 1:2], 0.0, ctab[:, 1, :])]
                rot_tables_multi(posf[0][0:64, 0:256], 256, jobs,
                                 [tuple(x[0:64] for x in tmps[0]), tuple(x[0:64] for x in tmps[1])])
                cs_ = alloc(stP, "cs", [128, 8], F32)
                S.dma("sp", cs_, c_d.re("(k p) -> p k", p=128), allow_slow_non_contiguous=True)
                csb = alloc(stP, "csb", [128, 8], F32)
                S.act(csb, cs_, AF.Silu)
                badaT = alloc(stP, "badaT", [128, 24], F32)
                S.dma("sp", badaT, bada_d.re("(k p) -> p k", p=128), allow_slow_non_contiguous=True)
                gpp = alloc(stP, "gpp", [128, 2, 8], F32)
                S.dma("sp", gpp[:, 0], gpre_d.re("(k p) -> p k", p=128), allow_slow_non_contiguous=True)
                S.dma("sp", gpp[:, 1], gpost_d.re("(k p) -> p k", p=128), allow_slow_non_contiguous=True)
                S.memset(ps[0][:, 0:24], 0.0)
                for k in range(8):
                    wf = wadaf[k % 2]
                    S.dma("sp", wf, wada_d[k * 128:(k + 1) * 128, :])
                    for jc in range(24):
                        S.mm(ps[0][:, jc:jc + 1], wf[:, jc * 128:(jc + 1) * 128], csb[:, k:k + 1],
                             start=False, stop=(k == 7), skip_group_check=True)
                mod = alloc(stP, "mod", [128, 24], F32)
                S.tt(mod, ps[0][:, 0:24], badaT, ALU.add)
                S.copy(shf, mod[:, 0:8])
                S.stt(Gs, mod[:, 8:16], 1.0, gpp[:, 0], ALU.add, ALU.mult)
                gG = alloc(stP, "gG", [128, 8], F32)
                S.tt(gG, mod[:, 16:24], gpp[:, 1], ALU.mult)
                dg = alloc(stP, "dg", [128, 128], F32)
                for k in range(8):
                    S.ts(dg, K["ident_f"], gG[:, k:k + 1], None, ALU.mult)
                    S.mm(ps[1 + k // 4][:, (k % 4) * 128:(k % 4 + 1) * 128], K["ones_f"], dg)
                S.copy(gGb[:, 0:512], ps[1])
                S.copy(gGb[:, 512:1024], ps[2])
                pef = alloc(stP, "pef", [128, 2, 16], F32)
                S.dma("sp", pef[:, 0], pek_d.re("(j p) -> p j", p=128), allow_slow_non_contiguous=True)
                S.dma("sp", pef[:, 1], pev_d.re("(j p) -> p j", p=128), allow_slow_non_contiguous=True)
                peb = alloc(stP, "peb", [128, 2, 16], BF16)
                S.copy(peb, pef)
                for kv in range(2):
                    for hc in range(2):
                        for j in range(16):
                            S.mm(ps[3][:, kv * 2 + hc:kv * 2 + hc + 1], w1[:, kv, j, hc * 128:(hc + 1) * 128],
                                 peb[:, kv, j:j + 1], start=(j == 0), stop=(j == 15))
                S.copy(b1.re("p a b -> p (a b)"), ps[3][:, 0:4])

            for i4 in range(4):
                c0 = 2560 + 64 * i4
                S.copy(wkc[:, :, i4, 0:64], win[:, :, c0:c0 + 64], eng="pool")
                S.copy(wkc[:, :, i4, 64:128], win[:, :, c0:c0 + 64], eng="pool")
            S.barrier()

            with ExitStack() as stW:
                xb_ = [alloc(stW, "xt%d" % i, [128, D], F32) for i in range(2)]
                junk = alloc(stW, "junk", [128, D], BF16)
                ssq = alloc(stW, "ssq", [128, 1], F32)
                rstd = alloc(stW, "rstd", [128, 1], F32)
                xn = [alloc(stW, "xn", [128, D], BF16)] * 2
                hTs = [alloc(stW, "hT%d" % i, [128, 8, 512], BF16) for i in range(2)]
                tab = alloc(stW, "tab", [128, 4, 512], F32)
                rqs = [alloc(stW, "rq%d" % i, [128, 4, 512], BF16) for i in range(2)]
                rks = [alloc(stW, "rk%d" % i, [128, 4, 512], BF16) for i in range(2)]
                xbs = [alloc(stW, "xbs%d" % i, [128, 512], BF16) for i in range(2)]
                t1s = [alloc(stW, "t1", [128, 512], F32)] * 2
                t2s = [alloc(stW, "t2", [128, 512], F32)] * 2
                qn = [alloc(stW, "qn%d" % i, [128, 512], BF16) for i in range(2)]
                kst = [alloc(stW, "kst%d" % i, [64, 512], BF16) for i in range(2)]
                vrets = [alloc(stW, "vret%d" % i, [128, 4, 512], BF16) for i in range(2)]
                sgs = [alloc(stW, "sg%d" % i, [128, 4, 512], BF16) for i in range(2)]
                sng = [alloc(stW, "sng%d" % i, [128, 512], BF16) for i in range(2)]
                vst = [alloc(stW, "vst%d" % i, [128, 2, 2, 64], BF16) for i in range(2)]
                gst = [alloc(stW, "gst%d" % i, [128, 24], F32) for i in range(2)]
                KC = [alloc(stW, "KC%d" % i, [128, 2, 2, 528], BF16) for i in range(2)]
                hid = alloc(stW, "hid", [128, 2, 2, 64], BF16)
                kcx = alloc(stW, "kcx", [64, 64], BF16)
                scbs = [alloc(stW, "scb%d" % i, [128, 8, 128], BF16) for i in range(2)]
                kzs = [alloc(stW, "kz%d" % i, [128, 512], BF16) for i in range(2)]
                R32 = alloc(stW, "R32", [128, 4, 128], F32)
                Rb = alloc(stW, "Rb", [128, 4, 128], BF16)
                o1 = alloc(stW, "o1", [128, 8, 64], F32)
                o2 = alloc(stW, "o2", [128, 8, 64], F32)
                st8 = alloc(stW, "st8", [128, 4, 8], F32)
                mst = [alloc(stW, "mst%d" % i, [128, 512], BF16) for i in range(2)]
                S.memset(R32, 0.0)
                S.memset(Rb, 0.0)
                for i in range(2):
                    S.memset(KC[i], 0.0)
                print("passA sbuf_base", nc.sbuf_base, "top", nc.sbuf_top)

                rotc = [0]
                rot_pending = []

                def rotary(src_ps, npart, perm, cosv, sinv, dest, scale, then=None):
                    slot = rotc[0] % 2
                    rotc[0] += 1
                    xb = xbs[slot][0:npart]
                    S.act(xb, src_ps, AF.Copy, scale=scale)

                    def fin():
                        t1, t2 = t1s[slot], t2s[slot]
                        S.mm(ps[7][0:npart, :], perm, xb)
                        S.tt(t1[0:npart], ps[7][0:npart, :], sinv, ALU.mult)
                        S.tt(t2[0:npart], xb, cosv, ALU.mult, eng="pool")
                        S.tt(dest, t1[0:npart], t2[0:npart], ALU.add)
                        if then is not None:
                            then()

                    while rot_pending:
                        rot_pending.pop(0)()
                    rot_pending.append(fin)

                def rot_flush():
                    while rot_pending:
                        rot_pending.pop(0)()

                pscyc = [0]

                def next_ps():
                    i = pscyc[0] % 3
                    pscyc[0] += 1
                    return ps[i]

                def genA1(t):
                    hT = hTs[t % 2]
                    for j in range(4):
                        tt_ = 4 * t + j
                        xt = xb_[tt_ % 2]
                        S.dma("sp", xt, x_d[tt_ * 128:(tt_ + 1) * 128, :])
                        S.act(junk, xt, AF.Square, accum_out=ssq)
                        S.ts(rstd, ssq, 1.0 / D, 1e-6, ALU.mult, ALU.add)
                        S.act(rstd, rstd, AF.Sqrt)
                        S.recip(rstd, rstd)
                        xnb = xn[tt_ % 2]
                        S.act(xnb, xt, AF.Copy, scale=rstd)
                        yield
                        pb = ps[7].cast(BF16)
                        for k in range(8):
                            S.tr(pb[:, k * 128:(k + 1) * 128], xnb[:, k * 128:(k + 1) * 128], K["ident_b"])
                        for k in range(8):
                            S.act(hT[:, k, j * 128:(j + 1) * 128], pb[:, k * 128:(k + 1) * 128], AF.Identity,
                                  scale=Gs[:, k:k + 1], bias=shf[:, k:k + 1])
                        yield

                def genA2(t):
                    T0 = 512 * t
                    hT, rq, rk, vret, sg = hTs[t % 2], rqs[t % 2], rks[t % 2], vrets[t % 2], sgs[t % 2]
                    S.dma("sp", tab, tabs_d.re("a p n -> p a n")[:, :, T0:T0 + 512])

                    def proj_fm(wsel, M):
                        p_ = next_ps()
                        for k in range(8):
                            S.mm(p_[0:M, :], wsel(k), hT[:, k, :], start=(k == 0), stop=(k == 7))
                        return p_

                    for p in range(4):
                        pq = proj_fm(lambda k, p=p: win[:, k, 128 * p:128 * (p + 1)], 128)
                        rotary(pq, 128, K["permR"], tab[:, 0, :], tab[:, 1, :], rq[:, p, :], 0.125)
                        yield
                        pk = proj_fm(lambda k, p=p: win[:, k, 512 + 128 * p:512 + 128 * (p + 1)], 128)
                        rotary(pk, 128, K["permR"], tab[:, 0, :], tab[:, 1, :], rk[:, p, :], 1.0)
                        yield
                    for p in range(4):
                        pq = proj_fm(lambda k, p=p: win[:, k, 2048 + 128 * p:2048 + 128 * (p + 1)], 128)
                        qb = qn[p % 2]

                        def st_q(p=p, qb=qb):
                            S.dma("sp", qT_d[2 * p:2 * p + 2].re("h d n -> (h d) n")[:, T0:T0 + 512], qb,
                                  sembuf=qb.bufs[0])
                        rotary(pq, 128, K["permN"], tab[:, 2, :], tab[:, 3, :], qb, 0.125, then=st_q)
                        yield
                    for i4, (c0, dst) in enumerate(((2816, ks_d), (2880, ks_d), (3072, kw_d), (3136, kw_d))):
                        g = i4 % 2
                        pk = proj_fm(lambda k, c0=c0: win[:, k, c0:c0 + 64], 64)
                        kb = kst[i4 % 2]

                        def st_k(dst=dst, g=g, kb=kb):
                            S.dma("sp", dst[:, g, T0:T0 + 512], kb, sembuf=kb.bufs[0])
                        rotary(pk[0:64, :], 64, K["permN"][0:64, 0:64], tab[0:64, 2, :], tab[0:64, 3, :], kb, 1.0,
                               then=st_k)
                        yield
                    KCc, KCp = KC[t % 2], KC[(t + 1) % 2]
                    for kv in range(2):
                        for g in range(2):
                            pk = proj_fm(lambda k, i4=kv * 2 + g: wkc[:, k, i4, :], 128)
                            S.copy(KCc[0:64, kv, g, 16:528], pk[0:64, :], eng="act")
                            S.copy(KCc[64:128, kv, g, 15:527], pk[64:128, :], eng="act")
                            if kv == 0 and g == 0:
                                rot_flush()
                            yield
                    if t > 0:
                        S.copy(KCc[0:64, :, :, 0:16], KCp[0:64, :, :, 512:528], eng="pool")
                        S.copy(KCc[64:128, :, :, 0:15], KCp[64:128, :, :, 512:527], eng="pool")

                    for j in range(4):
                        tt_ = 4 * t + j
                        lhs = lambda k: hT[:, k, j * 128:(j + 1) * 128]
                        for gi, (c0, n) in enumerate(((1024, 512), (1536, 512), (3352, 512), (2944, 408))):
                            p_ = next_ps()
                            for k in range(8):
                                S.mm(p_[:, 0:n], lhs(k), win[:, k, c0:c0 + n], start=(k == 0), stop=(k == 7))
                            if gi == 0:
                                S.copy(vret[:, j, :], p_, eng="act")
                            elif gi == 1:
                                S.act(sg[:, j, :], p_, AF.Silu)
                            elif gi == 2:
                                sb_ = sng[tt_ % 2]
                                S.act(sb_, p_, AF.Silu)
                                S.dma("sp", sng_d[tt_ * 128:(tt_ + 1) * 128, :], sb_, sembuf=sb_.bufs[0])
                            else:
                                vb = vst[tt_ % 2]
                                S.copy(vb[:, 0].re("p g d -> p (g d)"), p_[:, 0:128])
                                S.copy(vb[:, 1].re("p g d -> p (g d)"), p_[:, 256:384])
                                S.dma("sp", vs_d[tt_ * 128:(tt_ + 1) * 128], vb[:, 0], sembuf=vb.bufs[0])
                                S.dma("sp", vw_d[tt_ * 128:(tt_ + 1) * 128], vb[:, 1], sembuf=vb.bufs[0])
                                gb = gst[tt_ % 2]
                                S.act(gb, p_[:, 384:408], AF.Sigmoid)
                                S.dma("sp", gates_d[tt_ * 128:(tt_ + 1) * 128, :], gb, sembuf=gb.bufs[0])
                            yield

                    for kv in range(2):
                        for hc in range(2):
                            p_ = next_ps()
                            po = p_[:, 0:64].re("p (g r) -> p g r", g=2)
                            for j in range(16):
                                S.mm(po, w1[:, kv, j, hc * 128:(hc + 1) * 128],
                                     KCc[:, kv, :, 2 * j:2 * j + 16 * 31 + 1:16], start=(j == 0), stop=(j == 15))
                            S.act(hid[:, kv, hc, :], p_[:, 0:64], AF.Silu, bias=b1[:, kv, hc:hc + 1])
                            yield
                        p_ = next_ps()
                        for hc in range(2):
                            S.mm(p_[0:64, 0:64], w2[:, kv, hc, :], hid[:, kv, hc, :], start=(hc == 0), stop=(hc == 1))
                        r0 = 1 if t == 0 else 0
                        n0 = 32 * t - 1
                        for g in range(2):
                            dst = kvcT[:, kv, g, n0 + r0:n0 + 32]
                            src = p_[0:64, g * 32 + r0:g * 32 + 32]
                            if kv == 1:
                                S.copy(dst, src)
                            else:
                                S.copy(kcx[:, g * 32:(g + 1) * 32], p_[0:64, g * 32:(g + 1) * 32], eng="act")
                        if kv == 0:
                            yield
                            t1, t2 = t1s[0], t2s[0]
                            S.mm(ps[7][0:64, 0:64], K["permN"][0:64, 0:64], kcx)
                            for g in range(2):
                                cs0 = ctab[:, 0, n0 + r0:n0 + 32]
                                sn0 = ctab[:, 1, n0 + r0:n0 + 32]
                                S.tt(t1[0:64, 0:32 - r0], ps[7][0:64, g * 32 + r0:g * 32 + 32], sn0, ALU.mult)
                                S.tt(t2[0:64, 0:32 - r0], kcx[:, g * 32 + r0:g * 32 + 32], cs0, ALU.mult)
                                S.tt(kvcT[:, 0, g, n0 + r0:n0 + 32], t1[0:64, 0:32 - r0], t2[0:64, 0:32 - r0],
                                     ALU.add)
                        yield

                def genA(t):
                    a1 = genA1(t + 1) if t + 1 < NS else None
                    for i, _ in enumerate(genA2(t)):
                        yield
                        if a1 is not None and i % 4 == 3:
                            try:
                                next(a1)
                                yield
                            except StopIteration:
                                a1 = None
                    if a1 is not None:
                        for _ in a1:
                            yield

                def genR(t):
                    hT, rq, rk, vret, sg = hTs[t % 2], rqs[t % 2], rks[t % 2], vrets[t % 2], sgs[t % 2]
                    zb = V(K["zeta"].ap.unsqueeze(2).to_broadcast([128, 8, 64]), K["zeta"].bufs)

                    def stage1(j):
                        cs = slice(j * 128, (j + 1) * 128)
                        scb, kz = scbs[j % 2], kzs[j % 2]
                        for p in range(4):
                            for hh in range(2):
                                rows = slice(hh * 64, hh * 64 + 64)
                                S.mm(ps[4 + hh][:, p * 128:(p + 1) * 128], rk[rows, p, cs], rq[rows, p, cs])
                        for hh in range(2):
                            S.tt(scb[:, hh::2, :], ps[4 + hh].re("p (h i) -> p h i", h=4),
                                 K["decayT"][:, hh::2, :], ALU.mult)
                        pb = ps[6].cast(BF16)
                        for p in range(4):
                            S.tr(pb[:, p * 128:(p + 1) * 128], rk[:, p, cs], K["ident_b"])
                        S.tt(kz.re("p (h d) -> p h d", h=8), pb[:, 0:512].re("p (h d) -> p h d", h=8), zb, ALU.mult)

                    stage1(0)
                    yield
                    for j in range(4):
                        tt_ = 4 * t + j
                        cs = slice(j * 128, (j + 1) * 128)
                        scb, kz = scbs[j % 2], kzs[j % 2]
                        pi_ = ps[3]
                        for h in range(8):
                            S.mm(pi_[:, h * 64:(h + 1) * 64], scb[:, h, :], vret[:, j, h * 64:(h + 1) * 64])
                        for p in range(4):
                            for hh in range(2):
                                rows = slice(hh * 64, hh * 64 + 64)
                                S.mm(ps[4 + hh][:, p * 64:(p + 1) * 64], rq[rows, p, cs],
                                     Rb[rows, p, hh * 64:hh * 64 + 64])
                        pkv = ps[6]
                        for p in range(4):
                            S.mm(pkv[:, p * 128:(p + 1) * 128], kz[:, p * 128:(p + 1) * 128],
                                 vret[:, j, p * 128:(p + 1) * 128])
                        for hh in range(2):
                            xib = V(K["xi"].ap[:, hh::2].unsqueeze(2).to_broadcast([128, 4, 64]), K["xi"].bufs)
                            S.tt(o1[:, hh::2, :], ps[4 + hh][:, 0:256].re("p (h d) -> p h d", h=4), xib, ALU.mult)
                        S.tt(o1, o1, pi_.re("p (h d) -> p h d", h=8), ALU.add)
                        for p in range(4):
                            S.stt(R32[:, p, :], R32[:, p, :], K["cdv"][:, p:p + 1], pkv[:, p * 128:(p + 1) * 128],
                                  ALU.mult, ALU.add)
                        S.copy(Rb, R32, eng="pool")
                        yield
                        if j + 1 < 4:
                            stage1(j + 1)
                            yield
                        S.reduce(st8[:, 0, :], o1, ALU.add)
                        S.tt(o2, o1, o1, ALU.mult, eng="pool")
                        S.reduce(st8[:, 1, :], o2, ALU.add)
                        S.ts(st8[:, 0, :], st8[:, 0, :], 1.0 / 64, None, ALU.mult)
                        S.tt(st8[:, 2, :], st8[:, 0, :], st8[:, 0, :], ALU.mult)
                        S.stt(st8[:, 1, :], st8[:, 1, :], 1.0 / 64, st8[:, 2, :], ALU.mult, ALU.subtract)
                        S.ts(st8[:, 1, :], st8[:, 1, :], 1e-5, None, ALU.add)
                        yield
                        S.act(st8[:, 1, :], st8[:, 1, :], AF.Sqrt)
                        yield
                        S.recip(st8[:, 3, :], st8[:, 1, :])
                        yield
                        mb = V(st8.ap[:, 0, :].unsqueeze(2).to_broadcast([128, 8, 64]), st8.bufs)
                        rb_ = V(st8.ap[:, 3, :].unsqueeze(2).to_broadcast([128, 8, 64]), st8.bufs)
                        S.tt(o2, o1, mb, ALU.subtract)
                        S.tt(o2, o2, rb_, ALU.mult)
                        mo = mst[tt_ % 2]
                        S.tt(mo, o2.re("p h d -> p (h d)"), sg[:, j, :], ALU.mult)
                        S.dma("sp", mret_d[tt_ * 128:(tt_ + 1) * 128, :], mo, sembuf=mo.bufs[0])
                        yield

                def interleave(ga, gr, ratio=2):
                    a_done = ga is None
                    r_done = gr is None
                    while not (a_done and r_done):
                        if not r_done:
                            try:
                                next(gr)
                            except StopIteration:
                                r_done = True
                        for _ in range(ratio):
                            if not a_done:
                                try:
                                    next(ga)
                                except StopIteration:
                                    a_done = True

                for _ in genA1(0):
                    pass
                interleave(genA(0), None)
                for t in range(NS):
                    interleave(genA(t + 1) if t + 1 < NS else None, genR(t))
                hT = hTs[(NS - 1) % 2]

                if "passA" in debug:
                    S.barrier()
                    d_hT = dbg_out("hT", [128, 8, 512], BF16)
                    S.dma("sp", d_hT, hT, is_output=True)
                    d_kvc = dbg_out("kvcT", [64, 2, 2, 256], BF16)
                    S.dma("sp", d_kvc, kvcT, is_output=True)
                    for nm, src in (("qT", qT_d), ("sng", sng_d), ("gates", gates_d), ("mret", mret_d), ("ks", ks_d),
                                    ("kw", kw_d), ("vs", vs_d), ("vw", vw_d), ("tabs", tabs_d)):
                        shp = list(src.ap.shape)
                        dd = dbg_out(nm, shp, src.ap.dtype)
                        S.dma("sp", dd, src, is_output=True)
                    dG = dbg_out("gGb", [128, D])
                    S.dma("sp", dG, gGb, is_output=True)
            S.barrier()

        if "passA" in debug:
            with nc.Block() as block:
                S.emit(block)
            return nc, consts

        PASSB(nc, S, st0, alloc, ps, K, cd_, consts, kvcT, gGb, x_d, wout_d, out_d, qT_d, sng_d, gates_d, mret_d,
              ks_d, kw_d, vs_d, vw_d, debug, dbg_out)
        with nc.Block() as block:
            S.emit(block)
    return nc, consts


def PASSB(nc, S, st0, alloc, ps, K, cd_, consts, kvcT, gGb, x_d, wout_d, out_d, qT_d, sng_d, gates_d, mret_d,
          ks_d, kw_d, vs_d, vw_d, debug, dbg_out):
    with ExitStack() as stB:
        wout = alloc(stB, "wout", [128, 8, D], BF16)
        for k in range(8):
            S.dma("pool", wout[:, k, :], wout_d[k * 128:(k + 1) * 128, :])
        late_dmas = []
        ksA = alloc(stB, "ksA", [128, 2, S_LEN], BF16)
        kwT = alloc(stB, "kwT", [128, 2, S_LEN], BF16)
        S.memset(kwT[64:128], 0.0)
        kcA = alloc(stB, "kcA", [128, 2, 256], BF16)
        S.memset(kcA[64:128], 0.0)
        S.copy(kcA[0:64], kvcT[:, 0])
        vsA = alloc(stB, "vsA", [128, NT, 2, 65], BF16)
        vwA = alloc(stB, "vwA", [128, NT, 2, 65], BF16)
        S.memset(vsA[:, :, :, 64:65], 1.0)
        S.memset(vwA[:, :, :, 64:65], 1.0)
        late_dmas.append(lambda: S.dma("sp", kwT[0:64], kw_d))
        for g in range(2):
            late_dmas.append(lambda g=g: S.dma("sp", vwA[:, :, g, 0:64],
                                               vw_d.re("(t p) g d -> p t g d", p=128)[:, :, g, :]))
        late_dmas.append(lambda: S.dma("sp", ksA[0:64], ks_d))
        for g in range(2):
            late_dmas.append(lambda g=g: S.dma("sp", ksA[64:128, g, :], cd_["onehot"]))
        for g in range(2):
            late_dmas.append(lambda g=g: S.dma("sp", vsA[:, :, g, 0:64],
                                               vs_d.re("(t p) g d -> p t g d", p=128)[:, :, g, :]))
        vcA = alloc(stB, "vcA", [128, 2, 2, 65], BF16)
        S.memset(vcA, 1.0)
        for g in range(2):
            for nt in range(2):
                pb = ps[7].cast(BF16)
                S.tr(pb[:, 0:64], kvcT[:, 1, g, nt * 128:(nt + 1) * 128], K["ident_b"][0:64, 0:64])
                S.copy(vcA[:, nt, g, 0:64], pb[:, 0:64])
        Qs = [alloc(stB, "Qa%d" % i, [128, 8, 512], BF16) for i in range(2)]
        Qlo = [Buf("Qlo%d" % i) for i in range(2)]
        Qhi = [[Buf("Qhi%d_%d" % (i, g)) for g in range(2)] for i in range(2)]
        for i in range(2):
            S.memset(V(Qs[i].ap[64:128], Qhi[i]), 0.0)
        cmk = [alloc(stB, "cmk%d" % i, [128, 2, 512], BF16) for i in range(2)]
        fbt = [alloc(stB, "fbt%d" % i, [128, 4, 64], F32) for i in range(2)]
        gts = [alloc(stB, "gts%d" % i, [128, 4, 24], F32) for i in range(2)]
        sngb = [alloc(stB, "sngb%d" % i, [128, 4, 512], BF16) for i in range(2)]
        NPT = 13
        PT = [alloc(stB, "PT%d" % i, [128, 512], BF16) for i in range(NPT)]
        accs = [alloc(stB, "acc%d" % i, [128, 4, 512], F32) for i in range(2)]
        impacc = alloc(stB, "impacc", [128, 4, 64], F32)
        rls = [alloc(stB, "rl%d" % i, [128, 4], F32) for i in range(2)]
        sc4s = [alloc(stB, "sc4%d" % i, [128, 4], F32) for i in range(2)]
        scr = alloc(stB, "scr", [128, 64], F32)
        wk64 = alloc(stB, "wk64", [128, 64], F32)
        m8a = alloc(stB, "m8a", [128, 8], F32)
        m8b = alloc(stB, "m8b", [128, 8], F32)
        thr = alloc(stB, "thr", [128, 1], F32)
        selbs = [alloc(stB, "selb%d" % i, [128, 128], BF16) for i in range(4)]
        for i in range(4):
            S.memset(selbs[i], 0.0)
        scrs = [alloc(stB, "scr%d" % i, [128, 64], F32) for i in range(4)]
        wk64s = [alloc(stB, "wk64%d" % i, [128, 64], F32) for i in range(4)]
        m8as = [alloc(stB, "m8a%d" % i, [128, 8], F32) for i in range(4)]
        m8bs = [alloc(stB, "m8b%d" % i, [128, 8], F32) for i in range(4)]
        thrs = [alloc(stB, "thr%d" % i, [128, 1], F32) for i in range(4)]
        mix = [alloc(stB, "mix%d" % i, [128, D], BF16) for i in range(4)]
        mixTs = [alloc(stB, "mixT%d" % i, [128, 8, 128], BF16) for i in range(4)]
        xres = [alloc(stB, "xres%d" % i, [128, D], F32) for i in range(4)]
        zts = [alloc(stB, "zt%d" % i, [128, D], F32) for i in range(2)]
        junk2 = alloc(stB, "junk2", [128, D], BF16)
        ss2s = [alloc(stB, "ss2%d" % i, [128, 4], F32) for i in range(2)]
        ot = [alloc(stB, "ot%d" % i, [128, D], F32) for i in range(2)]
        cyc = {"pt": 0, "sc": 0, "o": 0, "imp": 0, "ev": 0}
        LOOK = 9
        pend = []

        fins = []

        def run_fins(limit_hseq=None, tick=False):
            keep = []
            ready = []
            for ent in fins:
                if tick:
                    ent[0] -= 1
                if ent[0] <= 0 or (limit_hseq is not None and ent[1] <= limit_hseq):
                    ready.append(ent)
                else:
                    keep.append(ent)
            if ready:
                last = max(fins.index(e) for e in ready)
                ready = fins[:last + 1]
                keep = fins[last + 1:]
            fins[:] = keep
            for ent in ready:
                ent[2]()

        def push(s1, s2, hseq, first_of_head):
            if first_of_head:
                while pend and pend[0][0] <= hseq - 2:
                    pend.pop(0)[1]()
                run_fins(limit_hseq=hseq - 2)
            s1()
            pend.append((hseq, s2))
            while len(pend) > LOOK:
                pend.pop(0)[1]()
            run_fins(tick=True)

        def flush():
            while pend or fins:
                while pend:
                    pend.pop(0)[1]()
                run_fins(limit_hseq=1 << 60)

        def load_inputs(t):
            T0 = 512 * t
            sl = t % 2
            S.dma("sp", V(Qs[sl].ap[0:64], [Qlo[sl]]), qT_d.re("h d n -> d h n")[:, :, T0:T0 + 512], sembuf=Qlo[sl])
            S.dma("sp", cmk[sl], cd_["cmpmask"][t].re("a p n -> p a n"))
            S.dma("sp", fbt[sl], cd_["fb"][4 * t:4 * t + 4].re("a p n -> p a n"))
            S.dma("sp", gts[sl], gates_d[T0:T0 + 512].re("(a p) n -> p a n", p=128))
            S.dma("sp", sngb[sl], sng_d[T0:T0 + 512].re("(a p) n -> p a n", p=128))

        oTs = [alloc(stB, "oT%d" % i, [65, 512], F32) for i in range(2)]
        zeros_f = alloc(stB, "zeros_f", [128, 272], F32)
        S.memset(zeros_f, 0.0)

        def attend(t, h, br, first, ktl, with_imp=False, after=None, otrans=False, act_off=False):
            sl = t % 2
            Q = Qs[sl]
            ob = ps[3 + cyc["o"] % 2]
            cyc["o"] += 1
            impb = None
            tb = None
            if with_imp or otrans:
                impb = ps[5 + cyc["imp"] % 2]
                cyc["imp"] += 1
            n = len(ktl)
            cyc["hseq"] = cyc.get("hseq", 0) + 1
            hseq = cyc["hseq"]

            def evac(src):
                rl = rls[cyc["ev"] % 2]
                sc4 = sc4s[cyc["ev"] % 2]
                cyc["ev"] += 1
                o3 = src[:, 0:272].re("p (q c) -> p q c", q=4)
                S.ts(rl, o3[:, :, 64], 1.0e-30, None, ALU.max)
                S.recip(rl, rl)
                S.tt(sc4, rl, gts[sl][:, :, 3 * h + br], ALU.mult)
                for qs in range(4):
                    dst = accs[t % 2][:, qs, h * 64:(h + 1) * 64]
                    if first and act_off:
                        S.act(dst, o3[:, qs, 0:64], AF.Copy, scale=sc4[:, qs:qs + 1])
                    elif first:
                        S.ts(dst, o3[:, qs, 0:64], sc4[:, qs:qs + 1], None, ALU.mult)
                    else:
                        S.stt(dst, o3[:, qs, 0:64], sc4[:, qs:qs + 1], dst, ALU.mult, ALU.add)
                return rl

            for idx, (kT, va, Krows, M, c0, c1, mask, ov) in enumerate(ktl):
                sp_ = ps[cyc["sc"] % 3]
                cyc["sc"] += 1
                pt = PT[cyc["pt"] % NPT]
                cyc["pt"] += 1

                def s1(idx=idx, kT=kT, Krows=Krows, M=M, c0=c0, c1=c1, mask=mask, sp_=sp_, pt=pt):
                    if idx == 0 and not otrans:
                        if act_off:
                            S.act(ob[:, 0:272], zeros_f[:, 0:272], AF.Copy)
                            S.act(impb[:, 0:272], zeros_f[:, 0:272], AF.Copy)
                        else:
                            S.memset(ob[:, 0:272], 0.0)
                            if impb is not None:
                                S.memset(impb[:, 0:272], 0.0)
                    qv = V(Q.ap[0:Krows, h, c0:c1], [Qlo[sl]] + ([Qhi[sl][h // 4]] if Krows == 128 else []))
                    S.mm(sp_[0:M, c0:c1], kT, qv, start=True, stop=(mask is None))
                    if mask is not None:
                        mv, m0, m1 = mask
                        S.mm(sp_[0:M, m0:m1], K["ident_b"][0:M, 0:M], mv, start=False, stop=True)
                    S.act(pt[0:M, c0:c1], sp_[0:M, c0:c1], AF.Exp)

                def s2(idx=idx, va=va, M=M, c0=c0, c1=c1, ov=ov, pt=pt):
                    if otrans:
                        assert idx > 0 or (c0 == 0 and c1 == 512)
                        S.mm(ob[0:65, c0:c1], va, pt[0:M, c0:c1], start=(idx == 0), stop=(idx == n - 1))
                        if idx == n - 1:
                            oT = oTs[cyc.get("ot", 0) % 2]
                            cyc["ot"] = cyc.get("ot", 0) + 1
                            S.copy(oT, ob[0:65, :])

                            def fin(oT=oT):
                                for qs in range(4):
                                    S.tr(impb[:, qs * 68:qs * 68 + 65], oT[0:65, qs * 128:(qs + 1) * 128],
                                         K["ident_f"][0:65, 0:65])
                                evac(impb)
                            fins.append([2, hseq, fin])
                        return
                    for qs in range(c0 // 128, c1 // 128):
                        S.mm(ob[:, qs * 68:qs * 68 + 65], pt[0:M, qs * 128:(qs + 1) * 128], va,
                             start=False, stop=False, skip_group_check=True)
                        if ov is not None:
                            S.mm(impb[:, qs * 68:qs * 68 + 65], pt[0:M, qs * 128:(qs + 1) * 128], ov,
                                 start=False, stop=False, skip_group_check=True)
                    if idx == n - 1:
                        rl = evac(ob)
                        if after is not None:
                            after(impb, rl)

                push(s1, s2, hseq, idx == 0)

        def b1_head(t, h, act_off):
            T0 = 512 * t
            sl = t % 2
            Q = Qs[sl]
            g, h4 = h // 4, h % 4
            nts = [nt for nt in range(2) if 16 * 128 * nt + 31 <= T0 + 511]
            ktl = []
            for nt in nts:
                M = 128 if nt == 0 else 127
                ktl.append((kcA[:, g, nt * 128:nt * 128 + M], vcA[0:M, nt, g, :], 128, M, 0, 512,
                            (cmk[sl][0:M, nt, :], 0, 512), K["ovl"][0:M, nt, :]))

            def after(impb, rl):
                i3 = impb[:, 0:272].re("p (q c) -> p q c", q=4)
                for qs in range(4):
                    if h4 == 0:
                        S.ts(impacc[:, qs, :], i3[:, qs, 0:64], rl[:, qs:qs + 1], None, ALU.mult)
                    else:
                        S.stt(impacc[:, qs, :], i3[:, qs, 0:64], rl[:, qs:qs + 1], impacc[:, qs, :],
                              ALU.mult, ALU.add)
                if h4 < 3:
                    return
                for qs in range(4):
                    S.tt(scrs[qs], impacc[:, qs, :], fbt[sl][:, qs, :], ALU.add)
                for qs in range(4):
                    S.max8(m8as[qs], scrs[qs])
                for qs in range(4):
                    S.match_replace(wk64s[qs], m8as[qs], scrs[qs], -3.0e38)
                for qs in range(4):
                    S.max8(m8bs[qs], wk64s[qs])
                for qs in range(4):
                    S.reduce(thrs[qs], m8bs[qs], ALU.min)
                for qs in range(4):
                    S.ts(thrs[qs], thrs[qs], -1.0e29, None, ALU.max)
                for qs in range(4):
                    S.ts(selbs[qs][:, 64:128], scrs[qs], thrs[qs], None, ALU.is_ge)
                for qs in range(4):
                    S.ts(selbs[qs][:, 64:128], selbs[qs][:, 64:128], 1.0, 30000.0, ALU.subtract, ALU.mult)
                pb = ps[7].cast(BF16)
                for qs in range(4):
                    S.tr(pb[:, qs * 128:(qs + 1) * 128], selbs[qs], K["ident_b"])
                for qs in range(4):
                    for hh in range(4):
                        dst = V(Q.ap[64:128, 4 * g + hh, qs * 128:(qs + 1) * 128], [Qhi[sl][g]])
                        S.copy(dst, pb[64:128, qs * 128:(qs + 1) * 128], eng=("act" if act_off else "dve"))

            attend(t, h, 0, True, ktl, with_imp=True, after=after, act_off=act_off)

        def b2_head(t, h):
            g = h // 4
            ktl = []
            for m in (4, 0, 1, 2, 3, 5, 6, 7):
                kti = 4 * (t - 1) + m
                if kti < 0:
                    continue
                if m <= 3:
                    c0, c1 = 0, 128 * (m + 1)
                    mask = (K["tri"][:, 1, :], 128 * m, 128 * m + 128)
                else:
                    c0, c1 = 128 * (m - 4), 512
                    mask = (K["tri"][:, 0, :], c0, c0 + 128)
                ktl.append((kwT[:, g, kti * 128:(kti + 1) * 128], vwA[:, kti, g, :], 128, 128, c0, c1, mask, None))
            attend(t, h, 2, False, ktl)

        def b3_head(t, h):
            g = h // 4
            ktl = []
            for kt in range(4 * t + 4):
                if kt < 4 * t:
                    c0, c1, mask = 0, 512, None
                else:
                    c0, c1 = 128 * (kt - 4 * t), 512
                    mask = (K["tri"][:, 0, :], c0, c0 + 128)
                ktl.append((ksA[:, g, kt * 128:(kt + 1) * 128], vsA[:, kt, g, :], 128, 128, c0, c1, mask, None))
            attend(t, h, 1, False, ktl)

        def make_b4(t):
            sl = t % 2

            def b4mix():
                for qs in range(4):
                    S.tt(mix[qs][:, 512:1024], accs[t % 2][:, qs, :], sngb[sl][:, qs, :], ALU.mult)

            def b4a(qs):
                mx = mix[qs]
                pb = ps[7].cast(BF16)
                for k in range(8):
                    S.tr(pb[:, k * 128:(k + 1) * 128], mx[:, k * 128:(k + 1) * 128], K["ident_b"])
                S.copy(mixTs[qs].re("p k n -> p (k n)"), pb, eng="act")

            def b4b(qs):
                tt_ = 4 * t + qs
                xr, mT = xres[qs], mixTs[qs]
                zt_, ssb = zts[tt_ % 2], ss2s[tt_ % 2]
                for half in range(2):
                    zp = ps[5 + half]
                    for k in range(8):
                        S.mm(zp, mT[:, k, :], wout[:, k, half * 512:(half + 1) * 512], start=(k == 0), stop=(k == 7))
                    S.act(junk2[:, half * 512:(half + 1) * 512], zp, AF.Square, accum_out=ssb[:, half:half + 1])
                    S.tt(zt_[:, half * 512:(half + 1) * 512], zp, gGb[:, half * 512:(half + 1) * 512], ALU.mult)
                S.tt(ssb[:, 2:3], ssb[:, 0:1], ssb[:, 1:2], ALU.add)
                S.ts(ssb[:, 2:3], ssb[:, 2:3], 1.0 / D, 1e-6, ALU.mult, ALU.add)
                S.act(ssb[:, 2:3], ssb[:, 2:3], AF.Ln)
                S.act(ssb[:, 3:4], ssb[:, 2:3], AF.Exp, scale=-0.5)
                o_ = ot[tt_ % 2]
                S.stt(o_, zt_, ssb[:, 3:4], xr, ALU.mult, ALU.add)
                S.dma("sp", out_d[tt_ * 128:(tt_ + 1) * 128, :], o_, sembuf=o_.bufs[0], is_output=True)

            return b4mix, [lambda: b4a(0), lambda: b4a(1), lambda: b4a(2), lambda: b4a(3),
                           lambda: b4b(0), lambda: b4b(1), lambda: b4b(2), lambda: b4b(3)]

        load_inputs(0)
        for fn in late_dmas:
            fn()
        for h in range(8):
            b1_head(0, h, True)
        b4_prev = None
        for t in range(NS):
            for h in range(8):
                b2_head(t, h)
                if b4_prev is not None:
                    b4_prev[h]()
            if t + 1 < NS:
                load_inputs(t + 1)
            for qs in range(4):
                tt_ = 4 * t + qs
                S.dma("sp", mix[qs][:, 0:512], mret_d[tt_ * 128:(tt_ + 1) * 128, :])
                S.dma("sp", xres[qs], x_d[tt_ * 128:(tt_ + 1) * 128, :])
            for h in range(8):
                b3_head(t, h)
                if t + 1 < NS:
                    b1_head(t + 1, h, False)
            flush()
            b4mix, b4_prev = make_b4(t)
            b4mix()
        for st_ in b4_prev:
            st_()


def core_inputs(inp, consts, b):
    m = {
        "x": np.ascontiguousarray(inp["x"][b]),
        "c": np.ascontiguousarray(inp["c"][b]),
        "positions": np.ascontiguousarray(inp["positions"][b:b + 1]).astype(np.int32),
        "w_ada": np.ascontiguousarray(inp["w_ada"][0]),
        "b_ada": np.ascontiguousarray(inp["b_ada"][0]),
        "g_pre": np.ascontiguousarray(inp["g_pre"][0]),
        "g_post": np.ascontiguousarray(inp["g_post"][0]),
        "w_in": np.ascontiguousarray(inp["w_in"][0]),
        "w_out": np.ascontiguousarray(inp["w_out"][0]),
        "cmp_pe_k": np.ascontiguousarray(inp["cmp_pe_k"][0]).reshape(-1),
        "cmp_w1_k": np.ascontiguousarray(inp["cmp_w1_k"][0]),
        "cmp_w2_k": np.ascontiguousarray(inp["cmp_w2_k"][0]),
        "cmp_pe_v": np.ascontiguousarray(inp["cmp_pe_v"][0]).reshape(-1),
        "cmp_w1_v": np.ascontiguousarray(inp["cmp_w1_v"][0]),
        "cmp_w2_v": np.ascontiguousarray(inp["cmp_w2_v"][0]),
    }
    for k, v in consts.items():
        m["k_" + k] = v
    return m


_CACHE = {}


def kernel(**inputs):
    if "nc" not in _CACHE:
        _CACHE["nc"] = build()
    nc, consts = _CACHE["nc"]
    inp = {k: np.asarray(v) for k, v in inputs.items()}
    in_maps = [core_inputs(inp, consts, b) for b in range(8)]
    res = run_bass_kernel_spmd(nc, in_maps, core_ids=list(range(8)))
    out = np.stack([np.asarray(r["out"]) for r in res.results], axis=0)
    return out.astype(np.float32)
```

```python
import numpy as np
from contextlib import ExitStack
import ml_dtypes
import concourse.bass as bass
import concourse.mybir as mybir
from concourse.bass_utils import run_bass_kernel_spmd

F32 = mybir.dt.float32
BF16 = mybir.dt.bfloat16
I32 = mybir.dt.int32
AF = mybir.ActivationFunctionType
ALU = mybir.AluOpType
AX = mybir.AxisListType

S_LEN = 4096
D = 1024
NT = 32
NS = 8
PW = 3864
PI = float(np.pi)
DBG_T = 0


class Buf:
    __slots__ = ("w", "r", "sem", "semcnt", "name", "uid", "excl", "dram")
    _n = [0]

    def __init__(self, name=""):
        Buf._n[0] += 1
        self.uid = Buf._n[0]
        self.w = None
        self.r = {}
        self.sem = None
        self.semcnt = 0
        self.name = name
        self.excl = False
        self.dram = False


class V:
    __slots__ = ("ap", "bufs")

    def __init__(self, ap, bufs):
        self.ap = ap
        self.bufs = bufs if isinstance(bufs, (list, tuple)) else [bufs]

    def __getitem__(self, k):
        return V(self.ap[k], self.bufs)

    def re(self, s, **kw):
        return V(self.ap.rearrange(s, **kw), self.bufs)

    def bc(self, shape):
        return V(self.ap.to_broadcast(list(shape)), self.bufs)

    def cast(self, dt):
        return V(self.ap.bitcast(dt), self.bufs)

    def sub(self, bufs):
        return V(self.ap, bufs)


COMPUTE = ("pe", "act", "dve", "pool")


class Sched:
    def __init__(self, nc, stack):
        self.nc = nc
        self.stack = stack
        self.streams = {e: [] for e in ("pe", "act", "dve", "pool", "sp")}
        self.cnt = {e: 0 for e in COMPUTE}
        self.waited = {e: {} for e in self.streams}
        self.sems = {e: stack.enter_context(nc.semaphore("s_" + e)) for e in COMPUTE}
        self.dma_sems = {}
        self.dma_bufs = []
        self.needed = {e: set() for e in COMPUTE}
        self.out_toks = []

    def _deps(self, eng, reads, writes):
        deps = {}

        def add(tok):
            k, i = tok
            if deps.get(k, -1) < i:
                deps[k] = i

        for b in reads:
            if b.excl:
                for k, i in b.r.items():
                    if k != eng:
                        add((k, i))
            if b.w is not None:
                if b.w[0] == eng and eng == "pe":
                    continue
                add(b.w)
        for b in writes:
            if b.w is not None and (b.w[0] != eng or eng != "pe"):
                add(b.w)
            for k, i in b.r.items():
                if k != eng or eng != "pe":
                    add((k, i))
        return self._emit_waits(eng, deps)

    def _emit_waits(self, eng, deps):
        waits = []
        wd = self.waited[eng]
        for k, i in deps.items():
            if wd.get(k, -1) < i:
                wd[k] = i
                waits.append((k, i))
                if k in COMPUTE:
                    self.needed[k].add(i)
        return waits

    def _mark(self, tok, reads, writes):
        k, i = tok
        for b in reads:
            if b.r.get(k, -1) < i:
                b.r[k] = i
        for b in writes:
            b.w = tok
            b.r = {}

    def op(self, eng, fn, reads, writes):
        rb = [b for v in reads if isinstance(v, V) for b in v.bufs]
        wb = [b for v in writes if isinstance(v, V) for b in v.bufs]
        waits = self._deps(eng, rb, wb)
        self.cnt[eng] += 1
        idx = self.cnt[eng]
        self.streams[eng].append(("c", waits, fn, idx))
        self._mark((eng, idx), rb, wb)

    def dma(self, q, out, in_, sembuf=None, is_output=False, **kw):
        rb = list(in_.bufs)
        wb = list(out.bufs)
        sb = sembuf or wb[0]
        if sb.sem is None:
            sb.sem = self.stack.enter_context(self.nc.semaphore("d_%d" % sb.uid))
            self.dma_sems[sb.uid] = sb.sem
            self.dma_bufs.append(sb)
        waits = self._deps(q, rb, [b for b in wb if not b.dram])
        sb.semcnt += 16
        tok = (("dma", sb.uid), sb.semcnt)
        self.streams[q].append(("d", waits, (out.ap, in_.ap, kw), sb.sem))
        self._mark(tok, rb, wb)
        if is_output:
            self.out_toks.append(tok)
        return tok

    def barrier(self):
        deps = {}
        for e in COMPUTE:
            if self.cnt[e] > 0:
                deps[e] = self.cnt[e]
        for b in self.dma_bufs:
            deps[("dma", b.uid)] = b.semcnt
        for e in self.streams:
            w = self._emit_waits(e, dict(deps))
            if w:
                self.streams[e].append(("w", w, None, None))

    def emit(self, block):
        fin = self._emit_waits("sp", {k: i for k, i in self.out_toks})
        self.streams["sp"].append(("w", fin, None, None))
        pref = {}
        for e in COMPUTE:
            m = {}
            c = 0
            for i in range(1, self.cnt[e] + 1):
                if i in self.needed[e]:
                    c += 1
                    m[i] = c
            pref[e] = m
        names = {"pe": "tensor", "act": "scalar", "dve": "vector", "pool": "gpsimd", "sp": "sync"}
        for e, st in self.streams.items():
            def body(eng, e=e, st=st):
                for kind, waits, fn, extra in st:
                    for k, i in waits:
                        if k in COMPUTE:
                            eng.wait_ge(self.sems[k], pref[k][i])
                        else:
                            eng.wait_ge(self.dma_sems[k[1]], i)
                    if kind == "c":
                        ins = fn(eng)
                        if extra in self.needed[e]:
                            ins.then_inc(self.sems[e], 1)
                    elif kind == "d":
                        oap, iap, kw = fn
                        eng.dma_start(out=oap, in_=iap, **kw).then_inc(extra, 16)
            getattr(block, names[e])(body)

    def mm(self, out, lhsT, rhs, start=True, stop=True, **kw):
        self.op("pe", lambda e: e.matmul(out.ap, lhsT.ap, rhs.ap, start=start, stop=stop, **kw),
                [lhsT, rhs], [out])

    def tr(self, out, in_, ident):
        self.op("pe", lambda e: e.transpose(out.ap, in_.ap, ident.ap), [in_, ident], [out])

    def act(self, out, in_, func, bias=None, scale=None, accum_out=None):
        kw = {}
        rd = [in_]
        if bias is not None:
            kw["bias"] = bias.ap if isinstance(bias, V) else bias
            rd.append(bias)
        if scale is not None:
            kw["scale"] = scale.ap if isinstance(scale, V) else scale
            rd.append(scale)
        wr = [out]
        if accum_out is not None:
            kw["accum_out"] = accum_out.ap
            wr.append(accum_out)
        self.op("act", lambda e: e.activation(out.ap, in_.ap, func, **kw), rd, wr)

    def tt(self, out, in0, in1, op, eng="dve"):
        self.op(eng, lambda e: e.tensor_tensor(out.ap, in0.ap, in1.ap, op), [in0, in1], [out])

    def ts(self, out, in0, s1, s2, op0, op1=None, eng="dve"):
        a1 = s1.ap if isinstance(s1, V) else s1
        a2 = s2.ap if isinstance(s2, V) else s2
        kw = {}
        if op1 is not None:
            kw["op1"] = op1
        self.op(eng, lambda e: e.tensor_scalar(out.ap, in0.ap, a1, a2, op0, **kw), [in0, s1, s2], [out])

    def stt(self, out, in0, scalar, in1, op0, op1, eng="dve"):
        a = scalar.ap if isinstance(scalar, V) else scalar
        self.op(eng, lambda e: e.scalar_tensor_tensor(out.ap, in0.ap, a, in1.ap, op0, op1),
                [in0, scalar, in1], [out])

    def copy(self, out, in_, eng="dve"):
        if eng == "act":
            self.op("act", lambda e: e.copy(out.ap, in_.ap), [in_], [out])
        else:
            self.op(eng, lambda e: e.tensor_copy(out.ap, in_.ap), [in_], [out])

    def memset(self, out, val, eng="dve"):
        self.op(eng, lambda e: e.memset(out.ap, val), [], [out])

    def reduce(self, out, in_, op, eng="dve"):
        self.op(eng, lambda e: e.tensor_reduce(out.ap, in_.ap, AX.X, op), [in_], [out])

    def recip(self, out, in_):
        self.op("dve", lambda e: e.reciprocal(out.ap, in_.ap), [in_], [out])

    def max8(self, out, in_):
        self.op("dve", lambda e: e.max(out.ap, in_.ap), [in_], [out])

    def match_replace(self, out, to_replace, values, imm):
        self.op("dve", lambda e: e.match_replace(out.ap, to_replace.ap, values.ap, imm),
                [to_replace, values], [out])


def make_consts():
    bf = ml_dtypes.bfloat16
    c = {}
    c["ident_f"] = np.eye(128, dtype=np.float32)
    c["ident_b"] = np.eye(128, dtype=np.float32).astype(bf)
    c["ones_f"] = np.ones((128, 128), np.float32)
    permR = np.zeros((128, 128), np.float32)
    permN = np.zeros((128, 128), np.float32)
    invR = np.zeros((128, 1), np.float32)
    invN = np.zeros((128, 1), np.float32)
    ir = np.power(np.float32(10000.0), -np.arange(32, dtype=np.float32) / np.float32(32))
    inn = np.power(np.float32(500000.0), -np.arange(8, dtype=np.float32) / np.float32(8))
    for m in range(128):
        blk, d = (m // 64) * 64, m % 64
        if d < 32:
            permR[blk + d + 32, m] = -1.0
        else:
            permR[blk + d - 32, m] = 1.0
        invR[m, 0] = ir[d % 32]
        if d < 8:
            permN[blk + d + 8, m] = -1.0
            invN[m, 0] = inn[d]
        elif d < 16:
            permN[blk + d - 8, m] = 1.0
            invN[m, 0] = inn[d - 8]
    c["permR"] = permR.astype(bf)
    c["permN"] = permN.astype(bf)
    c["inv2"] = np.concatenate([invR, invN], axis=1).astype(np.float64) / (2 * np.pi)
    c["inv2"] = c["inv2"].astype(np.float32)
    H = 8
    log_g = np.log1p(-np.power(2.0, -5.0 - np.arange(H, dtype=np.float64)))
    idx = np.arange(128, dtype=np.float64)
    dec = np.zeros((128, H, 128), np.float32)
    for h in range(H):
        diff = idx[None, :] - idx[:, None]
        dec[:, h, :] = np.where(diff >= 0, np.exp(np.maximum(diff, 0) * log_g[h]), 0.0)
    c["decayT"] = dec
    c["xi"] = np.exp((idx[:, None] + 1.0) * log_g[None, :]).astype(np.float32)
    c["zeta"] = np.exp((127.0 - idx[:, None]) * log_g[None, :]).astype(np.float32)
    cd = np.exp(128.0 * log_g)
    cdv = np.zeros((128, 4), np.float32)
    for m in range(128):
        for p in range(4):
            cdv[m, p] = cd[2 * p + m // 64]
    c["cdv"] = cdv
    keys = np.arange(S_LEN)
    c["onehot"] = (keys[None, :] // 64 == np.arange(64)[:, None]).astype(np.float32).astype(bf)
    kk = np.arange(128)[:, None]
    qq = np.arange(128)[None, :]
    c["tri"] = (-30000.0 * (1.0 - np.stack([(kk <= qq), (kk > qq)], axis=1).astype(np.float32))).astype(bf)
    cm = np.zeros((NS, 2, 128, 512), np.float32)
    for t in range(NS):
        for nt in range(2):
            n = np.arange(128)[:, None] + 128 * nt
            q = 512 * t + np.arange(512)[None, :]
            cm[t, nt] = ((16 * n + 31 <= q) & (n < 255))
    c["cmpmask"] = (-30000.0 * (1.0 - cm)).astype(bf)
    fb = np.zeros((NT, 128, 64), np.float32)
    for qt in range(NT):
        for ql in range(128):
            cur = (qt * 128 + ql) // 64
            fb[qt, ql, :] = np.where(np.arange(64) > cur, -1e30, 0.0)
            fb[qt, ql, 0] = 1e9
            if cur - 1 >= 0:
                fb[qt, ql, cur - 1] = 3e9
            fb[qt, ql, cur] = 2e9
    c["fb"] = fb
    Nc = 255
    cs = np.arange(Nc) * 16
    ce = cs + 31
    ss = np.arange(64) * 64
    ov = ((cs[:, None] <= ss[None, :] + 63) & (ce[:, None] >= ss[None, :])).astype(np.float32)
    ova = np.zeros((256, 65), np.float32)
    ova[:Nc, :64] = ov
    ova[:Nc, 64] = 1.0
    c["ovl"] = ova.astype(bf)
    return c


CONST_SPECS = None


def build(debug=()):
    nc = bass.Bass("TRN2", target_bir_lowering=False)
    consts = make_consts()
    dts = {np.dtype(np.float32): F32, np.dtype(ml_dtypes.bfloat16): BF16}

    def dbuf(name):
        b = Buf(name)
        b.dram = True
        return b

    def din(name, shape, dt):
        return V(nc.dram_tensor(name, list(shape), dt, kind="ExternalInput").ap(), dbuf(name))

    x_d = din("x", [S_LEN, D], F32)
    c_d = din("c", [D], F32)
    pos_d = din("positions", [1, S_LEN], I32)
    wada_d = din("w_ada", [D, 3 * D], F32)
    bada_d = din("b_ada", [3 * D], F32)
    gpre_d = din("g_pre", [D], F32)
    gpost_d = din("g_post", [D], F32)
    win_d = din("w_in", [D, PW], F32)
    wout_d = din("w_out", [D, D], F32)
    pek_d = din("cmp_pe_k", [2048], F32)
    w1k_d = din("cmp_w1_k", [2048, 256], F32)
    w2k_d = din("cmp_w2_k", [256, 64], F32)
    pev_d = din("cmp_pe_v", [2048], F32)
    w1v_d = din("cmp_w1_v", [2048, 256], F32)
    w2v_d = din("cmp_w2_v", [256, 64], F32)
    cd_ = {k: din("k_" + k, v.shape, dts[v.dtype]) for k, v in consts.items()}
    out_d = V(nc.dram_tensor("out", [S_LEN, D], F32, kind="ExternalOutput").ap(), dbuf("out"))
    dbg = {}

    def dbg_out(name, shape, dt=F32):
        dbg[name] = V(nc.dram_tensor("dbg_" + name, list(shape), dt, kind="ExternalOutput").ap(), Buf(name))
        return dbg[name]

    def scratch(name, shape, dt):
        return V(nc.dram_tensor(name, list(shape), dt).ap(), dbuf(name))

    tabs_d = scratch("tabs_s", [4, 128, S_LEN], F32)
    qT_d = scratch("qT_s", [8, 64, S_LEN], BF16)
    sng_d = scratch("sng_s", [S_LEN, 512], BF16)
    gates_d = scratch("gates_s", [S_LEN, 24], F32)
    mret_d = scratch("mret_s", [S_LEN, 512], BF16)
    ks_d = scratch("ks_s", [64, 2, S_LEN], BF16)
    kw_d = scratch("kw_s", [64, 2, S_LEN], BF16)
    vs_d = scratch("vs_s", [S_LEN, 2, 64], BF16)
    vw_d = scratch("vw_s", [S_LEN, 2, 64], BF16)

    with ExitStack() as st0:
        S = Sched(nc, st0)

        def alloc(st, name, shape, dt):
            t = st.enter_context(nc.sbuf_tensor(name, list(shape), dt))
            return V(t[:], Buf(name))

        ps = []
        for i in range(8):
            t = st0.enter_context(nc.psum_tensor("ps%d" % i, [128, 512], F32))
            ps.append(V(t[:], Buf("ps%d" % i)))
            ps[-1].bufs[0].excl = True

        K = {}
        deferred = []
        for name in ("ident_f", "ident_b", "ones_f", "permR", "permN", "inv2", "decayT", "xi", "zeta", "cdv",
                     "tri", "ovl"):
            v = consts[name]
            if name == "ovl":
                K[name] = alloc(st0, "c_" + name, [128, 2, 65], BF16)
                deferred.append(lambda name=name: S.dma("sp", K[name], cd_[name].re("(t p) c -> p t c", p=128)))
            else:
                K[name] = alloc(st0, "c_" + name, v.shape, dts[v.dtype])
                deferred.append(lambda name=name: S.dma("sp", K[name], cd_[name]))
        kvcT = alloc(st0, "kvcT", [64, 2, 2, 256], BF16)
        S.memset(kvcT, 0.0)
        ctab = alloc(st0, "ctab", [64, 2, 256], F32)
        Gs = alloc(st0, "Gs", [128, 8], F32)
        shf = alloc(st0, "shf", [128, 8], F32)
        gGb = alloc(st0, "gGb", [128, D], F32)
        b1 = alloc(st0, "b1", [128, 2, 2], F32)

        with ExitStack() as stA:
            win = alloc(stA, "win", [128, 8, PW], BF16)
            for k in range(8):
                deferred.append(lambda k=k: S.dma("pool", win[:, k, :], win_d[k * 128:(k + 1) * 128, :]))
            w1 = alloc(stA, "w1", [128, 2, 16, 256], BF16)
            w2 = alloc(stA, "w2", [128, 2, 2, 64], BF16)
            wkc = alloc(stA, "wkc", [128, 8, 4, 128], BF16)

            with ExitStack() as stP:
                wadaf = [alloc(stP, "wadaf%d" % i, [128, 3 * D], F32) for i in range(2)]
                def rot_tables_multi(posf, n, jobs, tmps):
                    for (inv_col, phase, outv), (u, ki, kf) in zip(jobs, tmps):
                        if phase == 0.0:
                            S.ts(u[:, 0:n], posf, inv_col, None, ALU.mult)
                        else:
                            S.ts(u[:, 0:n], posf, inv_col, phase, ALU.mult, ALU.add)
                    for (inv_col, phase, outv), (u, ki, kf) in zip(jobs, tmps):
                        S.copy(ki[:, 0:n], u[:, 0:n])
                    for (inv_col, phase, outv), (u, ki, kf) in zip(jobs, tmps):
                        S.copy(kf[:, 0:n], ki[:, 0:n])
                    for (inv_col, phase, outv), (u, ki, kf) in zip(jobs, tmps):
                        S.tt(u[:, 0:n], u[:, 0:n], kf[:, 0:n], ALU.subtract)
                    for (inv_col, phase, outv), (u, ki, kf) in zip(jobs, tmps):
                        S.act(outv, u[:, 0:n], AF.Sin, scale=2 * PI)

                posi_all = alloc(stP, "posi_all", [128, S_LEN], I32)
                S.dma("sp", posi_all, V(pos_d.ap[0:1, :].partition_broadcast(128), pos_d.bufs))
                for fn in deferred:
                    fn()
                S.dma("pool", w1[:, 0], w1k_d.re("(j p) h -> p j h", p=128))
                S.dma("pool", w1[:, 1], w1v_d.re("(j p) h -> p j h", p=128))
                S.dma("pool", w2[:, 0], w2k_d.re("(c p) d -> p c d", p=128))
                S.dma("pool", w2[:, 1], w2v_d.re("(c p) d -> p c d", p=128))
                posi = [alloc(stP, "posi%d" % i, [128, 512], I32) for i in range(2)]
                posf = [alloc(stP, "posf%d" % i, [128, 512], F32) for i in range(2)]
                tmps = [(alloc(stP, "tu%d" % i, [128, 512], F32), alloc(stP, "tki%d" % i, [128, 512], I32),
                         alloc(stP, "tkf%d" % i, [128, 512], F32)) for i in range(4)]
                tout2 = [[alloc(stP, "tout%d_%d" % (i, j), [128, 512], F32) for i in range(4)] for j in range(2)]
                for ch in range(NS):
                    tout = tout2[ch % 2]
                    sl = slice(ch * 512, (ch + 1) * 512)
                    pi_, pf_ = posi[ch % 2], posf[ch % 2]
                    S.copy(pf_, posi_all[:, sl])
                    jobs = [(K["inv2"][:, 0:1], 0.25, tout[0]), (K["inv2"][:, 0:1], 0.0, tout[1]),
                            (K["inv2"][:, 1:2], 0.25, tout[2]), (K["inv2"][:, 1:2], 0.0, tout[3])]
                    rot_tables_multi(pf_, 512, jobs, tmps)
                    for i in range(4):
                        S.dma("act", tabs_d[i][:, sl], tout[i], sembuf=tout[i].bufs[0])
                S.memset(posi[0][0:64, 0:256], 0)
                S.dma("sp", posi[0][0:64, 0:255],
                      V(pos_d.ap[0:1, 31:4096:16].partition_broadcast(64), pos_d.bufs),
                      allow_slow_non_contiguous=True)
                S.copy(posf[0][0:64, 0:256], posi[0][0:64, 0:256])
                jobs = [(K["inv2"][0:64, 1:2], 0.25, ctab[:, 0, :]), (K["inv2"][0:64, 1:2], 0.0, ctab[:, 1, :])]
                rot_tables_multi(posf[0][0:64, 0:256], 256, jobs,
                                 [tuple(x[0:64] for x in tmps[0]), tuple(x[0:64] for x in tmps[1])])
                cs_ = alloc(stP, "cs", [128, 8], F32)
                S.dma("sp", cs_, c_d.re("(k p) -> p k", p=128), allow_slow_non_contiguous=True)
                csb = alloc(stP, "csb", [128, 8], F32)
                S.act(csb, cs_, AF.Silu)
                badaT = alloc(stP, "badaT", [128, 24], F32)
                S.dma("sp", badaT, bada_d.re("(k p) -> p k", p=128), allow_slow_non_contiguous=True)
                gpp = alloc(stP, "gpp", [128, 2, 8], F32)
                S.dma("sp", gpp[:, 0], gpre_d.re("(k p) -> p k", p=128), allow_slow_non_contiguous=True)
                S.dma("sp", gpp[:, 1], gpost_d.re("(k p) -> p k", p=128), allow_slow_non_contiguous=True)
                S.memset(ps[0][:, 0:24], 0.0)
                for k in range(8):
                    wf = wadaf[k % 2]
                    S.dma("sp", wf, wada_d[k * 128:(k + 1) * 128, :])
                    for jc in range(24):
                        S.mm(ps[0][:, jc:jc + 1], wf[:, jc * 128:(jc + 1) * 128], csb[:, k:k + 1],
                             start=False, stop=(k == 7), skip_group_check=True)
                mod = alloc(stP, "mod", [128, 24], F32)
                S.tt(mod, ps[0][:, 0:24], badaT, ALU.add)
                S.copy(shf, mod[:, 0:8])
                S.stt(Gs, mod[:, 8:16], 1.0, gpp[:, 0], ALU.add, ALU.mult)
                gG = alloc(stP, "gG", [128, 8], F32)
                S.tt(gG, mod[:, 16:24], gpp[:, 1], ALU.mult)
                dg = alloc(stP, "dg", [128, 128], F32)
                for k in range(8):
                    S.ts(dg, K["ident_f"], gG[:, k:k + 1], None, ALU.mult)
                    S.mm(ps[1 + k // 4][:, (k % 4) * 128:(k % 4 + 1) * 128], K["ones_f"], dg)
                S.copy(gGb[:, 0:512], ps[1])
                S.copy(gGb[:, 512:1024], ps[2])
                pef = alloc(stP, "pef", [128, 2, 16], F32)
                S.dma("sp", pef[:, 0], pek_d.re("(j p) -> p j", p=128), allow_slow_non_contiguous=True)
                S.dma("sp", pef[:, 1], pev_d.re("(j p) -> p j", p=128), allow_slow_non_contiguous=True)
                peb = alloc(stP, "peb", [128, 2, 16], BF16)
                S.copy(peb, pef)
                for kv in range(2):
                    for hc in range(2):
                        for j in range(16):
                            S.mm(ps[3][:, kv * 2 + hc:kv * 2 + hc + 1], w1[:, kv, j, hc * 128:(hc + 1) * 128],
                                 peb[:, kv, j:j + 1], start=(j == 0), stop=(j == 15))
                S.copy(b1.re("p a b -> p (a b)"), ps[3][:, 0:4])

            for i4 in range(4):
                c0 = 2560 + 64 * i4
                S.copy(wkc[:, :, i4, 0:64], win[:, :, c0:c0 + 64], eng="pool")
                S.copy(wkc[:, :, i4, 64:128], win[:, :, c0:c0 + 64], eng="pool")
            S.barrier()

            with ExitStack() as stW:
                xb_ = [alloc(stW, "xt%d" % i, [128, D], F32) for i in range(2)]
                junk = alloc(stW, "junk", [128, D], BF16)
                ssq = alloc(stW, "ssq", [128, 1], F32)
                rstd = alloc(stW, "rstd", [128, 1], F32)
                xn = [alloc(stW, "xn", [128, D], BF16)] * 2
                hTs = [alloc(stW, "hT%d" % i, [128, 8, 512], BF16) for i in range(2)]
                tab = alloc(stW, "tab", [128, 4, 512], F32)
                rqs = [alloc(stW, "rq%d" % i, [128, 4, 512], BF16) for i in range(2)]
                rks = [alloc(stW, "rk%d" % i, [128, 4, 512], BF16) for i in range(2)]
                xbs = [alloc(stW, "xbs%d" % i, [128, 512], BF16) for i in range(2)]
                t1s = [alloc(stW, "t1", [128, 512], F32)] * 2
                t2s = [alloc(stW, "t2", [128, 512], F32)] * 2
                qn = [alloc(stW, "qn%d" % i, [128, 512], BF16) for i in range(2)]
                kst = [alloc(stW, "kst%d" % i, [64, 512], BF16) for i in range(2)]
                vrets = [alloc(stW, "vret%d" % i, [128, 4, 512], BF16) for i in range(2)]
                sgs = [alloc(stW, "sg%d" % i, [128, 4, 512], BF16) for i in range(2)]
                sng = [alloc(stW, "sng%d" % i, [128, 512], BF16) for i in range(2)]
                vst = [alloc(stW, "vst%d" % i, [128, 2, 2, 64], BF16) for i in range(2)]
                gst = [alloc(stW, "gst%d" % i, [128, 24], F32) for i in range(2)]
                KC = [alloc(stW, "KC%d" % i, [128, 2, 2, 528], BF16) for i in range(2)]
                hid = alloc(stW, "hid", [128, 2, 2, 64], BF16)
                kcx = alloc(stW, "kcx", [64, 64], BF16)
                scbs = [alloc(stW, "scb%d" % i, [128, 8, 128], BF16) for i in range(2)]
                kzs = [alloc(stW, "kz%d" % i, [128, 512], BF16) for i in range(2)]
                R32 = alloc(stW, "R32", [128, 4, 128], F32)
                Rb = alloc(stW, "Rb", [128, 4, 128], BF16)
                o1 = alloc(stW, "o1", [128, 8, 64], F32)
                o2 = alloc(stW, "o2", [128, 8, 64], F32)
                st8 = alloc(stW, "st8", [128, 4, 8], F32)
                mst = [alloc(stW, "mst%d" % i, [128, 512], BF16) for i in range(2)]
                S.memset(R32, 0.0)
                S.memset(Rb, 0.0)
                for i in range(2):
                    S.memset(KC[i], 0.0)
                print("passA sbuf_base", nc.sbuf_base, "top", nc.sbuf_top)

                rotc = [0]
                rot_pending = []

                def rotary(src_ps, npart, perm, cosv, sinv, dest, scale, then=None):
                    slot = rotc[0] % 2
                    rotc[0] += 1
                    xb = xbs[slot][0:npart]
                    S.act(xb, src_ps, AF.Copy, scale=scale)

                    def fin():
                        t1, t2 = t1s[slot], t2s[slot]
                        S.mm(ps[7][0:npart, :], perm, xb)
                        S.tt(t1[0:npart], ps[7][0:npart, :], sinv, ALU.mult)
                        S.tt(t2[0:npart], xb, cosv, ALU.mult, eng="pool")
                        S.tt(dest, t1[0:npart], t2[0:npart], ALU.add)
                        if then is not None:
                            then()

                    while rot_pending:
                        rot_pending.pop(0)()
                    rot_pending.append(fin)

                def rot_flush():
                    while rot_pending:
                        rot_pending.pop(0)()

                pscyc = [0]

                def next_ps():
                    i = pscyc[0] % 3
                    pscyc[0] += 1
                    return ps[i]

                def genA1(t):
                    hT = hTs[t % 2]
                    for j in range(4):
                        tt_ = 4 * t + j
                        xt = xb_[tt_ % 2]
                        S.dma("pool", xt, x_d[tt_ * 128:(tt_ + 1) * 128, :])
                        S.act(junk, xt, AF.Square, accum_out=ssq)
                        S.ts(rstd, ssq, 1.0 / D, 1e-6, ALU.mult, ALU.add)
                        S.act(rstd, rstd, AF.Sqrt)
                        S.recip(rstd, rstd)
                        xnb = xn[tt_ % 2]
                        S.act(xnb, xt, AF.Copy, scale=rstd)
                        yield
                        pb = ps[7].cast(BF16)
                        for k in range(8):
                            S.tr(pb[:, k * 128:(k + 1) * 128], xnb[:, k * 128:(k + 1) * 128], K["ident_b"])
                        for k in range(8):
                            S.act(hT[:, k, j * 128:(j + 1) * 128], pb[:, k * 128:(k + 1) * 128], AF.Identity,
                                  scale=Gs[:, k:k + 1], bias=shf[:, k:k + 1])
                        yield

                def genA2(t):
                    T0 = 512 * t
                    hT, rq, rk, vret, sg = hTs[t % 2], rqs[t % 2], rks[t % 2], vrets[t % 2], sgs[t % 2]
                    S.dma("sp", tab, tabs_d.re("a p n -> p a n")[:, :, T0:T0 + 512])

                    def proj_fm(wsel, M):
                        p_ = next_ps()
                        for k in range(8):
                            S.mm(p_[0:M, :], wsel(k), hT[:, k, :], start=(k == 0), stop=(k == 7))
                        return p_

                    for p in range(4):
                        pq = proj_fm(lambda k, p=p: win[:, k, 128 * p:128 * (p + 1)], 128)
                        rotary(pq, 128, K["permR"], tab[:, 0, :], tab[:, 1, :], rq[:, p, :], 0.125)
                        yield
                        pk = proj_fm(lambda k, p=p: win[:, k, 512 + 128 * p:512 + 128 * (p + 1)], 128)
                        rotary(pk, 128, K["permR"], tab[:, 0, :], tab[:, 1, :], rk[:, p, :], 1.0)
                        yield
                    for p in range(4):
                        pq = proj_fm(lambda k, p=p: win[:, k, 2048 + 128 * p:2048 + 128 * (p + 1)], 128)
                        qb = qn[p % 2]

                        def st_q(p=p, qb=qb):
                            S.dma("sp", qT_d[2 * p:2 * p + 2].re("h d n -> (h d) n")[:, T0:T0 + 512], qb,
                                  sembuf=qb.bufs[0])
                        rotary(pq, 128, K["permN"], tab[:, 2, :], tab[:, 3, :], qb, 0.125, then=st_q)
                        yield
                    for i4, (c0, dst) in enumerate(((2816, ks_d), (2880, ks_d), (3072, kw_d), (3136, kw_d))):
                        g = i4 % 2
                        pk = proj_fm(lambda k, c0=c0: win[:, k, c0:c0 + 64], 64)
                        kb = kst[i4 % 2]

                        def st_k(dst=dst, g=g, kb=kb):
                            S.dma("sp", dst[:, g, T0:T0 + 512], kb, sembuf=kb.bufs[0])
                        rotary(pk[0:64, :], 64, K["permN"][0:64, 0:64], tab[0:64, 2, :], tab[0:64, 3, :], kb, 1.0,
                               then=st_k)
                        yield
                    KCc, KCp = KC[t % 2], KC[(t + 1) % 2]
                    for kv in range(2):
                        for g in range(2):
                            pk = proj_fm(lambda k, i4=kv * 2 + g: wkc[:, k, i4, :], 128)
                            S.copy(KCc[0:64, kv, g, 16:528], pk[0:64, :], eng="act")
                            S.copy(KCc[64:128, kv, g, 15:527], pk[64:128, :], eng="act")
                            if kv == 0 and g == 0:
                                rot_flush()
                            yield
                    if t > 0:
                        S.copy(KCc[0:64, :, :, 0:16], KCp[0:64, :, :, 512:528], eng="pool")
                        S.copy(KCc[64:128, :, :, 0:15], KCp[64:128, :, :, 512:527], eng="pool")

                    for j in range(4):
                        tt_ = 4 * t + j
                        lhs = lambda k: hT[:, k, j * 128:(j + 1) * 128]
                        for gi, (c0, n) in enumerate(((1024, 512), (1536, 512), (3352, 512), (2944, 408))):
                            p_ = next_ps()
                            for k in range(8):
                                S.mm(p_[:, 0:n], lhs(k), win[:, k, c0:c0 + n], start=(k == 0), stop=(k == 7))
                            if gi == 0:
                                S.copy(vret[:, j, :], p_, eng="act")
                            elif gi == 1:
                                S.act(sg[:, j, :], p_, AF.Silu)
                            elif gi == 2:
                                sb_ = sng[tt_ % 2]
                                S.act(sb_, p_, AF.Silu)
                                S.dma("sp", sng_d[tt_ * 128:(tt_ + 1) * 128, :], sb_, sembuf=sb_.bufs[0])
                            else:
                                vb = vst[tt_ % 2]
                                S.copy(vb[:, 0].re("p g d -> p (g d)"), p_[:, 0:128])
                                S.copy(vb[:, 1].re("p g d -> p (g d)"), p_[:, 256:384])
                                S.dma("sp", vs_d[tt_ * 128:(tt_ + 1) * 128], vb[:, 0], sembuf=vb.bufs[0])
                                S.dma("sp", vw_d[tt_ * 128:(tt_ + 1) * 128], vb[:, 1], sembuf=vb.bufs[0])
                                gb = gst[tt_ % 2]
                                S.act(gb, p_[:, 384:408], AF.Sigmoid)
                                S.dma("sp", gates_d[tt_ * 128:(tt_ + 1) * 128, :], gb, sembuf=gb.bufs[0])
                            yield

                    for kv in range(2):
                        for hc in range(2):
                            p_ = next_ps()
                            po = p_[:, 0:64].re("p (g r) -> p g r", g=2)
                            for j in range(16):
                                S.mm(po, w1[:, kv, j, hc * 128:(hc + 1) * 128],
                                     KCc[:, kv, :, 2 * j:2 * j + 16 * 31 + 1:16], start=(j == 0), stop=(j == 15))
                            S.act(hid[:, kv, hc, :], p_[:, 0:64], AF.Silu, bias=b1[:, kv, hc:hc + 1])
                            yield
                        p_ = next_ps()
                        for hc in range(2):
                            S.mm(p_[0:64, 0:64], w2[:, kv, hc, :], hid[:, kv, hc, :], start=(hc == 0), stop=(hc == 1))
                        r0 = 1 if t == 0 else 0
                        n0 = 32 * t - 1
                        for g in range(2):
                            dst = kvcT[:, kv, g, n0 + r0:n0 + 32]
                            src = p_[0:64, g * 32 + r0:g * 32 + 32]
                            if kv == 1:
                                S.copy(dst, src)
                            else:
                                S.copy(kcx[:, g * 32:(g + 1) * 32], p_[0:64, g * 32:(g + 1) * 32], eng="act")
                        if kv == 0:
                            yield
                            t1, t2 = t1s[0], t2s[0]
                            S.mm(ps[7][0:64, 0:64], K["permN"][0:64, 0:64], kcx)
                            for g in range(2):
                                cs0 = ctab[:, 0, n0 + r0:n0 + 32]
                                sn0 = ctab[:, 1, n0 + r0:n0 + 32]
                                S.tt(t1[0:64, 0:32 - r0], ps[7][0:64, g * 32 + r0:g * 32 + 32], sn0, ALU.mult)
                                S.tt(t2[0:64, 0:32 - r0], kcx[:, g * 32 + r0:g * 32 + 32], cs0, ALU.mult)
                                S.tt(kvcT[:, 0, g, n0 + r0:n0 + 32], t1[0:64, 0:32 - r0], t2[0:64, 0:32 - r0],
                                     ALU.add)
                        yield

                def genA(t):
                    a1 = genA1(t + 1) if t + 1 < NS else None
                    for i, _ in enumerate(genA2(t)):
                        yield
                        if a1 is not None and i % 4 == 3:
                            try:
                                next(a1)
                                yield
                            except StopIteration:
                                a1 = None
                    if a1 is not None:
                        for _ in a1:
                            yield

                def genR(t):
                    hT, rq, rk, vret, sg = hTs[t % 2], rqs[t % 2], rks[t % 2], vrets[t % 2], sgs[t % 2]
                    zb = V(K["zeta"].ap.unsqueeze(2).to_broadcast([128, 8, 64]), K["zeta"].bufs)

                    def stage1(j):
                        cs = slice(j * 128, (j + 1) * 128)
                        scb, kz = scbs[j % 2], kzs[j % 2]
                        for p in range(4):
                            for hh in range(2):
                                rows = slice(hh * 64, hh * 64 + 64)
                                S.mm(ps[4 + hh][:, p * 128:(p + 1) * 128], rk[rows, p, cs], rq[rows, p, cs])
                        for hh in range(2):
                            S.tt(scb[:, hh::2, :], ps[4 + hh].re("p (h i) -> p h i", h=4),
                                 K["decayT"][:, hh::2, :], ALU.mult)
                        pb = ps[6].cast(BF16)
                        for p in range(4):
                            S.tr(pb[:, p * 128:(p + 1) * 128], rk[:, p, cs], K["ident_b"])
                        S.tt(kz.re("p (h d) -> p h d", h=8), pb[:, 0:512].re("p (h d) -> p h d", h=8), zb, ALU.mult)

                    stage1(0)
                    yield
                    for j in range(4):
                        tt_ = 4 * t + j
                        cs = slice(j * 128, (j + 1) * 128)
                        scb, kz = scbs[j % 2], kzs[j % 2]
                        pi_ = ps[3]
                        for h in range(8):
                            S.mm(pi_[:, h * 64:(h + 1) * 64], scb[:, h, :], vret[:, j, h * 64:(h + 1) * 64])
                        for p in range(4):
                            for hh in range(2):
                                rows = slice(hh * 64, hh * 64 + 64)
                                S.mm(ps[4 + hh][:, p * 64:(p + 1) * 64], rq[rows, p, cs],
                                     Rb[rows, p, hh * 64:hh * 64 + 64])
                        pkv = ps[6]
                        for p in range(4):
                            S.mm(pkv[:, p * 128:(p + 1) * 128], kz[:, p * 128:(p + 1) * 128],
                                 vret[:, j, p * 128:(p + 1) * 128])
                        for hh in range(2):
                            xib = V(K["xi"].ap[:, hh::2].unsqueeze(2).to_broadcast([128, 4, 64]), K["xi"].bufs)
                            S.tt(o1[:, hh::2, :], ps[4 + hh][:, 0:256].re("p (h d) -> p h d", h=4), xib, ALU.mult)
                        S.tt(o1, o1, pi_.re("p (h d) -> p h d", h=8), ALU.add)
                        for p in range(4):
                            S.stt(R32[:, p, :], R32[:, p, :], K["cdv"][:, p:p + 1], pkv[:, p * 128:(p + 1) * 128],
                                  ALU.mult, ALU.add)
                        S.copy(Rb, R32, eng="pool")
                        yield
                        if j + 1 < 4:
                            stage1(j + 1)
                            yield
                        S.reduce(st8[:, 0, :], o1, ALU.add)
                        S.tt(o2, o1, o1, ALU.mult, eng="pool")
                        S.reduce(st8[:, 1, :], o2, ALU.add)
                        S.ts(st8[:, 0, :], st8[:, 0, :], 1.0 / 64, None, ALU.mult)
                        S.tt(st8[:, 2, :], st8[:, 0, :], st8[:, 0, :], ALU.mult)
                        S.stt(st8[:, 1, :], st8[:, 1, :], 1.0 / 64, st8[:, 2, :], ALU.mult, ALU.subtract)
                        S.ts(st8[:, 1, :], st8[:, 1, :], 1e-5, None, ALU.add)
                        yield
                        S.act(st8[:, 1, :], st8[:, 1, :], AF.Sqrt)
                        yield
                        S.recip(st8[:, 3, :], st8[:, 1, :])
                        yield
                        mb = V(st8.ap[:, 0, :].unsqueeze(2).to_broadcast([128, 8, 64]), st8.bufs)
                        rb_ = V(st8.ap[:, 3, :].unsqueeze(2).to_broadcast([128, 8, 64]), st8.bufs)
                        S.tt(o2, o1, mb, ALU.subtract)
                        S.tt(o2, o2, rb_, ALU.mult)
                        mo = mst[tt_ % 2]
                        S.tt(mo, o2.re("p h d -> p (h d)"), sg[:, j, :], ALU.mult)
                        S.dma("sp", mret_d[tt_ * 128:(tt_ + 1) * 128, :], mo, sembuf=mo.bufs[0])
                        yield

                def interleave(ga, gr, ratio=2):
                    a_done = ga is None
                    r_done = gr is None
                    while not (a_done and r_done):
                        if not r_done:
                            try:
                                next(gr)
                            except StopIteration:
                                r_done = True
                        for _ in range(ratio):
                            if not a_done:
                                try:
                                    next(ga)
                                except StopIteration:
                                    a_done = True

                for _ in genA1(0):
                    pass
                interleave(genA(0), None)
                for t in range(NS):
                    interleave(genA(t + 1) if t + 1 < NS else None, genR(t))
                hT = hTs[(NS - 1) % 2]

                if "passA" in debug:
                    S.barrier()
                    d_hT = dbg_out("hT", [128, 8, 512], BF16)
                    S.dma("sp", d_hT, hT, is_output=True)
                    d_kvc = dbg_out("kvcT", [64, 2, 2, 256], BF16)
                    S.dma("sp", d_kvc, kvcT, is_output=True)
                    for nm, src in (("qT", qT_d), ("sng", sng_d), ("gates", gates_d), ("mret", mret_d), ("ks", ks_d),
                                    ("kw", kw_d), ("vs", vs_d), ("vw", vw_d), ("tabs", tabs_d)):
                        shp = list(src.ap.shape)
                        dd = dbg_out(nm, shp, src.ap.dtype)
                        S.dma("sp", dd, src, is_output=True)
                    dG = dbg_out("gGb", [128, D])
                    S.dma("sp", dG, gGb, is_output=True)
            S.barrier()

        if "passA" in debug:
            with nc.Block() as block:
                S.emit(block)
            return nc, consts

        PASSB(nc, S, st0, alloc, ps, K, cd_, consts, kvcT, gGb, x_d, wout_d, out_d, qT_d, sng_d, gates_d, mret_d,
              ks_d, kw_d, vs_d, vw_d, debug, dbg_out)
        with nc.Block() as block:
            S.emit(block)
    return nc, consts


def PASSB(nc, S, st0, alloc, ps, K, cd_, consts, kvcT, gGb, x_d, wout_d, out_d, qT_d, sng_d, gates_d, mret_d,
          ks_d, kw_d, vs_d, vw_d, debug, dbg_out):
    with ExitStack() as stB:
        wout = alloc(stB, "wout", [128, 8, D], BF16)
        for k in range(8):
            S.dma("pool", wout[:, k, :], wout_d[k * 128:(k + 1) * 128, :])
        late_dmas = []
        ksA = alloc(stB, "ksA", [128, 2, S_LEN], BF16)
        kwT = alloc(stB, "kwT", [128, 2, S_LEN], BF16)
        S.memset(kwT[64:128], 0.0)
        kcA = alloc(stB, "kcA", [128, 2, 256], BF16)
        S.memset(kcA[64:128], 0.0)
        S.copy(kcA[0:64], kvcT[:, 0])
        vsA = alloc(stB, "vsA", [128, NT, 2, 65], BF16)
        vwA = alloc(stB, "vwA", [128, NT, 2, 65], BF16)
        S.memset(vsA[:, :, :, 64:65], 1.0)
        S.memset(vwA[:, :, :, 64:65], 1.0)
        late_dmas.append(lambda: S.dma("sp", kwT[0:64], kw_d))
        for g in range(2):
            late_dmas.append(lambda g=g: S.dma("sp", vwA[:, :, g, 0:64],
                                               vw_d.re("(t p) g d -> p t g d", p=128)[:, :, g, :]))
        late_dmas.append(lambda: S.dma("sp", ksA[0:64], ks_d))
        for g in range(2):
            late_dmas.append(lambda g=g: S.dma("sp", ksA[64:128, g, :], cd_["onehot"]))
        for g in range(2):
            late_dmas.append(lambda g=g: S.dma("sp", vsA[:, :, g, 0:64],
                                               vs_d.re("(t p) g d -> p t g d", p=128)[:, :, g, :]))
        vcA = alloc(stB, "vcA", [128, 2, 2, 65], BF16)
        S.memset(vcA, 1.0)
        for g in range(2):
            for nt in range(2):
                pb = ps[7].cast(BF16)
                S.tr(pb[:, 0:64], kvcT[:, 1, g, nt * 128:(nt + 1) * 128], K["ident_b"][0:64, 0:64])
                S.copy(vcA[:, nt, g, 0:64], pb[:, 0:64])
        Qs = [alloc(stB, "Qa%d" % i, [128, 8, 512], BF16) for i in range(2)]
        Qlo = [Buf("Qlo%d" % i) for i in range(2)]
        Qhi = [[Buf("Qhi%d_%d" % (i, g)) for g in range(2)] for i in range(2)]
        for i in range(2):
            S.memset(V(Qs[i].ap[64:128], Qhi[i]), 0.0)
        cmk = [alloc(stB, "cmk%d" % i, [128, 2, 512], BF16) for i in range(2)]
        fbt = [alloc(stB, "fbt%d" % i, [128, 4, 64], F32) for i in range(2)]
        gts = [alloc(stB, "gts%d" % i, [128, 4, 24], F32) for i in range(2)]
        sngb = [alloc(stB, "sngb%d" % i, [128, 4, 512], BF16) for i in range(2)]
        NPT = 10
        PT = [alloc(stB, "PT%d" % i, [128, 512], BF16) for i in range(NPT)]
        accs = [alloc(stB, "acc%d" % i, [128, 4, 512], F32) for i in range(2)]
        impacc = alloc(stB, "impacc", [128, 4, 64], F32)
        rls = [alloc(stB, "rl%d" % i, [128, 4], F32) for i in range(2)]
        sc4s = [alloc(stB, "sc4%d" % i, [128, 4], F32) for i in range(2)]
        scr = alloc(stB, "scr", [128, 64], F32)
        wk64 = alloc(stB, "wk64", [128, 64], F32)
        m8a = alloc(stB, "m8a", [128, 8], F32)
        m8b = alloc(stB, "m8b", [128, 8], F32)
        thr = alloc(stB, "thr", [128, 1], F32)
        selbs = [alloc(stB, "selb%d" % i, [128, 128], BF16) for i in range(4)]
        for i in range(4):
            S.memset(selbs[i], 0.0)
        scrs = [alloc(stB, "scr%d" % i, [128, 64], F32) for i in range(4)]
        wk64s = [alloc(stB, "wk64%d" % i, [128, 64], F32) for i in range(4)]
        m8as = [alloc(stB, "m8a%d" % i, [128, 8], F32) for i in range(4)]
        m8bs = [alloc(stB, "m8b%d" % i, [128, 8], F32) for i in range(4)]
        thrs = [alloc(stB, "thr%d" % i, [128, 1], F32) for i in range(4)]
        mix = [alloc(stB, "mix%d" % i, [128, D], BF16) for i in range(4)]
        mixTs = [alloc(stB, "mixT%d" % i, [128, 8, 128], BF16) for i in range(4)]
        xres = [alloc(stB, "xres%d" % i, [128, D], F32) for i in range(4)]
        zts = [alloc(stB, "zt%d" % i, [128, D], F32) for i in range(2)]
        junk2 = alloc(stB, "junk2", [128, D], BF16)
        ss2s = [alloc(stB, "ss2%d" % i, [128, 4], F32) for i in range(2)]
        ot = [alloc(stB, "ot%d" % i, [128, D], F32) for i in range(2)]
        cyc = {"pt": 0, "sc": 0, "o": 0, "imp": 0, "ev": 0}
        LOOK = 6
        pend = []

        fins = []

        def run_fins(limit_hseq=None, tick=False):
            keep = []
            ready = []
            for ent in fins:
                if tick:
                    ent[0] -= 1
                if ent[0] <= 0 or (limit_hseq is not None and ent[1] <= limit_hseq):
                    ready.append(ent)
                else:
                    keep.append(ent)
            if ready:
                last = max(fins.index(e) for e in ready)
                ready = fins[:last + 1]
                keep = fins[last + 1:]
            fins[:] = keep
            for ent in ready:
                ent[2]()

        def push(s1, s2, hseq, first_of_head):
            if first_of_head:
                while pend and pend[0][0] <= hseq - 2:
                    pend.pop(0)[1]()
                run_fins(limit_hseq=hseq - 2)
            s1()
            pend.append((hseq, s2))
            while len(pend) > LOOK:
                pend.pop(0)[1]()
            run_fins(tick=True)

        def flush():
            while pend or fins:
                while pend:
                    pend.pop(0)[1]()
                run_fins(limit_hseq=1 << 60)

        def load_inputs(t):
            T0 = 512 * t
            sl = t % 2
            S.dma("sp", V(Qs[sl].ap[0:64], [Qlo[sl]]), qT_d.re("h d n -> d h n")[:, :, T0:T0 + 512], sembuf=Qlo[sl])
            S.dma("sp", cmk[sl], cd_["cmpmask"][t].re("a p n -> p a n"))
            S.dma("sp", fbt[sl], cd_["fb"][4 * t:4 * t + 4].re("a p n -> p a n"))
            S.dma("sp", gts[sl], gates_d[T0:T0 + 512].re("(a p) n -> p a n", p=128))
            S.dma("sp", sngb[sl], sng_d[T0:T0 + 512].re("(a p) n -> p a n", p=128))

        oTs = [alloc(stB, "oT%d" % i, [65, 512], F32) for i in range(2)]
        zeros_f = alloc(stB, "zeros_f", [128, 272], F32)
        S.memset(zeros_f, 0.0)

        def attend(t, h, br, first, ktl, with_imp=False, after=None, otrans=False, act_off=False):
            sl = t % 2
            Q = Qs[sl]
            ob = ps[3 + cyc["o"] % 2]
            cyc["o"] += 1
            impb = None
            tb = None
            if with_imp or otrans:
                impb = ps[5 + cyc["imp"] % 2]
                cyc["imp"] += 1
            n = len(ktl)
            cyc["hseq"] = cyc.get("hseq", 0) + 1
            hseq = cyc["hseq"]

            def evac(src):
                rl = rls[cyc["ev"] % 2]
                sc4 = sc4s[cyc["ev"] % 2]
                cyc["ev"] += 1
                o3 = src[:, 0:272].re("p (q c) -> p q c", q=4)
                S.ts(rl, o3[:, :, 64], 1.0e-30, None, ALU.max)
                S.recip(rl, rl)
                S.tt(sc4, rl, gts[sl][:, :, 3 * h + br], ALU.mult)
                for qs in range(4):
                    dst = accs[t % 2][:, qs, h * 64:(h + 1) * 64]
                    if first and act_off:
                        S.act(dst, o3[:, qs, 0:64], AF.Copy, scale=sc4[:, qs:qs + 1])
                    elif first:
                        S.ts(dst, o3[:, qs, 0:64], sc4[:, qs:qs + 1], None, ALU.mult)
                    else:
                        S.stt(dst, o3[:, qs, 0:64], sc4[:, qs:qs + 1], dst, ALU.mult, ALU.add)
                return rl

            for idx, (kT, va, Krows, M, c0, c1, mask, ov) in enumerate(ktl):
                sp_ = ps[cyc["sc"] % 3]
                cyc["sc"] += 1
                pt = PT[cyc["pt"] % NPT]
                cyc["pt"] += 1

                def s1(idx=idx, kT=kT, Krows=Krows, M=M, c0=c0, c1=c1, mask=mask, sp_=sp_, pt=pt):
                    if idx == 0 and not otrans:
                        if act_off:
                            S.act(ob[:, 0:272], zeros_f[:, 0:272], AF.Copy)
                            S.act(impb[:, 0:272], zeros_f[:, 0:272], AF.Copy)
                        else:
                            S.memset(ob[:, 0:272], 0.0)
                            if impb is not None:
                                S.memset(impb[:, 0:272], 0.0)
                    qv = V(Q.ap[0:Krows, h, c0:c1], [Qlo[sl]] + ([Qhi[sl][h // 4]] if Krows == 128 else []))
                    S.mm(sp_[0:M, c0:c1], kT, qv, start=True, stop=(mask is None))
                    if mask is not None:
                        mv, m0, m1 = mask
                        S.mm(sp_[0:M, m0:m1], K["ident_b"][0:M, 0:M], mv, start=False, stop=True)
                    S.act(pt[0:M, c0:c1], sp_[0:M, c0:c1], AF.Exp)

                def s2(idx=idx, va=va, M=M, c0=c0, c1=c1, ov=ov, pt=pt):
                    if otrans:
                        assert idx > 0 or (c0 == 0 and c1 == 512)
                        S.mm(ob[0:65, c0:c1], va, pt[0:M, c0:c1], start=(idx == 0), stop=(idx == n - 1))
                        if idx == n - 1:
                            oT = oTs[cyc.get("ot", 0) % 2]
                            cyc["ot"] = cyc.get("ot", 0) + 1
                            S.copy(oT, ob[0:65, :])

                            def fin(oT=oT):
                                for qs in range(4):
                                    S.tr(impb[:, qs * 68:qs * 68 + 65], oT[0:65, qs * 128:(qs + 1) * 128],
                                         K["ident_f"][0:65, 0:65])
                                evac(impb)
                            fins.append([2, hseq, fin])
                        return
                    for qs in range(c0 // 128, c1 // 128):
                        S.mm(ob[:, qs * 68:qs * 68 + 65], pt[0:M, qs * 128:(qs + 1) * 128], va,
                             start=False, stop=False, skip_group_check=True)
                        if ov is not None:
                            S.mm(impb[:, qs * 68:qs * 68 + 65], pt[0:M, qs * 128:(qs + 1) * 128], ov,
                                 start=False, stop=False, skip_group_check=True)
                    if idx == n - 1:
                        rl = evac(ob)
                        if after is not None:
                            after(impb, rl)

                push(s1, s2, hseq, idx == 0)

        def b1_head(t, h, act_off):
            T0 = 512 * t
            sl = t % 2
            Q = Qs[sl]
            g, h4 = h // 4, h % 4
            nts = [nt for nt in range(2) if 16 * 128 * nt + 31 <= T0 + 511]
            ktl = []
            for nt in nts:
                M = 128 if nt == 0 else 127
                ktl.append((kcA[:, g, nt * 128:nt * 128 + M], vcA[0:M, nt, g, :], 128, M, 0, 512,
                            (cmk[sl][0:M, nt, :], 0, 512), K["ovl"][0:M, nt, :]))

            def after(impb, rl):
                i3 = impb[:, 0:272].re("p (q c) -> p q c", q=4)
                for qs in range(4):
                    if h4 == 0:
                        S.ts(impacc[:, qs, :], i3[:, qs, 0:64], rl[:, qs:qs + 1], None, ALU.mult)
                    else:
                        S.stt(impacc[:, qs, :], i3[:, qs, 0:64], rl[:, qs:qs + 1], impacc[:, qs, :],
                              ALU.mult, ALU.add)
                if h4 < 3:
                    return
                for qs in range(4):
                    S.tt(scrs[qs], impacc[:, qs, :], fbt[sl][:, qs, :], ALU.add)
                for qs in range(4):
                    S.max8(m8as[qs], scrs[qs])
                for qs in range(4):
                    S.match_replace(wk64s[qs], m8as[qs], scrs[qs], -3.0e38)
                for qs in range(4):
                    S.max8(m8bs[qs], wk64s[qs])
                for qs in range(4):
                    S.reduce(thrs[qs], m8bs[qs], ALU.min)
                for qs in range(4):
                    S.ts(thrs[qs], thrs[qs], -1.0e29, None, ALU.max)
                for qs in range(4):
                    S.ts(selbs[qs][:, 64:128], scrs[qs], thrs[qs], None, ALU.is_ge)
                for qs in range(4):
                    S.ts(selbs[qs][:, 64:128], selbs[qs][:, 64:128], 1.0, 30000.0, ALU.subtract, ALU.mult)
                pb = ps[7].cast(BF16)
                for qs in range(4):
                    S.tr(pb[:, qs * 128:(qs + 1) * 128], selbs[qs], K["ident_b"])
                for qs in range(4):
                    for hh in range(4):
                        dst = V(Q.ap[64:128, 4 * g + hh, qs * 128:(qs + 1) * 128], [Qhi[sl][g]])
                        S.copy(dst, pb[64:128, qs * 128:(qs + 1) * 128], eng=("act" if act_off else "dve"))

            attend(t, h, 0, True, ktl, with_imp=True, after=after, act_off=act_off)

        def b2_head(t, h):
            g = h // 4
            ktl = []
            for m in (4, 0, 1, 2, 3, 5, 6, 7):
                kti = 4 * (t - 1) + m
                if kti < 0:
                    continue
                if m <= 3:
                    c0, c1 = 0, 128 * (m + 1)
                    mask = (K["tri"][:, 1, :], 128 * m, 128 * m + 128)
                else:
                    c0, c1 = 128 * (m - 4), 512
                    mask = (K["tri"][:, 0, :], c0, c0 + 128)
                ktl.append((kwT[:, g, kti * 128:(kti + 1) * 128], vwA[:, kti, g, :], 128, 128, c0, c1, mask, None))
            attend(t, h, 2, False, ktl)

        def b3_head(t, h):
            g = h // 4
            ktl = []
            for kt in range(4 * t + 4):
                if kt < 4 * t:
                    c0, c1, mask = 0, 512, None
                else:
                    c0, c1 = 128 * (kt - 4 * t), 512
                    mask = (K["tri"][:, 0, :], c0, c0 + 128)
                ktl.append((ksA[:, g, kt * 128:(kt + 1) * 128], vsA[:, kt, g, :], 128, 128, c0, c1, mask, None))
            attend(t, h, 1, False, ktl)

        def make_b4(t):
            sl = t % 2

            def b4mix():
                for qs in range(4):
                    S.tt(mix[qs][:, 512:1024], accs[t % 2][:, qs, :], sngb[sl][:, qs, :], ALU.mult)

            def b4a(qs):
                mx = mix[qs]
                pb = ps[7].cast(BF16)
                for k in range(8):
                    S.tr(pb[:, k * 128:(k + 1) * 128], mx[:, k * 128:(k + 1) * 128], K["ident_b"])
                S.copy(mixTs[qs].re("p k n -> p (k n)"), pb, eng="act")

            def b4b(qs):
                tt_ = 4 * t + qs
                xr, mT = xres[qs], mixTs[qs]
                zt_, ssb = zts[tt_ % 2], ss2s[tt_ % 2]
                for half in range(2):
                    zp = ps[5 + half]
                    for k in range(8):
                        S.mm(zp, mT[:, k, :], wout[:, k, half * 512:(half + 1) * 512], start=(k == 0), stop=(k == 7))
                    S.act(junk2[:, half * 512:(half + 1) * 512], zp, AF.Square, accum_out=ssb[:, half:half + 1])
                    S.tt(zt_[:, half * 512:(half + 1) * 512], zp, gGb[:, half * 512:(half + 1) * 512], ALU.mult)
                S.tt(ssb[:, 2:3], ssb[:, 0:1], ssb[:, 1:2], ALU.add)
                S.ts(ssb[:, 2:3], ssb[:, 2:3], 1.0 / D, 1e-6, ALU.mult, ALU.add)
                S.act(ssb[:, 2:3], ssb[:, 2:3], AF.Ln)
                S.act(ssb[:, 3:4], ssb[:, 2:3], AF.Exp, scale=-0.5)
                o_ = ot[tt_ % 2]
                S.stt(o_, zt_, ssb[:, 3:4], xr, ALU.mult, ALU.add)
                S.dma("pool", out_d[tt_ * 128:(tt_ + 1) * 128, :], o_, sembuf=o_.bufs[0], is_output=True)

            return b4mix, [lambda: b4a(0), lambda: b4a(1), lambda: b4a(2), lambda: b4a(3),
                           lambda: b4b(0), lambda: b4b(1), lambda: b4b(2), lambda: b4b(3)]

        load_inputs(0)
        for fn in late_dmas:
            fn()
        for h in range(8):
            b1_head(0, h, True)
        b4_prev = None
        for t in range(NS):
            for h in range(8):
                b2_head(t, h)
                if b4_prev is not None:
                    b4_prev[h]()
            if t + 1 < NS:
                load_inputs(t + 1)
            for qs in range(4):
                tt_ = 4 * t + qs
                S.dma("sp", mix[qs][:, 0:512], mret_d[tt_ * 128:(tt_ + 1) * 128, :])
                S.dma("sp", xres[qs], x_d[tt_ * 128:(tt_ + 1) * 128, :])
            for h in range(8):
                b3_head(t, h)
                if t + 1 < NS:
                    b1_head(t + 1, h, False)
            flush()
            b4mix, b4_prev = make_b4(t)
            b4mix()
        for st_ in b4_prev:
            st_()


def core_inputs(inp, consts, b):
    m = {
        "x": np.ascontiguousarray(inp["x"][b]),
        "c": np.ascontiguousarray(inp["c"][b]),
        "positions": np.ascontiguousarray(inp["positions"][b:b + 1]).astype(np.int32),
        "w_ada": np.ascontiguousarray(inp["w_ada"][0]),
        "b_ada": np.ascontiguousarray(inp["b_ada"][0]),
        "g_pre": np.ascontiguousarray(inp["g_pre"][0]),
        "g_post": np.ascontiguousarray(inp["g_post"][0]),
        "w_in": np.ascontiguousarray(inp["w_in"][0]),
        "w_out": np.ascontiguousarray(inp["w_out"][0]),
        "cmp_pe_k": np.ascontiguousarray(inp["cmp_pe_k"][0]).reshape(-1),
        "cmp_w1_k": np.ascontiguousarray(inp["cmp_w1_k"][0]),
        "cmp_w2_k": np.ascontiguousarray(inp["cmp_w2_k"][0]),
        "cmp_pe_v": np.ascontiguousarray(inp["cmp_pe_v"][0]).reshape(-1),
        "cmp_w1_v": np.ascontiguousarray(inp["cmp_w1_v"][0]),
        "cmp_w2_v": np.ascontiguousarray(inp["cmp_w2_v"][0]),
    }
    for k, v in consts.items():
        m["k_" + k] = v
    return m


_CACHE = {}


def kernel(**inputs):
    if "nc" not in _CACHE:
        _CACHE["nc"] = build()
    nc, consts = _CACHE["nc"]
    inp = {k: np.asarray(v) for k, v in inputs.items()}
    in_maps = [core_inputs(inp, consts, b) for b in range(8)]
    res = run_bass_kernel_spmd(nc, in_maps, core_ids=list(range(8)))
    out = np.stack([np.asarray(r["out"]) for r in res.results], axis=0)
    return out.astype(np.float32)
```

```python
import numpy as np
from contextlib import ExitStack
import ml_dtypes
import concourse.bass as bass
import concourse.mybir as mybir
from concourse.bass_utils import run_bass_kernel_spmd

F32 = mybir.dt.float32
BF16 = mybir.dt.bfloat16
I32 = mybir.dt.int32
AF = mybir.ActivationFunctionType
ALU = mybir.AluOpType
AX = mybir.AxisListType

S_LEN = 4096
D = 1024
NT = 32
NS = 8
PW = 3864
PI = float(np.pi)
DBG_T = 0


class Buf:
    __slots__ = ("w", "r", "sem", "semcnt", "name", "uid", "excl", "dram")
    _n = [0]

    def __init__(self, name=""):
        Buf._n[0] += 1
        self.uid = Buf._n[0]
        self.w = None
        self.r = {}
        self.sem = None
        self.semcnt = 0
        self.name = name
        self.excl = False
        self.dram = False


class V:
    __slots__ = ("ap", "bufs")

    def __init__(self, ap, bufs):
        self.ap = ap
        self.bufs = bufs if isinstance(bufs, (list, tuple)) else [bufs]

    def __getitem__(self, k):
        return V(self.ap[k], self.bufs)

    def re(self, s, **kw):
        return V(self.ap.rearrange(s, **kw), self.bufs)

    def bc(self, shape):
        return V(self.ap.to_broadcast(list(shape)), self.bufs)

    def cast(self, dt):
        return V(self.ap.bitcast(dt), self.bufs)

    def sub(self, bufs):
        return V(self.ap, bufs)


COMPUTE = ("pe", "act", "dve", "pool")


class Sched:
    def __init__(self, nc, stack):
        self.nc = nc
        self.stack = stack
        self.streams = {e: [] for e in ("pe", "act", "dve", "pool", "sp")}
        self.cnt = {e: 0 for e in COMPUTE}
        self.waited = {e: {} for e in self.streams}
        self.sems = {e: stack.enter_context(nc.semaphore("s_" + e)) for e in COMPUTE}
        self.dma_sems = {}
        self.dma_bufs = []
        self.needed = {e: set() for e in COMPUTE}
        self.out_toks = []

    def _deps(self, eng, reads, writes):
        deps = {}

        def add(tok):
            k, i = tok
            if deps.get(k, -1) < i:
                deps[k] = i

        for b in reads:
            if b.excl:
                for k, i in b.r.items():
                    if k != eng:
                        add((k, i))
            if b.w is not None:
                if b.w[0] == eng and eng == "pe":
                    continue
                add(b.w)
        for b in writes:
            if b.w is not None and (b.w[0] != eng or eng != "pe"):
                add(b.w)
            for k, i in b.r.items():
                if k != eng or eng != "pe":
                    add((k, i))
        return self._emit_waits(eng, deps)

    def _emit_waits(self, eng, deps):
        waits = []
        wd = self.waited[eng]
        for k, i in deps.items():
            if wd.get(k, -1) < i:
                wd[k] = i
                waits.append((k, i))
                if k in COMPUTE:
                    self.needed[k].add(i)
        return waits

    def _mark(self, tok, reads, writes):
        k, i = tok
        for b in reads:
            if b.r.get(k, -1) < i:
                b.r[k] = i
        for b in writes:
            b.w = tok
            b.r = {}

    def op(self, eng, fn, reads, writes):
        rb = [b for v in reads if isinstance(v, V) for b in v.bufs]
        wb = [b for v in writes if isinstance(v, V) for b in v.bufs]
        waits = self._deps(eng, rb, wb)
        self.cnt[eng] += 1
        idx = self.cnt[eng]
        self.streams[eng].append(("c", waits, fn, idx))
        self._mark((eng, idx), rb, wb)

    def dma(self, q, out, in_, sembuf=None, is_output=False, **kw):
        rb = list(in_.bufs)
        wb = list(out.bufs)
        sb = sembuf or wb[0]
        if sb.sem is None:
            sb.sem = self.stack.enter_context(self.nc.semaphore("d_%d" % sb.uid))
            self.dma_sems[sb.uid] = sb.sem
            self.dma_bufs.append(sb)
        waits = self._deps(q, rb, [b for b in wb if not b.dram])
        sb.semcnt += 16
        tok = (("dma", sb.uid), sb.semcnt)
        self.streams[q].append(("d", waits, (out.ap, in_.ap, kw), sb.sem))
        self._mark(tok, rb, wb)
        if is_output:
            self.out_toks.append(tok)
        return tok

    def barrier(self, skip=()):
        deps = {}
        for e in COMPUTE:
            if self.cnt[e] > 0:
                deps[e] = self.cnt[e]
        skip_ids = {b.uid for b in skip}
        for b in self.dma_bufs:
            if b.uid in skip_ids:
                continue
            deps[("dma", b.uid)] = b.semcnt
        for e in self.streams:
            w = self._emit_waits(e, dict(deps))
            if w:
                self.streams[e].append(("w", w, None, None))

    def emit(self, block):
        fin = self._emit_waits("sp", {k: i for k, i in self.out_toks})
        self.streams["sp"].append(("w", fin, None, None))
        pref = {}
        for e in COMPUTE:
            m = {}
            c = 0
            for i in range(1, self.cnt[e] + 1):
                if i in self.needed[e]:
                    c += 1
                    m[i] = c
            pref[e] = m
        names = {"pe": "tensor", "act": "scalar", "dve": "vector", "pool": "gpsimd", "sp": "sync"}
        for e, st in self.streams.items():
            def body(eng, e=e, st=st):
                for kind, waits, fn, extra in st:
                    for k, i in waits:
                        if k in COMPUTE:
                            eng.wait_ge(self.sems[k], pref[k][i])
                        else:
                            eng.wait_ge(self.dma_sems[k[1]], i)
                    if kind == "c":
                        ins = fn(eng)
                        if extra in self.needed[e]:
                            ins.then_inc(self.sems[e], 1)
                    elif kind == "d":
                        oap, iap, kw = fn
                        eng.dma_start(out=oap, in_=iap, **kw).then_inc(extra, 16)
            getattr(block, names[e])(body)

    def mm(self, out, lhsT, rhs, start=True, stop=True, **kw):
        self.op("pe", lambda e: e.matmul(out.ap, lhsT.ap, rhs.ap, start=start, stop=stop, **kw),
                [lhsT, rhs], [out])

    def tr(self, out, in_, ident):
        self.op("pe", lambda e: e.transpose(out.ap, in_.ap, ident.ap), [in_, ident], [out])

    def act(self, out, in_, func, bias=None, scale=None, accum_out=None):
        kw = {}
        rd = [in_]
        if bias is not None:
            kw["bias"] = bias.ap if isinstance(bias, V) else bias
            rd.append(bias)
        if scale is not None:
            kw["scale"] = scale.ap if isinstance(scale, V) else scale
            rd.append(scale)
        wr = [out]
        if accum_out is not None:
            kw["accum_out"] = accum_out.ap
            wr.append(accum_out)
        self.op("act", lambda e: e.activation(out.ap, in_.ap, func, **kw), rd, wr)

    def tt(self, out, in0, in1, op, eng="dve"):
        self.op(eng, lambda e: e.tensor_tensor(out.ap, in0.ap, in1.ap, op), [in0, in1], [out])

    def ts(self, out, in0, s1, s2, op0, op1=None, eng="dve"):
        a1 = s1.ap if isinstance(s1, V) else s1
        a2 = s2.ap if isinstance(s2, V) else s2
        kw = {}
        if op1 is not None:
            kw["op1"] = op1
        self.op(eng, lambda e: e.tensor_scalar(out.ap, in0.ap, a1, a2, op0, **kw), [in0, s1, s2], [out])

    def stt(self, out, in0, scalar, in1, op0, op1, eng="dve"):
        a = scalar.ap if isinstance(scalar, V) else scalar
        self.op(eng, lambda e: e.scalar_tensor_tensor(out.ap, in0.ap, a, in1.ap, op0, op1),
                [in0, scalar, in1], [out])

    def copy(self, out, in_, eng="dve"):
        if eng == "act":
            self.op("act", lambda e: e.copy(out.ap, in_.ap), [in_], [out])
        else:
            self.op(eng, lambda e: e.tensor_copy(out.ap, in_.ap), [in_], [out])

    def memset(self, out, val, eng="dve"):
        self.op(eng, lambda e: e.memset(out.ap, val), [], [out])

    def reduce(self, out, in_, op, eng="dve"):
        self.op(eng, lambda e: e.tensor_reduce(out.ap, in_.ap, AX.X, op), [in_], [out])

    def recip(self, out, in_):
        self.op("dve", lambda e: e.reciprocal(out.ap, in_.ap), [in_], [out])

    def max8(self, out, in_):
        self.op("dve", lambda e: e.max(out.ap, in_.ap), [in_], [out])

    def match_replace(self, out, to_replace, values, imm):
        self.op("dve", lambda e: e.match_replace(out.ap, to_replace.ap, values.ap, imm),
                [to_replace, values], [out])


def make_consts():
    bf = ml_dtypes.bfloat16
    c = {}
    c["ident_f"] = np.eye(128, dtype=np.float32)
    c["ident_b"] = np.eye(128, dtype=np.float32).astype(bf)
    c["ones_f"] = np.ones((128, 128), np.float32)
    permR = np.zeros((128, 128), np.float32)
    permN = np.zeros((128, 128), np.float32)
    invR = np.zeros((128, 1), np.float32)
    invN = np.zeros((128, 1), np.float32)
    ir = np.power(np.float32(10000.0), -np.arange(32, dtype=np.float32) / np.float32(32))
    inn = np.power(np.float32(500000.0), -np.arange(8, dtype=np.float32) / np.float32(8))
    for m in range(128):
        blk, d = (m // 64) * 64, m % 64
        if d < 32:
            permR[blk + d + 32, m] = -1.0
        else:
            permR[blk + d - 32, m] = 1.0
        invR[m, 0] = ir[d % 32]
        if d < 8:
            permN[blk + d + 8, m] = -1.0
            invN[m, 0] = inn[d]
        elif d < 16:
            permN[blk + d - 8, m] = 1.0
            invN[m, 0] = inn[d - 8]
    c["permR"] = permR.astype(bf)
    c["permN"] = permN.astype(bf)
    c["inv2"] = np.concatenate([invR, invN], axis=1).astype(np.float64) / (2 * np.pi)
    c["inv2"] = c["inv2"].astype(np.float32)
    H = 8
    log_g = np.log1p(-np.power(2.0, -5.0 - np.arange(H, dtype=np.float64)))
    idx = np.arange(128, dtype=np.float64)
    dec = np.zeros((128, H, 128), np.float32)
    for h in range(H):
        diff = idx[None, :] - idx[:, None]
        dec[:, h, :] = np.where(diff >= 0, np.exp(np.maximum(diff, 0) * log_g[h]), 0.0)
    c["decayT"] = dec
    c["xi"] = np.exp((idx[:, None] + 1.0) * log_g[None, :]).astype(np.float32)
    c["zeta"] = np.exp((127.0 - idx[:, None]) * log_g[None, :]).astype(np.float32)
    cd = np.exp(128.0 * log_g)
    cdv = np.zeros((128, 4), np.float32)
    for m in range(128):
        for p in range(4):
            cdv[m, p] = cd[2 * p + m // 64]
    c["cdv"] = cdv
    keys = np.arange(S_LEN)
    c["onehot"] = (keys[None, :] // 64 == np.arange(64)[:, None]).astype(np.float32).astype(bf)
    kk = np.arange(128)[:, None]
    qq = np.arange(128)[None, :]
    c["tri"] = (-30000.0 * (1.0 - np.stack([(kk <= qq), (kk > qq)], axis=1).astype(np.float32))).astype(bf)
    cm = np.zeros((NS, 2, 128, 512), np.float32)
    for t in range(NS):
        for nt in range(2):
            n = np.arange(128)[:, None] + 128 * nt
            q = 512 * t + np.arange(512)[None, :]
            cm[t, nt] = ((16 * n + 31 <= q) & (n < 255))
    c["cmpmask"] = (-30000.0 * (1.0 - cm)).astype(bf)
    fb = np.zeros((NT, 128, 64), np.float32)
    for qt in range(NT):
        for ql in range(128):
            cur = (qt * 128 + ql) // 64
            fb[qt, ql, :] = np.where(np.arange(64) > cur, -1e30, 0.0)
            fb[qt, ql, 0] = 1e9
            if cur - 1 >= 0:
                fb[qt, ql, cur - 1] = 3e9
            fb[qt, ql, cur] = 2e9
    c["fb"] = fb
    Nc = 255
    cs = np.arange(Nc) * 16
    ce = cs + 31
    ss = np.arange(64) * 64
    ov = ((cs[:, None] <= ss[None, :] + 63) & (ce[:, None] >= ss[None, :])).astype(np.float32)
    ova = np.zeros((256, 65), np.float32)
    ova[:Nc, :64] = ov
    ova[:Nc, 64] = 1.0
    c["ovl"] = ova.astype(bf)
    return c


CONST_SPECS = None


def build(debug=()):
    nc = bass.Bass("TRN2", target_bir_lowering=False)
    consts = make_consts()
    dts = {np.dtype(np.float32): F32, np.dtype(ml_dtypes.bfloat16): BF16}

    def dbuf(name):
        b = Buf(name)
        b.dram = True
        return b

    def din(name, shape, dt):
        return V(nc.dram_tensor(name, list(shape), dt, kind="ExternalInput").ap(), dbuf(name))

    x_d = din("x", [S_LEN, D], F32)
    c_d = din("c", [D], F32)
    pos_d = din("positions", [1, S_LEN], I32)
    wada_d = din("w_ada", [D, 3 * D], F32)
    bada_d = din("b_ada", [3 * D], F32)
    gpre_d = din("g_pre", [D], F32)
    gpost_d = din("g_post", [D], F32)
    win_d = din("w_in", [D, PW], F32)
    wout_d = din("w_out", [D, D], F32)
    pek_d = din("cmp_pe_k", [2048], F32)
    w1k_d = din("cmp_w1_k", [2048, 256], F32)
    w2k_d = din("cmp_w2_k", [256, 64], F32)
    pev_d = din("cmp_pe_v", [2048], F32)
    w1v_d = din("cmp_w1_v", [2048, 256], F32)
    w2v_d = din("cmp_w2_v", [256, 64], F32)
    cd_ = {k: din("k_" + k, v.shape, dts[v.dtype]) for k, v in consts.items()}
    out_d = V(nc.dram_tensor("out", [S_LEN, D], F32, kind="ExternalOutput").ap(), dbuf("out"))
    dbg = {}

    def dbg_out(name, shape, dt=F32):
        dbg[name] = V(nc.dram_tensor("dbg_" + name, list(shape), dt, kind="ExternalOutput").ap(), Buf(name))
        return dbg[name]

    def scratch(name, shape, dt):
        return V(nc.dram_tensor(name, list(shape), dt).ap(), dbuf(name))

    tabs_d = scratch("tabs_s", [4, 128, S_LEN], F32)
    qT_d = scratch("qT_s", [8, 64, S_LEN], BF16)
    sng_d = scratch("sng_s", [S_LEN, 512], BF16)
    gates_d = scratch("gates_s", [S_LEN, 24], F32)
    mret_d = scratch("mret_s", [S_LEN, 512], BF16)
    ks_d = scratch("ks_s", [64, 2, S_LEN], BF16)
    kw_d = scratch("kw_s", [64, 2, S_LEN], BF16)
    vs_d = scratch("vs_s", [S_LEN, 2, 64], BF16)
    vw_d = scratch("vw_s", [S_LEN, 2, 64], BF16)

    with ExitStack() as st0:
        S = Sched(nc, st0)

        def alloc(st, name, shape, dt):
            t = st.enter_context(nc.sbuf_tensor(name, list(shape), dt))
            return V(t[:], Buf(name))

        ps = []
        for i in range(8):
            t = st0.enter_context(nc.psum_tensor("ps%d" % i, [128, 512], F32))
            ps.append(V(t[:], Buf("ps%d" % i)))
            ps[-1].bufs[0].excl = True

        K = {}
        deferred = []
        for name in ("ident_f", "ident_b", "ones_f", "permR", "permN", "inv2", "decayT", "xi", "zeta", "cdv",
                     "tri", "ovl"):
            v = consts[name]
            if name == "ovl":
                K[name] = alloc(st0, "c_" + name, [128, 2, 65], BF16)
                deferred.append(lambda name=name: S.dma("sp", K[name], cd_[name].re("(t p) c -> p t c", p=128)))
            else:
                K[name] = alloc(st0, "c_" + name, v.shape, dts[v.dtype])
                deferred.append(lambda name=name: S.dma("sp", K[name], cd_[name]))
        kvcT = alloc(st0, "kvcT", [64, 2, 2, 256], BF16)
        S.memset(kvcT, 0.0)
        ctab = alloc(st0, "ctab", [64, 2, 256], F32)
        Gs = alloc(st0, "Gs", [128, 8], F32)
        shf = alloc(st0, "shf", [128, 8], F32)
        gGb = alloc(st0, "gGb", [128, D], F32)
        b1 = alloc(st0, "b1", [128, 2, 2], F32)

        with ExitStack() as stA:
            win = alloc(stA, "win", [128, 8, PW], BF16)
            for k in range(8):
                deferred.append(lambda k=k: S.dma("pool", win[:, k, :], win_d[k * 128:(k + 1) * 128, :]))
            w1 = alloc(stA, "w1", [128, 2, 16, 256], BF16)
            w2 = alloc(stA, "w2", [128, 2, 2, 64], BF16)
            wkc = alloc(stA, "wkc", [128, 8, 4, 128], BF16)

            with ExitStack() as stP:
                wadaf = [alloc(stP, "wadaf%d" % i, [128, 3 * D], F32) for i in range(2)]
                def rot_tables_multi(posf, n, jobs, tmps):
                    for (inv_col, phase, outv), (u, ki, kf) in zip(jobs, tmps):
                        if phase == 0.0:
                            S.ts(u[:, 0:n], posf, inv_col, None, ALU.mult)
                        else:
                            S.ts(u[:, 0:n], posf, inv_col, phase, ALU.mult, ALU.add)
                    for (inv_col, phase, outv), (u, ki, kf) in zip(jobs, tmps):
                        S.copy(ki[:, 0:n], u[:, 0:n])
                    for (inv_col, phase, outv), (u, ki, kf) in zip(jobs, tmps):
                        S.copy(kf[:, 0:n], ki[:, 0:n])
                    for (inv_col, phase, outv), (u, ki, kf) in zip(jobs, tmps):
                        S.tt(u[:, 0:n], u[:, 0:n], kf[:, 0:n], ALU.subtract)
                    for (inv_col, phase, outv), (u, ki, kf) in zip(jobs, tmps):
                        S.act(outv, u[:, 0:n], AF.Sin, scale=2 * PI)

                posi_all = alloc(stP, "posi_all", [128, S_LEN], I32)
                S.dma("sp", posi_all, V(pos_d.ap[0:1, :].partition_broadcast(128), pos_d.bufs))
                for fn in deferred:
                    fn()
                S.dma("pool", w1[:, 0], w1k_d.re("(j p) h -> p j h", p=128))
                S.dma("pool", w1[:, 1], w1v_d.re("(j p) h -> p j h", p=128))
                S.dma("pool", w2[:, 0], w2k_d.re("(c p) d -> p c d", p=128))
                S.dma("pool", w2[:, 1], w2v_d.re("(c p) d -> p c d", p=128))
                posi = [alloc(stP, "posi%d" % i, [128, 512], I32) for i in range(2)]
                posf = [alloc(stP, "posf%d" % i, [128, 512], F32) for i in range(2)]
                tmps = [(alloc(stP, "tu%d" % i, [128, 512], F32), alloc(stP, "tki%d" % i, [128, 512], I32),
                         alloc(stP, "tkf%d" % i, [128, 512], F32)) for i in range(4)]
                tout2 = [[alloc(stP, "tout%d_%d" % (i, j), [128, 512], F32) for i in range(4)] for j in range(2)]
                for ch in range(NS):
                    tout = tout2[ch % 2]
                    sl = slice(ch * 512, (ch + 1) * 512)
                    pi_, pf_ = posi[ch % 2], posf[ch % 2]
                    S.copy(pf_, posi_all[:, sl])
                    jobs = [(K["inv2"][:, 0:1], 0.25, tout[0]), (K["inv2"][:, 0:1], 0.0, tout[1]),
                            (K["inv2"][:, 1:2], 0.25, tout[2]), (K["inv2"][:, 1:2], 0.0, tout[3])]
                    rot_tables_multi(pf_, 512, jobs, tmps)
                    for i in range(4):
                        S.dma("act", tabs_d[i][:, sl], tout[i], sembuf=tout[i].bufs[0])
                S.memset(posi[0][0:64, 0:256], 0)
                S.dma("sp", posi[0][0:64, 0:255],
                      V(pos_d.ap[0:1, 31:4096:16].partition_broadcast(64), pos_d.bufs),
                      allow_slow_non_contiguous=True)
                S.copy(posf[0][0:64, 0:256], posi[0][0:64, 0:256])
                jobs = [(K["inv2"][0:64, 1:2], 0.25, ctab[:, 0, :]), (K["inv2"][0:64, 1:2], 0.0, ctab[:, 1, :])]
                rot_tables_multi(posf[0][0:64, 0:256], 256, jobs,
                                 [tuple(x[0:64] for x in tmps[0]), tuple(x[0:64] for x in tmps[1])])
                cs_ = alloc(stP, "cs", [128, 8], F32)
                S.dma("sp", cs_, c_d.re("(k p) -> p k", p=128), allow_slow_non_contiguous=True)
                csb = alloc(stP, "csb", [128, 8], F32)
                S.act(csb, cs_, AF.Silu)
                badaT = alloc(stP, "badaT", [128, 24], F32)
                S.dma("sp", badaT, bada_d.re("(k p) -> p k", p=128), allow_slow_non_contiguous=True)
                gpp = alloc(stP, "gpp", [128, 2, 8], F32)
                S.dma("sp", gpp[:, 0], gpre_d.re("(k p) -> p k", p=128), allow_slow_non_contiguous=True)
                S.dma("sp", gpp[:, 1], gpost_d.re("(k p) -> p k", p=128), allow_slow_non_contiguous=True)
                S.memset(ps[0][:, 0:24], 0.0)
                for k in range(8):
                    wf = wadaf[k % 2]
                    S.dma("sp", wf, wada_d[k * 128:(k + 1) * 128, :])
                    for jc in range(24):
                        S.mm(ps[0][:, jc:jc + 1], wf[:, jc * 128:(jc + 1) * 128], csb[:, k:k + 1],
                             start=False, stop=(k == 7), skip_group_check=True)
                mod = alloc(stP, "mod", [128, 24], F32)
                S.tt(mod, ps[0][:, 0:24], badaT, ALU.add)
                S.copy(shf, mod[:, 0:8])
                S.stt(Gs, mod[:, 8:16], 1.0, gpp[:, 0], ALU.add, ALU.mult)
                gG = alloc(stP, "gG", [128, 8], F32)
                S.tt(gG, mod[:, 16:24], gpp[:, 1], ALU.mult)
                dg = alloc(stP, "dg", [128, 128], F32)
                for k in range(8):
                    S.ts(dg, K["ident_f"], gG[:, k:k + 1], None, ALU.mult)
                    S.mm(ps[1 + k // 4][:, (k % 4) * 128:(k % 4 + 1) * 128], K["ones_f"], dg)
                S.copy(gGb[:, 0:512], ps[1])
                S.copy(gGb[:, 512:1024], ps[2])
                pef = alloc(stP, "pef", [128, 2, 16], F32)
                S.dma("sp", pef[:, 0], pek_d.re("(j p) -> p j", p=128), allow_slow_non_contiguous=True)
                S.dma("sp", pef[:, 1], pev_d.re("(j p) -> p j", p=128), allow_slow_non_contiguous=True)
                peb = alloc(stP, "peb", [128, 2, 16], BF16)
                S.copy(peb, pef)
                for kv in range(2):
                    for hc in range(2):
                        for j in range(16):
                            S.mm(ps[3][:, kv * 2 + hc:kv * 2 + hc + 1], w1[:, kv, j, hc * 128:(hc + 1) * 128],
                                 peb[:, kv, j:j + 1], start=(j == 0), stop=(j == 15))
                S.copy(b1.re("p a b -> p (a b)"), ps[3][:, 0:4])

            S.barrier(skip=(w1.bufs[0], w2.bufs[0]))
            for i4 in range(4):
                c0 = 2560 + 64 * i4
                S.copy(wkc[:, :, i4, 0:64], win[:, :, c0:c0 + 64], eng="pool")
                S.copy(wkc[:, :, i4, 64:128], win[:, :, c0:c0 + 64], eng="pool")

            with ExitStack() as stW:
                xb_ = [alloc(stW, "xt%d" % i, [128, D], F32) for i in range(2)]
                junk = alloc(stW, "junk", [128, D], BF16)
                ssq = alloc(stW, "ssq", [128, 1], F32)
                rstd = alloc(stW, "rstd", [128, 1], F32)
                xn = [alloc(stW, "xn", [128, D], BF16)] * 2
                hTs = [alloc(stW, "hT%d" % i, [128, 8, 512], BF16) for i in range(2)]
                tab = alloc(stW, "tab", [128, 4, 512], F32)
                rqs = [alloc(stW, "rq%d" % i, [128, 4, 512], BF16) for i in range(2)]
                rks = [alloc(stW, "rk%d" % i, [128, 4, 512], BF16) for i in range(2)]
                xbs = [alloc(stW, "xbs%d" % i, [128, 512], BF16) for i in range(2)]
                t1s = [alloc(stW, "t1", [128, 512], F32)] * 2
                t2s = [alloc(stW, "t2", [128, 512], F32)] * 2
                qn = [alloc(stW, "qn%d" % i, [128, 512], BF16) for i in range(2)]
                kst = [alloc(stW, "kst%d" % i, [64, 512], BF16) for i in range(2)]
                vrets = [alloc(stW, "vret%d" % i, [128, 4, 512], BF16) for i in range(2)]
                sgs = [alloc(stW, "sg%d" % i, [128, 4, 512], BF16) for i in range(2)]
                sng = [alloc(stW, "sng%d" % i, [128, 512], BF16) for i in range(2)]
                vst = [alloc(stW, "vst%d" % i, [128, 2, 2, 64], BF16) for i in range(2)]
                gst = [alloc(stW, "gst%d" % i, [128, 24], F32) for i in range(2)]
                KC = [alloc(stW, "KC%d" % i, [128, 2, 2, 528], BF16) for i in range(2)]
                hid = alloc(stW, "hid", [128, 2, 2, 64], BF16)
                kcx = alloc(stW, "kcx", [64, 64], BF16)
                scbs = [alloc(stW, "scb%d" % i, [128, 8, 128], BF16) for i in range(2)]
                kzs = [alloc(stW, "kz%d" % i, [128, 512], BF16) for i in range(2)]
                R32 = alloc(stW, "R32", [128, 4, 128], F32)
                Rb = alloc(stW, "Rb", [128, 4, 128], BF16)
                o1 = alloc(stW, "o1", [128, 8, 64], F32)
                o2 = alloc(stW, "o2", [128, 8, 64], F32)
                st8 = alloc(stW, "st8", [128, 4, 8], F32)
                mst = [alloc(stW, "mst%d" % i, [128, 512], BF16) for i in range(2)]
                S.memset(R32, 0.0)
                S.memset(Rb, 0.0)
                for i in range(2):
                    S.memset(KC[i], 0.0)
                print("passA sbuf_base", nc.sbuf_base, "top", nc.sbuf_top)

                rotc = [0]
                rot_pending = []

                def rotary(src_ps, npart, perm, cosv, sinv, dest, scale, then=None):
                    slot = rotc[0] % 2
                    rotc[0] += 1
                    xb = xbs[slot][0:npart]
                    S.act(xb, src_ps, AF.Copy, scale=scale)

                    def fin():
                        t1, t2 = t1s[slot], t2s[slot]
                        S.mm(ps[7][0:npart, :], perm, xb)
                        S.tt(t1[0:npart], ps[7][0:npart, :], sinv, ALU.mult)
                        S.tt(t2[0:npart], xb, cosv, ALU.mult, eng="pool")
                        S.tt(dest, t1[0:npart], t2[0:npart], ALU.add)
                        if then is not None:
                            then()

                    while rot_pending:
                        rot_pending.pop(0)()
                    rot_pending.append(fin)

                def rot_flush():
                    while rot_pending:
                        rot_pending.pop(0)()

                pscyc = [0]

                def next_ps():
                    i = pscyc[0] % 3
                    pscyc[0] += 1
                    return ps[i]

                def genA1(t):
                    hT = hTs[t % 2]
                    for j in range(4):
                        tt_ = 4 * t + j
                        xt = xb_[tt_ % 2]
                        S.dma("sp", xt, x_d[tt_ * 128:(tt_ + 1) * 128, :])
                        S.act(junk, xt, AF.Square, accum_out=ssq)
                        S.ts(rstd, ssq, 1.0 / D, 1e-6, ALU.mult, ALU.add)
                        S.act(rstd, rstd, AF.Sqrt)
                        S.recip(rstd, rstd)
                        xnb = xn[tt_ % 2]
                        S.act(xnb, xt, AF.Copy, scale=rstd)
                        yield
                        pb = ps[7].cast(BF16)
                        for k in range(8):
                            S.tr(pb[:, k * 128:(k + 1) * 128], xnb[:, k * 128:(k + 1) * 128], K["ident_b"])
                        for k in range(8):
                            S.act(hT[:, k, j * 128:(j + 1) * 128], pb[:, k * 128:(k + 1) * 128], AF.Identity,
                                  scale=Gs[:, k:k + 1], bias=shf[:, k:k + 1])
                        yield

                def genA2(t):
                    T0 = 512 * t
                    hT, rq, rk, vret, sg = hTs[t % 2], rqs[t % 2], rks[t % 2], vrets[t % 2], sgs[t % 2]
                    S.dma("sp", tab, tabs_d.re("a p n -> p a n")[:, :, T0:T0 + 512])

                    def proj_fm(wsel, M):
                        p_ = next_ps()
                        for k in range(8):
                            S.mm(p_[0:M, :], wsel(k), hT[:, k, :], start=(k == 0), stop=(k == 7))
                        return p_

                    for p in range(4):
                        pq = proj_fm(lambda k, p=p: win[:, k, 128 * p:128 * (p + 1)], 128)
                        rotary(pq, 128, K["permR"], tab[:, 0, :], tab[:, 1, :], rq[:, p, :], 0.125)
                        yield
                        pk = proj_fm(lambda k, p=p: win[:, k, 512 + 128 * p:512 + 128 * (p + 1)], 128)
                        rotary(pk, 128, K["permR"], tab[:, 0, :], tab[:, 1, :], rk[:, p, :], 1.0)
                        yield
                    for p in range(4):
                        pq = proj_fm(lambda k, p=p: win[:, k, 2048 + 128 * p:2048 + 128 * (p + 1)], 128)
                        qb = qn[p % 2]

                        def st_q(p=p, qb=qb):
                            S.dma("sp", qT_d[2 * p:2 * p + 2].re("h d n -> (h d) n")[:, T0:T0 + 512], qb,
                                  sembuf=qb.bufs[0])
                        rotary(pq, 128, K["permN"], tab[:, 2, :], tab[:, 3, :], qb, 0.125, then=st_q)
                        yield
                    for i4, (c0, dst) in enumerate(((2816, ks_d), (2880, ks_d), (3072, kw_d), (3136, kw_d))):
                        g = i4 % 2
                        pk = proj_fm(lambda k, c0=c0: win[:, k, c0:c0 + 64], 64)
                        kb = kst[i4 % 2]

                        def st_k(dst=dst, g=g, kb=kb):
                            S.dma("sp", dst[:, g, T0:T0 + 512], kb, sembuf=kb.bufs[0])
                        rotary(pk[0:64, :], 64, K["permN"][0:64, 0:64], tab[0:64, 2, :], tab[0:64, 3, :], kb, 1.0,
                               then=st_k)
                        yield
                    KCc, KCp = KC[t % 2], KC[(t + 1) % 2]
                    for kv in range(2):
                        for g in range(2):
                            pk = proj_fm(lambda k, i4=kv * 2 + g: wkc[:, k, i4, :], 128)
                            S.copy(KCc[0:64, kv, g, 16:528], pk[0:64, :], eng="act")
                            S.copy(KCc[64:128, kv, g, 15:527], pk[64:128, :], eng="act")
                            if kv == 0 and g == 0:
                                rot_flush()
                            yield
                    if t > 0:
                        S.copy(KCc[0:64, :, :, 0:16], KCp[0:64, :, :, 512:528], eng="pool")
                        S.copy(KCc[64:128, :, :, 0:15], KCp[64:128, :, :, 512:527], eng="pool")

                    for j in range(4):
                        tt_ = 4 * t + j
                        lhs = lambda k: hT[:, k, j * 128:(j + 1) * 128]
                        for gi, (c0, n) in enumerate(((1024, 512), (1536, 512), (3352, 512), (2944, 408))):
                            p_ = next_ps()
                            for k in range(8):
                                S.mm(p_[:, 0:n], lhs(k), win[:, k, c0:c0 + n], start=(k == 0), stop=(k == 7))
                            if gi == 0:
                                S.copy(vret[:, j, :], p_, eng="act")
                            elif gi == 1:
                                S.act(sg[:, j, :], p_, AF.Silu)
                            elif gi == 2:
                                sb_ = sng[tt_ % 2]
                                S.act(sb_, p_, AF.Silu)
                                S.dma("sp", sng_d[tt_ * 128:(tt_ + 1) * 128, :], sb_, sembuf=sb_.bufs[0])
                            else:
                                vb = vst[tt_ % 2]
                                S.copy(vb[:, 0].re("p g d -> p (g d)"), p_[:, 0:128])
                                S.copy(vb[:, 1].re("p g d -> p (g d)"), p_[:, 256:384])
                                S.dma("sp", vs_d[tt_ * 128:(tt_ + 1) * 128], vb[:, 0], sembuf=vb.bufs[0])
                                S.dma("sp", vw_d[tt_ * 128:(tt_ + 1) * 128], vb[:, 1], sembuf=vb.bufs[0])
                                gb = gst[tt_ % 2]
                                S.act(gb, p_[:, 384:408], AF.Sigmoid)
                                S.dma("sp", gates_d[tt_ * 128:(tt_ + 1) * 128, :], gb, sembuf=gb.bufs[0])
                            yield

                    for kv in range(2):
                        for hc in range(2):
                            p_ = next_ps()
                            po = p_[:, 0:64].re("p (g r) -> p g r", g=2)
                            for j in range(16):
                                S.mm(po, w1[:, kv, j, hc * 128:(hc + 1) * 128],
                                     KCc[:, kv, :, 2 * j:2 * j + 16 * 31 + 1:16], start=(j == 0), stop=(j == 15))
                            S.act(hid[:, kv, hc, :], p_[:, 0:64], AF.Silu, bias=b1[:, kv, hc:hc + 1])
                            yield
                        p_ = next_ps()
                        for hc in range(2):
                            S.mm(p_[0:64, 0:64], w2[:, kv, hc, :], hid[:, kv, hc, :], start=(hc == 0), stop=(hc == 1))
                        r0 = 1 if t == 0 else 0
                        n0 = 32 * t - 1
                        for g in range(2):
                            dst = kvcT[:, kv, g, n0 + r0:n0 + 32]
                            src = p_[0:64, g * 32 + r0:g * 32 + 32]
                            if kv == 1:
                                S.copy(dst, src)
                            else:
                                S.copy(kcx[:, g * 32:(g + 1) * 32], p_[0:64, g * 32:(g + 1) * 32], eng="act")
                        if kv == 0:
                            yield
                            t1, t2 = t1s[0], t2s[0]
                            S.mm(ps[7][0:64, 0:64], K["permN"][0:64, 0:64], kcx)
                            for g in range(2):
                                cs0 = ctab[:, 0, n0 + r0:n0 + 32]
                                sn0 = ctab[:, 1, n0 + r0:n0 + 32]
                                S.tt(t1[0:64, 0:32 - r0], ps[7][0:64, g * 32 + r0:g * 32 + 32], sn0, ALU.mult)
                                S.tt(t2[0:64, 0:32 - r0], kcx[:, g * 32 + r0:g * 32 + 32], cs0, ALU.mult)
                                S.tt(kvcT[:, 0, g, n0 + r0:n0 + 32], t1[0:64, 0:32 - r0], t2[0:64, 0:32 - r0],
                                     ALU.add)
                        yield

                def genA(t):
                    a1 = genA1(t + 1) if t + 1 < NS else None
                    for i, _ in enumerate(genA2(t)):
                        yield
                        if a1 is not None and i % 4 == 3:
                            try:
                                next(a1)
                                yield
                            except StopIteration:
                                a1 = None
                    if a1 is not None:
                        for _ in a1:
                            yield

                def genR(t):
                    hT, rq, rk, vret, sg = hTs[t % 2], rqs[t % 2], rks[t % 2], vrets[t % 2], sgs[t % 2]
                    zb = V(K["zeta"].ap.unsqueeze(2).to_broadcast([128, 8, 64]), K["zeta"].bufs)

                    def stage1(j):
                        cs = slice(j * 128, (j + 1) * 128)
                        scb, kz = scbs[j % 2], kzs[j % 2]
                        for p in range(4):
                            for hh in range(2):
                                rows = slice(hh * 64, hh * 64 + 64)
                                S.mm(ps[4 + hh][:, p * 128:(p + 1) * 128], rk[rows, p, cs], rq[rows, p, cs])
                        for hh in range(2):
                            S.tt(scb[:, hh::2, :], ps[4 + hh].re("p (h i) -> p h i", h=4),
                                 K["decayT"][:, hh::2, :], ALU.mult)
                        pb = ps[6].cast(BF16)
                        for p in range(4):
                            S.tr(pb[:, p * 128:(p + 1) * 128], rk[:, p, cs], K["ident_b"])
                        S.tt(kz.re("p (h d) -> p h d", h=8), pb[:, 0:512].re("p (h d) -> p h d", h=8), zb, ALU.mult)

                    stage1(0)
                    yield
                    for j in range(4):
                        tt_ = 4 * t + j
                        cs = slice(j * 128, (j + 1) * 128)
                        scb, kz = scbs[j % 2], kzs[j % 2]
                        pi_ = ps[3]
                        for h in range(8):
                            S.mm(pi_[:, h * 64:(h + 1) * 64], scb[:, h, :], vret[:, j, h * 64:(h + 1) * 64])
                        for p in range(4):
                            for hh in range(2):
                                rows = slice(hh * 64, hh * 64 + 64)
                                S.mm(ps[4 + hh][:, p * 64:(p + 1) * 64], rq[rows, p, cs],
                                     Rb[rows, p, hh * 64:hh * 64 + 64])
                        pkv = ps[6]
                        for p in range(4):
                            S.mm(pkv[:, p * 128:(p + 1) * 128], kz[:, p * 128:(p + 1) * 128],
                                 vret[:, j, p * 128:(p + 1) * 128])
                        for hh in range(2):
                            xib = V(K["xi"].ap[:, hh::2].unsqueeze(2).to_broadcast([128, 4, 64]), K["xi"].bufs)
                            S.tt(o1[:, hh::2, :], ps[4 + hh][:, 0:256].re("p (h d) -> p h d", h=4), xib, ALU.mult)
                        S.tt(o1, o1, pi_.re("p (h d) -> p h d", h=8), ALU.add)
                        for p in range(4):
                            S.stt(R32[:, p, :], R32[:, p, :], K["cdv"][:, p:p + 1], pkv[:, p * 128:(p + 1) * 128],
                                  ALU.mult, ALU.add)
                        S.copy(Rb, R32, eng="pool")
                        yield
                        if j + 1 < 4:
                            stage1(j + 1)
                            yield
                        S.reduce(st8[:, 0, :], o1, ALU.add)
                        S.tt(o2, o1, o1, ALU.mult, eng="pool")
                        S.reduce(st8[:, 1, :], o2, ALU.add)
                        S.ts(st8[:, 0, :], st8[:, 0, :], 1.0 / 64, None, ALU.mult)
                        S.tt(st8[:, 2, :], st8[:, 0, :], st8[:, 0, :], ALU.mult)
                        S.stt(st8[:, 1, :], st8[:, 1, :], 1.0 / 64, st8[:, 2, :], ALU.mult, ALU.subtract)
                        S.ts(st8[:, 1, :], st8[:, 1, :], 1e-5, None, ALU.add)
                        yield
                        S.act(st8[:, 1, :], st8[:, 1, :], AF.Sqrt)
                        yield
                        S.recip(st8[:, 3, :], st8[:, 1, :])
                        yield
                        mb = V(st8.ap[:, 0, :].unsqueeze(2).to_broadcast([128, 8, 64]), st8.bufs)
                        rb_ = V(st8.ap[:, 3, :].unsqueeze(2).to_broadcast([128, 8, 64]), st8.bufs)
                        S.tt(o2, o1, mb, ALU.subtract)
                        S.tt(o2, o2, rb_, ALU.mult)
                        mo = mst[tt_ % 2]
                        S.tt(mo, o2.re("p h d -> p (h d)"), sg[:, j, :], ALU.mult)
                        S.dma("sp", mret_d[tt_ * 128:(tt_ + 1) * 128, :], mo, sembuf=mo.bufs[0])
                        yield

                def interleave(ga, gr, ratio=2):
                    a_done = ga is None
                    r_done = gr is None
                    while not (a_done and r_done):
                        if not r_done:
                            try:
                                next(gr)
                            except StopIteration:
                                r_done = True
                        for _ in range(ratio):
                            if not a_done:
                                try:
                                    next(ga)
                                except StopIteration:
                                    a_done = True

                for _ in genA1(0):
                    pass
                interleave(genA(0), None)
                for t in range(NS):
                    interleave(genA(t + 1) if t + 1 < NS else None, genR(t))
                hT = hTs[(NS - 1) % 2]

                if "passA" in debug:
                    S.barrier()
                    d_hT = dbg_out("hT", [128, 8, 512], BF16)
                    S.dma("sp", d_hT, hT, is_output=True)
                    d_kvc = dbg_out("kvcT", [64, 2, 2, 256], BF16)
                    S.dma("sp", d_kvc, kvcT, is_output=True)
                    for nm, src in (("qT", qT_d), ("sng", sng_d), ("gates", gates_d), ("mret", mret_d), ("ks", ks_d),
                                    ("kw", kw_d), ("vs", vs_d), ("vw", vw_d), ("tabs", tabs_d)):
                        shp = list(src.ap.shape)
                        dd = dbg_out(nm, shp, src.ap.dtype)
                        S.dma("sp", dd, src, is_output=True)
                    dG = dbg_out("gGb", [128, D])
                    S.dma("sp", dG, gGb, is_output=True)
            S.barrier()

        if "passA" in debug:
            with nc.Block() as block:
                S.emit(block)
            return nc, consts

        PASSB(nc, S, st0, alloc, ps, K, cd_, consts, kvcT, gGb, x_d, wout_d, out_d, qT_d, sng_d, gates_d, mret_d,
              ks_d, kw_d, vs_d, vw_d, debug, dbg_out)
        with nc.Block() as block:
            S.emit(block)
    return nc, consts


def PASSB(nc, S, st0, alloc, ps, K, cd_, consts, kvcT, gGb, x_d, wout_d, out_d, qT_d, sng_d, gates_d, mret_d,
          ks_d, kw_d, vs_d, vw_d, debug, dbg_out):
    with ExitStack() as stB:
        wout = alloc(stB, "wout", [128, 8, D], BF16)
        for k in range(8):
            S.dma("pool", wout[:, k, :], wout_d[k * 128:(k + 1) * 128, :])
        late_dmas = []
        ksA = alloc(stB, "ksA", [128, 2, S_LEN], BF16)
        kwT = alloc(stB, "kwT", [128, 2, S_LEN], BF16)
        S.memset(kwT[64:128], 0.0)
        kcA = alloc(stB, "kcA", [128, 2, 256], BF16)
        S.memset(kcA[64:128], 0.0)
        S.copy(kcA[0:64], kvcT[:, 0])
        vsA = alloc(stB, "vsA", [128, NT, 2, 65], BF16)
        vwA = alloc(stB, "vwA", [128, NT, 2, 65], BF16)
        S.memset(vsA[:, :, :, 64:65], 1.0)
        S.memset(vwA[:, :, :, 64:65], 1.0)
        late_dmas.append(lambda: S.dma("sp", kwT[0:64], kw_d))
        for g in range(2):
            late_dmas.append(lambda g=g: S.dma("sp", vwA[:, :, g, 0:64],
                                               vw_d.re("(t p) g d -> p t g d", p=128)[:, :, g, :]))
        late_dmas.append(lambda: S.dma("sp", ksA[0:64], ks_d))
        for g in range(2):
            late_dmas.append(lambda g=g: S.dma("sp", ksA[64:128, g, :], cd_["onehot"]))
        for g in range(2):
            late_dmas.append(lambda g=g: S.dma("sp", vsA[:, :, g, 0:64],
                                               vs_d.re("(t p) g d -> p t g d", p=128)[:, :, g, :]))
        vcA = alloc(stB, "vcA", [128, 2, 2, 65], BF16)
        S.memset(vcA, 1.0)
        for g in range(2):
            for nt in range(2):
                pb = ps[7].cast(BF16)
                S.tr(pb[:, 0:64], kvcT[:, 1, g, nt * 128:(nt + 1) * 128], K["ident_b"][0:64, 0:64])
                S.copy(vcA[:, nt, g, 0:64], pb[:, 0:64])
        Qs = [alloc(stB, "Qa%d" % i, [128, 8, 512], BF16) for i in range(2)]
        Qlo = [Buf("Qlo%d" % i) for i in range(2)]
        Qhi = [[Buf("Qhi%d_%d" % (i, g)) for g in range(2)] for i in range(2)]
        for i in range(2):
            S.memset(V(Qs[i].ap[64:128], Qhi[i]), 0.0)
        cmk = [alloc(stB, "cmk%d" % i, [128, 2, 512], BF16) for i in range(2)]
        fbt = [alloc(stB, "fbt%d" % i, [128, 4, 64], F32) for i in range(2)]
        gts = [alloc(stB, "gts%d" % i, [128, 4, 24], F32) for i in range(2)]
        sngb = [alloc(stB, "sngb%d" % i, [128, 4, 512], BF16) for i in range(2)]
        NPT = 10
        PT = [alloc(stB, "PT%d" % i, [128, 512], BF16) for i in range(NPT)]
        accs = [alloc(stB, "acc%d" % i, [128, 4, 512], F32) for i in range(2)]
        impacc = alloc(stB, "impacc", [128, 4, 64], F32)
        rls = [alloc(stB, "rl%d" % i, [128, 4], F32) for i in range(2)]
        sc4s = [alloc(stB, "sc4%d" % i, [128, 4], F32) for i in range(2)]
        scr = alloc(stB, "scr", [128, 64], F32)
        wk64 = alloc(stB, "wk64", [128, 64], F32)
        m8a = alloc(stB, "m8a", [128, 8], F32)
        m8b = alloc(stB, "m8b", [128, 8], F32)
        thr = alloc(stB, "thr", [128, 1], F32)
        selbs = [alloc(stB, "selb%d" % i, [128, 128], BF16) for i in range(4)]
        for i in range(4):
            S.memset(selbs[i], 0.0)
        scrs = [alloc(stB, "scr%d" % i, [128, 64], F32) for i in range(4)]
        wk64s = [alloc(stB, "wk64%d" % i, [128, 64], F32) for i in range(4)]
        m8as = [alloc(stB, "m8a%d" % i, [128, 8], F32) for i in range(4)]
        m8bs = [alloc(stB, "m8b%d" % i, [128, 8], F32) for i in range(4)]
        thrs = [alloc(stB, "thr%d" % i, [128, 1], F32) for i in range(4)]
        mix = [alloc(stB, "mix%d" % i, [128, D], BF16) for i in range(4)]
        mixTs = [alloc(stB, "mixT%d" % i, [128, 8, 128], BF16) for i in range(4)]
        xres = [alloc(stB, "xres%d" % i, [128, D], F32) for i in range(4)]
        zts = [alloc(stB, "zt%d" % i, [128, D], F32) for i in range(2)]
        junk2 = alloc(stB, "junk2", [128, D], BF16)
        ss2s = [alloc(stB, "ss2%d" % i, [128, 4], F32) for i in range(2)]
        ot = [alloc(stB, "ot%d" % i, [128, D], F32) for i in range(2)]
        cyc = {"pt": 0, "sc": 0, "o": 0, "imp": 0, "ev": 0}
        LOOK = 6
        pend = []

        fins = []

        def run_fins(limit_hseq=None, tick=False):
            keep = []
            ready = []
            for ent in fins:
                if tick:
                    ent[0] -= 1
                if ent[0] <= 0 or (limit_hseq is not None and ent[1] <= limit_hseq):
                    ready.append(ent)
                else:
                    keep.append(ent)
            if ready:
                last = max(fins.index(e) for e in ready)
                ready = fins[:last + 1]
                keep = fins[last + 1:]
            fins[:] = keep
            for ent in ready:
                ent[2]()

        def push(s1, s2, hseq, first_of_head):
            if first_of_head:
                while pend and pend[0][0] <= hseq - 2:
                    pend.pop(0)[1]()
                run_fins(limit_hseq=hseq - 2)
            s1()
            pend.append((hseq, s2))
            while len(pend) > LOOK:
                pend.pop(0)[1]()
            run_fins(tick=True)

        def flush():
            while pend or fins:
                while pend:
                    pend.pop(0)[1]()
                run_fins(limit_hseq=1 << 60)

        def load_inputs(t):
            T0 = 512 * t
            sl = t % 2
            S.dma("sp", V(Qs[sl].ap[0:64], [Qlo[sl]]), qT_d.re("h d n -> d h n")[:, :, T0:T0 + 512], sembuf=Qlo[sl])
            S.dma("sp", cmk[sl], cd_["cmpmask"][t].re("a p n -> p a n"))
            S.dma("sp", fbt[sl], cd_["fb"][4 * t:4 * t + 4].re("a p n -> p a n"))
            S.dma("sp", gts[sl], gates_d[T0:T0 + 512].re("(a p) n -> p a n", p=128))
            S.dma("sp", sngb[sl], sng_d[T0:T0 + 512].re("(a p) n -> p a n", p=128))

        oTs = [alloc(stB, "oT%d" % i, [65, 512], F32) for i in range(2)]
        zeros_f = alloc(stB, "zeros_f", [128, 272], F32)
        S.memset(zeros_f, 0.0)

        def attend(t, h, br, first, ktl, with_imp=False, after=None, otrans=False, act_off=False):
            sl = t % 2
            Q = Qs[sl]
            ob = ps[3 + cyc["o"] % 2]
            cyc["o"] += 1
            impb = None
            tb = None
            if with_imp or otrans:
                impb = ps[5 + cyc["imp"] % 2]
                cyc["imp"] += 1
            n = len(ktl)
            cyc["hseq"] = cyc.get("hseq", 0) + 1
            hseq = cyc["hseq"]

            def evac(src):
                rl = rls[cyc["ev"] % 2]
                sc4 = sc4s[cyc["ev"] % 2]
                cyc["ev"] += 1
                o3 = src[:, 0:272].re("p (q c) -> p q c", q=4)
                S.ts(rl, o3[:, :, 64], 1.0e-30, None, ALU.max)
                S.recip(rl, rl)
                S.tt(sc4, rl, gts[sl][:, :, 3 * h + br], ALU.mult)
                for qs in range(4):
                    dst = accs[t % 2][:, qs, h * 64:(h + 1) * 64]
                    if first and act_off:
                        S.act(dst, o3[:, qs, 0:64], AF.Copy, scale=sc4[:, qs:qs + 1])
                    elif first:
                        S.ts(dst, o3[:, qs, 0:64], sc4[:, qs:qs + 1], None, ALU.mult)
                    else:
                        S.stt(dst, o3[:, qs, 0:64], sc4[:, qs:qs + 1], dst, ALU.mult, ALU.add)
                return rl

            for idx, (kT, va, Krows, M, c0, c1, mask, ov) in enumerate(ktl):
                sp_ = ps[cyc["sc"] % 3]
                cyc["sc"] += 1
                pt = PT[cyc["pt"] % NPT]
                cyc["pt"] += 1

                def s1(idx=idx, kT=kT, Krows=Krows, M=M, c0=c0, c1=c1, mask=mask, sp_=sp_, pt=pt):
                    if idx == 0 and not otrans:
                        if act_off:
                            S.act(ob[:, 0:272], zeros_f[:, 0:272], AF.Copy)
                            S.act(impb[:, 0:272], zeros_f[:, 0:272], AF.Copy)
                        else:
                            S.memset(ob[:, 0:272], 0.0)
                            if impb is not None:
                                S.memset(impb[:, 0:272], 0.0)
                    qv = V(Q.ap[0:Krows, h, c0:c1], [Qlo[sl]] + ([Qhi[sl][h // 4]] if Krows == 128 else []))
                    S.mm(sp_[0:M, c0:c1], kT, qv, start=True, stop=(mask is None))
                    if mask is not None:
                        mv, m0, m1 = mask
                        S.mm(sp_[0:M, m0:m1], K["ident_b"][0:M, 0:M], mv, start=False, stop=True)
                    S.act(pt[0:M, c0:c1], sp_[0:M, c0:c1], AF.Exp)

                def s2(idx=idx, va=va, M=M, c0=c0, c1=c1, ov=ov, pt=pt):
                    if otrans:
                        assert idx > 0 or (c0 == 0 and c1 == 512)
                        S.mm(ob[0:65, c0:c1], va, pt[0:M, c0:c1], start=(idx == 0), stop=(idx == n - 1))
                        if idx == n - 1:
                            oT = oTs[cyc.get("ot", 0) % 2]
                            cyc["ot"] = cyc.get("ot", 0) + 1
                            S.copy(oT, ob[0:65, :])

                            def fin(oT=oT):
                                for qs in range(4):
                                    S.tr(impb[:, qs * 68:qs * 68 + 65], oT[0:65, qs * 128:(qs + 1) * 128],
                                         K["ident_f"][0:65, 0:65])
                                evac(impb)
                            fins.append([2, hseq, fin])
                        return
                    for qs in range(c0 // 128, c1 // 128):
                        S.mm(ob[:, qs * 68:qs * 68 + 65], pt[0:M, qs * 128:(qs + 1) * 128], va,
                             start=False, stop=False, skip_group_check=True)
                        if ov is not None:
                            S.mm(impb[:, qs * 68:qs * 68 + 65], pt[0:M, qs * 128:(qs + 1) * 128], ov,
                                 start=False, stop=False, skip_group_check=True)
                    if idx == n - 1:
                        rl = evac(ob)
                        if after is not None:
                            after(impb, rl)

                push(s1, s2, hseq, idx == 0)

        def b1_head(t, h, act_off):
            T0 = 512 * t
            sl = t % 2
            Q = Qs[sl]
            g, h4 = h // 4, h % 4
            nts = [nt for nt in range(2) if 16 * 128 * nt + 31 <= T0 + 511]
            ktl = []
            for nt in nts:
                M = 128 if nt == 0 else 127
                ktl.append((kcA[:, g, nt * 128:nt * 128 + M], vcA[0:M, nt, g, :], 128, M, 0, 512,
                            (cmk[sl][0:M, nt, :], 0, 512), K["ovl"][0:M, nt, :]))

            def after(impb, rl):
                i3 = impb[:, 0:272].re("p (q c) -> p q c", q=4)
                for qs in range(4):
                    if h4 == 0:
                        S.ts(impacc[:, qs, :], i3[:, qs, 0:64], rl[:, qs:qs + 1], None, ALU.mult)
                    else:
                        S.stt(impacc[:, qs, :], i3[:, qs, 0:64], rl[:, qs:qs + 1], impacc[:, qs, :],
                              ALU.mult, ALU.add)
                if h4 < 3:
                    return
                for qs in range(4):
                    S.tt(scrs[qs], impacc[:, qs, :], fbt[sl][:, qs, :], ALU.add)
                for qs in range(4):
                    S.max8(m8as[qs], scrs[qs])
                for qs in range(4):
                    S.match_replace(wk64s[qs], m8as[qs], scrs[qs], -3.0e38)
                for qs in range(4):
                    S.max8(m8bs[qs], wk64s[qs])
                for qs in range(4):
                    S.reduce(thrs[qs], m8bs[qs], ALU.min)
                for qs in range(4):
                    S.ts(thrs[qs], thrs[qs], -1.0e29, None, ALU.max)
                for qs in range(4):
                    S.ts(selbs[qs][:, 64:128], scrs[qs], thrs[qs], None, ALU.is_ge)
                for qs in range(4):
                    S.ts(selbs[qs][:, 64:128], selbs[qs][:, 64:128], 1.0, 30000.0, ALU.subtract, ALU.mult)
                pb = ps[7].cast(BF16)
                for qs in range(4):
                    S.tr(pb[:, qs * 128:(qs + 1) * 128], selbs[qs], K["ident_b"])
                for qs in range(4):
                    for hh in range(4):
                        dst = V(Q.ap[64:128, 4 * g + hh, qs * 128:(qs + 1) * 128], [Qhi[sl][g]])
                        S.copy(dst, pb[64:128, qs * 128:(qs + 1) * 128], eng=("act" if act_off else "dve"))

            attend(t, h, 0, True, ktl, with_imp=True, after=after, act_off=act_off)

        def b2_head(t, h):
            g = h // 4
            ktl = []
            for m in (4, 0, 1, 2, 3, 5, 6, 7):
                kti = 4 * (t - 1) + m
                if kti < 0:
                    continue
                if m <= 3:
                    c0, c1 = 0, 128 * (m + 1)
                    mask = (K["tri"][:, 1, :], 128 * m, 128 * m + 128)
                else:
                    c0, c1 = 128 * (m - 4), 512
                    mask = (K["tri"][:, 0, :], c0, c0 + 128)
                ktl.append((kwT[:, g, kti * 128:(kti + 1) * 128], vwA[:, kti, g, :], 128, 128, c0, c1, mask, None))
            attend(t, h, 2, False, ktl)

        def b3_head(t, h):
            g = h // 4
            ktl = []
            for kt in range(4 * t + 4):
                if kt < 4 * t:
                    c0, c1, mask = 0, 512, None
                else:
                    c0, c1 = 128 * (kt - 4 * t), 512
                    mask = (K["tri"][:, 0, :], c0, c0 + 128)
                ktl.append((ksA[:, g, kt * 128:(kt + 1) * 128], vsA[:, kt, g, :], 128, 128, c0, c1, mask, None))
            attend(t, h, 1, False, ktl)

        def make_b4(t):
            sl = t % 2

            def b4mix():
                for qs in range(4):
                    S.tt(mix[qs][:, 512:1024], accs[t % 2][:, qs, :], sngb[sl][:, qs, :], ALU.mult)

            def b4a(qs):
                mx = mix[qs]
                pb = ps[7].cast(BF16)
                for k in range(8):
                    S.tr(pb[:, k * 128:(k + 1) * 128], mx[:, k * 128:(k + 1) * 128], K["ident_b"])
                S.copy(mixTs[qs].re("p k n -> p (k n)"), pb, eng="act")

            def b4b(qs):
                tt_ = 4 * t + qs
                xr, mT = xres[qs], mixTs[qs]
                zt_, ssb = zts[tt_ % 2], ss2s[tt_ % 2]
                for half in range(2):
                    zp = ps[5 + half]
                    for k in range(8):
                        S.mm(zp, mT[:, k, :], wout[:, k, half * 512:(half + 1) * 512], start=(k == 0), stop=(k == 7))
                    S.act(junk2[:, half * 512:(half + 1) * 512], zp, AF.Square, accum_out=ssb[:, half:half + 1])
                    S.tt(zt_[:, half * 512:(half + 1) * 512], zp, gGb[:, half * 512:(half + 1) * 512], ALU.mult)
                S.tt(ssb[:, 2:3], ssb[:, 0:1], ssb[:, 1:2], ALU.add)
                S.ts(ssb[:, 2:3], ssb[:, 2:3], 1.0 / D, 1e-6, ALU.mult, ALU.add)
                S.act(ssb[:, 2:3], ssb[:, 2:3], AF.Ln)
                S.act(ssb[:, 3:4], ssb[:, 2:3], AF.Exp, scale=-0.5)
                o_ = ot[tt_ % 2]
                S.stt(o_, zt_, ssb[:, 3:4], xr, ALU.mult, ALU.add)
                S.dma("sp", out_d[tt_ * 128:(tt_ + 1) * 128, :], o_, sembuf=o_.bufs[0], is_output=True)

            return b4mix, [lambda: b4a(0), lambda: b4a(1), lambda: b4a(2), lambda: b4a(3),
                           lambda: b4b(0), lambda: b4b(1), lambda: b4b(2), lambda: b4b(3)]

        load_inputs(0)
        for fn in late_dmas:
            fn()
        for h in range(8):
            b1_head(0, h, True)
        b4_prev = None
        for t in range(NS):
            for h in range(8):
                b2_head(t, h)
                if b4_prev is not None:
                    b4_prev[h]()
            if t + 1 < NS:
                load_inputs(t + 1)
            for qs in range(4):
                tt_ = 4 * t + qs
                S.dma("sp", mix[qs][:, 0:512], mret_d[tt_ * 128:(tt_ + 1) * 128, :])
                S.dma("sp", xres[qs], x_d[tt_ * 128:(tt_ + 1) * 128, :])
            for h in range(8):
                b3_head(t, h)
                if t + 1 < NS:
                    b1_head(t + 1, h, False)
            flush()
            b4mix, b4_prev = make_b4(t)
            b4mix()
        for st_ in b4_prev:
            st_()


def core_inputs(inp, consts, b):
    m = {
        "x": np.ascontiguousarray(inp["x"][b]),
        "c": np.ascontiguousarray(inp["c"][b]),
        "positions": np.ascontiguousarray(inp["positions"][b:b + 1]).astype(np.int32),
        "w_ada": np.ascontiguousarray(inp["w_ada"][0]),
        "b_ada": np.ascontiguousarray(inp["b_ada"][0]),
        "g_pre": np.ascontiguousarray(inp["g_pre"][0]),
        "g_post": np.ascontiguousarray(inp["g_post"][0]),
        "w_in": np.ascontiguousarray(inp["w_in"][0]),
        "w_out": np.ascontiguousarray(inp["w_out"][0]),
        "cmp_pe_k": np.ascontiguousarray(inp["cmp_pe_k"][0]).reshape(-1),
        "cmp_w1_k": np.ascontiguousarray(inp["cmp_w1_k"][0]),
        "cmp_w2_k": np.ascontiguousarray(inp["cmp_w2_k"][0]),
        "cmp_pe_v": np.ascontiguousarray(inp["cmp_pe_v"][0]).reshape(-1),
        "cmp_w1_v": np.ascontiguousarray(inp["cmp_w1_v"][0]),
        "cmp_w2_v": np.ascontiguousarray(inp["cmp_w2_v"][0]),
    }
    for k, v in consts.items():
        m["k_" + k] = v
    return m


_CACHE = {}


def kernel(**inputs):
    if "nc" not in _CACHE:
        _CACHE["nc"] = build()
    nc, consts = _CACHE["nc"]
    inp = {k: np.asarray(v) for k, v in inputs.items()}
    in_maps = [core_inputs(inp, consts, b) for b in range(8)]
    res = run_bass_kernel_spmd(nc, in_maps, core_ids=list(range(8)))
    out = np.stack([np.asarray(r["out"]) for r in res.results], axis=0)
    return out.astype(np.float32)
```
